# Optimizing a Trainium2 kernel written in Bass

```python
import jax, jax.numpy as jnp
from jax import lax
import numpy as np

D_MODEL = 1024
BATCH = 16
SEQ = 4096
DEPTH = 2

MEM_LEN = 256
ML_HEADS = 4
ML_DQK = 64
ML_DV = 128
ML_CONV = 4
ML_CHUNK = 64
ML_W = ML_HEADS * ML_DV
ML_QK_W = 2 * ML_HEADS * ML_DQK
RW_HEADS = 8
RW_DH = 64
RW_W = RW_HEADS * RW_DH
RW_DECAY_LORA = 64
RW_AAA_LORA = 64
RW_GATE_LORA = 128
RW_IN_W = 3 * RW_W + RW_DECAY_LORA + RW_AAA_LORA + RW_GATE_LORA
RW_SIZES = (RW_W, RW_W, RW_W, RW_DECAY_LORA, RW_AAA_LORA, RW_GATE_LORA)
RW_GN_EPS = 64e-5
CA_HEADS = 4
CA_DH = 128
CA_W = CA_HEADS * CA_DH
N_BRANCH = 3
IN_SIZES = (ML_QK_W, ML_W, ML_W, ML_HEADS, ML_HEADS, RW_IN_W, CA_W, N_BRANCH * D_MODEL)
D_IN = sum(IN_SIZES)
N_EXPERTS = 32
TOP_K = 4
D_FF = 1024
SWIGLU_LIMIT = 7.0
SWIGLU_ALPHA = 1.702
MOE_BLOCK = 128
DN_ALPHA = (2 * DEPTH) ** 0.25
DN_BETA = (8 * DEPTH) ** -0.25
LN_EPS = 1e-5

kernel_name = 'hybrid_mlstm_rwkv7_xattn_moe_deepnorm'


def split_cols(u, sizes):
    idx, acc = [], 0
    for s in sizes[:-1]:
        acc += s
        idx.append(acc)
    return jnp.split(u, idx, axis=-1)


def layer_norm(x, g, b, eps=LN_EPS):
    xf = x.astype(jnp.float32)
    mu = jnp.mean(xf, axis=-1, keepdims=True)
    var = jnp.mean(jnp.square(xf - mu), axis=-1, keepdims=True)
    return ((xf - mu) * lax.rsqrt(var + eps) * g + b).astype(x.dtype)


def head_norm(h, eps):
    hf = h.astype(jnp.float32)
    mu = jnp.mean(hf, axis=-1, keepdims=True)
    var = jnp.mean(jnp.square(hf - mu), axis=-1, keepdims=True)
    y = (hf - mu) * lax.rsqrt(var + eps)
    return y.reshape(*h.shape[:-2], h.shape[-2] * h.shape[-1])


def causal_dwconv(u, w, b):
    c = u.shape[-1]
    y = lax.conv_general_dilated(u, w[:, None, :].astype(u.dtype), window_strides=(1,),
                                 padding=[(w.shape[0] - 1, 0)],
                                 dimension_numbers=('NWC', 'WIO', 'NWC'),
                                 feature_group_count=c)
    return y + b


def token_shift(u, mu):
    u_prev = jnp.pad(u[:, :-1], ((0, 0), (1, 0), (0, 0)))
    return u + (u_prev - u) * mu


def mlstm_chunkwise(q, k, v, ig, lf):
    B, S, H, dk = q.shape
    L = ML_CHUNK
    nc = S // L

    def to_chunks(t):
        t = t.reshape(B, nc, L, H, *t.shape[3:])
        return jnp.moveaxis(t, (1, 3), (0, 2))

    causal = jnp.tril(jnp.ones((L, L), dtype=bool))

    def step(carry, inp):
        C, n, m = carry
        qc, kc, vc, ic, fc = inp
        bcum = jnp.cumsum(fc, axis=-1)
        dmat = bcum[..., :, None] - bcum[..., None, :] + ic[..., None, :]
        dmat = jnp.where(causal, dmat, -jnp.inf)
        m_inter = bcum + m[..., None]
        m_t = jnp.maximum(m_inter, jnp.max(dmat, axis=-1))
        w_intra = jnp.exp(dmat - m_t[..., None]) * jnp.einsum('bhtk,bhsk->bhts', qc, kc)
        s_inter = jnp.exp(m_inter - m_t)
        num = (s_inter[..., None] * jnp.einsum('bhvk,bhtk->bhtv', C, qc)
               + jnp.einsum('bhts,bhsv->bhtv', w_intra, vc))
        den = s_inter * jnp.einsum('bhk,bhtk->bht', n, qc) + jnp.sum(w_intra, axis=-1)
        h = num / jnp.maximum(jnp.abs(den), jnp.exp(-m_t))[..., None]
        b_last = bcum[..., -1]
        g = b_last[..., None] - bcum + ic
        m_new = jnp.maximum(b_last + m, jnp.max(g, axis=-1))
        carry_scale = jnp.exp(b_last + m - m_new)
        wg = jnp.exp(g - m_new[..., None])
        C_new = carry_scale[..., None, None] * C + jnp.einsum('bhs,bhsv,bhsk->bhvk', wg, vc, kc)
        n_new = carry_scale[..., None] * n + jnp.einsum('bhs,bhsk->bhk', wg, kc)
        return (C_new, n_new, m_new), h

    dv = v.shape[-1]
    init = (jnp.zeros((B, H, dv, dk), jnp.float32), jnp.zeros((B, H, dk), jnp.float32),
            jnp.zeros((B, H), jnp.float32))
    xs = (to_chunks(q), to_chunks(k), to_chunks(v), to_chunks(ig), to_chunks(lf))
    _, h = lax.scan(step, init, xs)
    return jnp.moveaxis(h, (0, 2), (1, 3)).reshape(B, S, H, dv)


def rwkv7_scan(r, w, k, v, kk, a):
    B, S, H, N = r.shape

    def step(state, inp):
        rt, wt, kt, vt, kkt, at = inp
        sa = jnp.einsum('bhvk,bhk->bhv', state, kkt)
        state = (state * wt[:, :, None, :] - sa[..., None] * (kkt * at)[:, :, None, :]
                 + vt[..., None] * kt[:, :, None, :])
        return state, jnp.einsum('bhvk,bhk->bhv', state, rt)

    xs = tuple(jnp.moveaxis(t, 1, 0) for t in (r, w, k, v, kk, a))
    _, out = lax.scan(step, jnp.zeros((B, H, N, N), jnp.float32), xs)
    return jnp.moveaxis(out, 0, 1)


def hybrid_mixer(x, mem_n, w_in, ml_conv_w, ml_conv_b, ml_ig_b, ml_fg_b, ml_norm_g,
                 rw_mu, rw_w0, rw_w_up, rw_a0, rw_a_up, rw_g_up, rw_kk, rw_ka, rw_rk,
                 rw_ln_g, rw_ln_b, ca_w_kv, gate_b, w_br_ml, w_br_rw, w_br_ca, w_o):
    B, S, _ = x.shape
    f32 = jnp.float32
    u = x @ w_in
    u_qk, ml_v, ml_og, ml_ig, ml_fg, u_rw, ca_q, u_gate = split_cols(u, IN_SIZES)

    qk = jax.nn.silu(causal_dwconv(u_qk, ml_conv_w, ml_conv_b))
    q, k = jnp.split(qk, 2, axis=-1)
    q = q.reshape(B, S, ML_HEADS, ML_DQK).astype(f32) * (ML_DQK ** -0.5)
    k = k.reshape(B, S, ML_HEADS, ML_DQK).astype(f32)
    v = ml_v.reshape(B, S, ML_HEADS, ML_DV).astype(f32)
    ig = (ml_ig + ml_ig_b).astype(f32)
    lf = jax.nn.log_sigmoid((ml_fg + ml_fg_b).astype(f32))
    h = mlstm_chunkwise(q, k, v, ig, lf)
    h_ml = jax.nn.sigmoid(ml_og) * (head_norm(h, LN_EPS) * ml_norm_g).astype(x.dtype)

    u_rw = token_shift(u_rw, rw_mu)
    r, kr, vr, wd, ad, gd = split_cols(u_rw, RW_SIZES)
    w_log = -jax.nn.softplus(-(rw_w0 + jnp.tanh(wd) @ rw_w_up)) - 0.5
    decay = jnp.exp(-jnp.exp(w_log.astype(f32)))
    a = jax.nn.sigmoid(rw_a0 + ad @ rw_a_up)
    g = jax.nn.sigmoid(gd) @ rw_g_up

    def heads(t):
        return t.reshape(B, S, RW_HEADS, RW_DH).astype(f32)

    kk = heads(kr * rw_kk)
    kk = kk * lax.rsqrt(jnp.maximum(jnp.sum(jnp.square(kk), axis=-1, keepdims=True), 1e-24))
    kr = kr * (1.0 + (a - 1.0) * rw_ka)
    rh, kh, vh, ah, wh = heads(r), heads(kr), heads(vr), heads(a), heads(decay)
    o = rwkv7_scan(rh, wh, kh, vh, kk, ah)
    bonus = (jnp.sum(rh * kh * rw_rk, axis=-1, keepdims=True) * vh).reshape(B, S, RW_W)
    h_rw = ((head_norm(o, RW_GN_EPS) * rw_ln_g + rw_ln_b) + bonus).astype(x.dtype) * g

    km, vm = jnp.split(mem_n @ ca_w_kv, 2, axis=-1)
    km = km.reshape(B, MEM_LEN, CA_HEADS, CA_DH).astype(f32)
    vm = vm.reshape(B, MEM_LEN, CA_HEADS, CA_DH).astype(f32)
    qc = ca_q.reshape(B, S, CA_HEADS, CA_DH).astype(f32)
    p = jax.nn.softmax(jnp.einsum('bshd,bmhd->bhsm', qc, km) * (CA_DH ** -0.5), axis=-1)
    h_ca = jnp.einsum('bhsm,bmhd->bshd', p, vm).reshape(B, S, CA_W).astype(x.dtype)

    gates = jax.nn.sigmoid(u_gate + gate_b).reshape(B, S, N_BRANCH, D_MODEL)
    y = (gates[:, :, 0] * (h_ml @ w_br_ml) + gates[:, :, 1] * (h_rw @ w_br_rw)
         + gates[:, :, 2] * (h_ca @ w_br_ca))
    return y @ w_o


def clamped_swiglu(h):
    x_glu = jnp.minimum(h[..., ::2], SWIGLU_LIMIT)
    x_lin = jnp.clip(h[..., 1::2], -SWIGLU_LIMIT, SWIGLU_LIMIT)
    return x_glu * jax.nn.sigmoid(SWIGLU_ALPHA * x_glu) * (x_lin + 1.0)


def moe_ffn(x, router_w, router_b, w_gu, b_gu, w_dn, b_dn):
    B, S, D = x.shape
    n_tok = B * S
    xt = x.reshape(n_tok, D)
    logits = (xt @ router_w + router_b).astype(jnp.float32)
    top_val, top_idx = lax.top_k(logits, TOP_K)
    gate = jax.nn.softmax(top_val, axis=-1)
    n_asg = n_tok * TOP_K
    e_flat = top_idx.reshape(n_asg).astype(jnp.int32)
    tok_flat = jnp.arange(n_asg, dtype=jnp.int32) // TOP_K
    counts = jax.ops.segment_sum(jnp.ones((n_asg,), jnp.int32), e_flat, num_segments=N_EXPERTS)
    padded = (counts + MOE_BLOCK - 1) // MOE_BLOCK * MOE_BLOCK
    pad_end = jnp.cumsum(padded)
    pad_start = pad_end - padded
    start = jnp.cumsum(counts) - counts
    order = jnp.argsort(e_flat)
    e_sorted = e_flat[order]
    dest_sorted = pad_start[e_sorted] + (jnp.arange(n_asg, dtype=jnp.int32) - start[e_sorted])
    n_blocks = -(-n_asg // MOE_BLOCK) + N_EXPERTS
    n_rows = n_blocks * MOE_BLOCK
    disp_tok = jnp.full((n_rows,), n_tok, jnp.int32).at[dest_sorted].set(tok_flat[order])
    x_pad = jnp.concatenate([xt, jnp.zeros((1, D), xt.dtype)], axis=0)
    x_disp = x_pad[disp_tok].reshape(n_blocks, MOE_BLOCK, D)
    blk_start = jnp.arange(n_blocks, dtype=jnp.int32) * MOE_BLOCK
    blk_e = jnp.minimum(jnp.searchsorted(pad_end, blk_start, side='right'), N_EXPERTS - 1).astype(jnp.int32)

    def expert_block(args):
        xb, e = args
        h = clamped_swiglu(xb @ w_gu[e] + b_gu[e])
        return h @ w_dn[e] + b_dn[e]

    y_disp = lax.map(expert_block, (x_disp, blk_e)).reshape(n_rows, D)
    dest = jnp.zeros((n_asg,), jnp.int32).at[order].set(dest_sorted)
    y = y_disp[dest].reshape(n_tok, TOP_K, D)
    out = jnp.einsum('nkd,nk->nd', y, gate.astype(y.dtype))
    return out.reshape(B, S, D)


def setup_inputs(seed: int = 0) -> dict:
    key = jax.random.key(seed)
    ks = iter(jax.random.split(key, 48))
    f32 = jnp.float32
    L, D = DEPTH, D_MODEL

    def nrm(shape, scale):
        return jax.random.normal(next(ks), shape, f32) * scale

    def gain(shape):
        return 1.0 + nrm(shape, 0.02)

    return {
        'x': nrm((BATCH, SEQ, D), 1.0),
        'mem': nrm((BATCH, MEM_LEN, D), 1.0),
        'ln_in_g': gain((D,)),
        'ln_in_b': nrm((D,), 0.02),
        'mem_ln_g': gain((D,)),
        'mem_ln_b': nrm((D,), 0.02),
        'w_in': nrm((L, D, D_IN), D ** -0.5),
        'ml_conv_w': nrm((L, ML_CONV, ML_QK_W), ML_CONV ** -0.5),
        'ml_conv_b': nrm((L, ML_QK_W), 0.02),
        'ml_ig_b': nrm((L, ML_HEADS), 0.1),
        'ml_fg_b': jnp.linspace(3.0, 6.0, ML_HEADS, dtype=f32)[None] + nrm((L, ML_HEADS), 0.1),
        'ml_norm_g': gain((L, ML_W)),
        'rw_mu': jax.random.uniform(next(ks), (L, RW_IN_W), f32, 0.1, 0.9),
        'rw_w0': jnp.linspace(-6.5, -1.5, RW_W, dtype=f32)[None] + nrm((L, RW_W), 0.1),
        'rw_w_up': nrm((L, RW_DECAY_LORA, RW_W), 0.5 * RW_DECAY_LORA ** -0.5),
        'rw_a0': nrm((L, RW_W), 0.1),
        'rw_a_up': nrm((L, RW_AAA_LORA, RW_W), 0.5 * RW_AAA_LORA ** -0.5),
        'rw_g_up': nrm((L, RW_GATE_LORA, RW_W), RW_GATE_LORA ** -0.5),
        'rw_kk': 0.85 + nrm((L, RW_W), 0.05),
        'rw_ka': 1.0 + nrm((L, RW_W), 0.05),
        'rw_rk': nrm((L, RW_HEADS, RW_DH), 0.1),
        'rw_ln_g': gain((L, RW_W)),
        'rw_ln_b': nrm((L, RW_W), 0.02),
        'ca_w_kv': nrm((L, D, 2 * CA_W), D ** -0.5),
        'gate_b': nrm((L, N_BRANCH * D), 0.1),
        'w_br_ml': nrm((L, ML_W, D), DN_BETA * ML_W ** -0.5),
        'w_br_rw': nrm((L, RW_W, D), DN_BETA * RW_W ** -0.5),
        'w_br_ca': nrm((L, CA_W, D), DN_BETA * CA_W ** -0.5),
        'w_o': nrm((L, D, D), DN_BETA * D ** -0.5),
        'ln1_g': gain((L, D)),
        'ln1_b': nrm((L, D), 0.02),
        'router_w': nrm((L, D, N_EXPERTS), D ** -0.5),
        'router_b': nrm((L, N_EXPERTS), 0.01),
        'w_gu': nrm((L, N_EXPERTS, D, 2 * D_FF), D ** -0.5),
        'b_gu': nrm((L, N_EXPERTS, 2 * D_FF), 0.02),
        'w_dn': nrm((L, N_EXPERTS, D_FF, D), DN_BETA * D_FF ** -0.5),
        'b_dn': nrm((L, N_EXPERTS, D), 0.02),
        'ln2_g': gain((L, D)),
        'ln2_b': nrm((L, D), 0.02),
    }


def reference(x, mem, ln_in_g, ln_in_b, mem_ln_g, mem_ln_b, w_in, ml_conv_w, ml_conv_b,
              ml_ig_b, ml_fg_b, ml_norm_g, rw_mu, rw_w0, rw_w_up, rw_a0, rw_a_up, rw_g_up,
              rw_kk, rw_ka, rw_rk, rw_ln_g, rw_ln_b, ca_w_kv, gate_b, w_br_ml, w_br_rw,
              w_br_ca, w_o, ln1_g, ln1_b, router_w, router_b, w_gu, b_gu, w_dn, b_dn,
              ln2_g, ln2_b):
    x = layer_norm(x, ln_in_g, ln_in_b)
    mem_n = layer_norm(mem, mem_ln_g, mem_ln_b)
    for l in range(DEPTH):
        y = hybrid_mixer(x, mem_n, w_in[l], ml_conv_w[l], ml_conv_b[l], ml_ig_b[l], ml_fg_b[l],
                         ml_norm_g[l], rw_mu[l], rw_w0[l], rw_w_up[l], rw_a0[l], rw_a_up[l],
                         rw_g_up[l], rw_kk[l], rw_ka[l], rw_rk[l], rw_ln_g[l], rw_ln_b[l],
                         ca_w_kv[l], gate_b[l], w_br_ml[l], w_br_rw[l], w_br_ca[l], w_o[l])
        x = layer_norm(DN_ALPHA * x + y, ln1_g[l], ln1_b[l])
        y = moe_ffn(x, router_w[l], router_b[l], w_gu[l], b_gu[l], w_dn[l], b_dn[l])
        x = layer_norm(DN_ALPHA * x + y, ln2_g[l], ln2_b[l])
    return x
```

```python
import numpy as np
import concourse.bass as bass
import concourse.mybir as mybir
from concourse.bass_utils import run_bass_kernel_spmd
from contextlib import ExitStack

F32 = mybir.dt.float32
BF16 = mybir.dt.bfloat16
U32 = mybir.dt.uint32
ALU = mybir.AluOpType
AF = mybir.ActivationFunctionType
AX = mybir.AxisListType


class Buf:
    __slots__ = ("name", "lw", "rd", "wsem", "rsem")

    def __init__(self, name):
        self.name = name
        self.lw = None
        self.rd = []
        self.wsem = None
        self.rsem = None


class Prog:
    def __init__(self, nc, stack):
        self.nc = nc
        self.gstack = stack
        self.stack = stack
        self.engs = {}
        for n in ("tensor", "vector", "scalar", "gpsimd", "sync"):
            e = getattr(nc, n)
            sem = stack.enter_context(nc.semaphore("es_" + n))
            self.engs[n] = dict(h=e, sem=sem, cnt=0, seen={}, seen_d={})
        self.sems = []
        self.free_sems = {"hw": [], "sw": []}
        self.sem_kind = {}
        self.phase_bufs = []
        self.ninstr = 0
        self.mute = False
        self.pid = 0
        import os
        self.cut = float(os.environ.get('ML_CUT', '99'))

    def sb(self, name, shape, dt):
        return self.stack.enter_context(self.nc.sbuf_tensor("%s_p%d" % (name, self.pid), list(shape), dt))

    def ps(self, name, shape, dt=F32):
        return self.stack.enter_context(self.nc.psum_tensor("%s_p%d" % (name, self.pid), list(shape), dt))

    def _getsem(self, en):
        kind = "sw" if en == "gpsimd" else "hw"
        if self.free_sems[kind]:
            return self.free_sems[kind].pop()
        h = self.gstack.enter_context(self.nc.semaphore("ds%d" % len(self.sems)))
        self.sems.append([h, 0])
        self.sem_kind[len(self.sems) - 1] = kind
        return len(self.sems) - 1

    def _need(self, en, dep):
        E = self.engs[en]
        if dep[0] == 'e':
            _, pn, cnt = dep
            if pn == en and en == "tensor":
                return
            if E["seen"].get(pn, 0) >= cnt:
                return
            E["seen"][pn] = cnt
            E["h"].wait_ge(self.engs[pn]["sem"], cnt)
        else:
            _, k, val = dep
            if E["seen_d"].get(k, 0) >= val:
                return
            E["seen_d"][k] = val
            E["h"].wait_ge(self.sems[k][0], val)

    def _deps(self, en, reads, writes):
        for b in reads:
            if b.lw is not None:
                self._need(en, b.lw)
        for b in writes:
            if b.lw is not None:
                self._need(en, b.lw)
            for r in b.rd:
                self._need(en, r)

    def mark(self, k):
        if k > self.cut:
            self.mute = True

    def op(self, en, fn, reads=(), writes=()):
        if self.mute:
            return None
        self._deps(en, reads, writes)
        E = self.engs[en]
        ins = fn(E["h"])
        E["cnt"] += 1
        ins.then_inc(E["sem"], 1)
        tag = ('e', en, E["cnt"])
        for b in reads:
            b.rd.append(tag)
        for b in writes:
            b.lw = tag
            b.rd = []
        self.ninstr += 1
        return ins

    def load(self, en, out, in_, sbuf, extra_reads=(), **kw):
        if self.mute:
            return None
        self._deps(en, extra_reads, [sbuf])
        if sbuf.wsem is None:
            sbuf.wsem = self._getsem(en)
            self.phase_bufs.append(sbuf)
        s = self.sems[sbuf.wsem]
        s[1] += 16
        ins = self.engs[en]["h"].dma_start(out=out, in_=in_, **kw)
        ins.then_inc(s[0], 16)
        sbuf.lw = ('d', sbuf.wsem, s[1])
        sbuf.rd = []
        self.ninstr += 1
        return ins

    def store(self, en, out, in_, sbuf, **kw):
        if self.mute:
            return None
        self._deps(en, [sbuf], [])
        if sbuf.rsem is None:
            sbuf.rsem = self._getsem(en)
            self.phase_bufs.append(sbuf)
        s = self.sems[sbuf.rsem]
        s[1] += 16
        ins = self.engs[en]["h"].dma_start(out=out, in_=in_, **kw)
        ins.then_inc(s[0], 16)
        sbuf.rd.append(('d', sbuf.rsem, s[1]))
        self.ninstr += 1
        return ins

    def barrier(self):
        S = self.engs["sync"]
        for n, E in self.engs.items():
            if n != "sync" and E["cnt"] > 0:
                self._need("sync", ('e', n, E["cnt"]))
        for k, (h, v) in enumerate(self.sems):
            if v > 0:
                self._need("sync", ('d', k, v))
        ins = S["h"].nop()
        S["cnt"] += 1
        ins.then_inc(S["sem"], 1)
        for n in self.engs:
            if n != "sync":
                self._need(n, ('e', "sync", S["cnt"]))
        for n, E in self.engs.items():
            for m, F in self.engs.items():
                if m != n:
                    E["seen"][m] = max(E["seen"].get(m, 0), F["cnt"])
            for k, (h, v) in enumerate(self.sems):
                E["seen_d"][k] = max(E["seen_d"].get(k, 0), v)
        for b in self.phase_bufs:
            for k in (b.wsem, b.rsem):
                if k is not None:
                    self.free_sems[self.sem_kind[k]].append(k)
            b.wsem = b.rsem = None
        self.phase_bufs = []

    class _Phase:
        def __init__(self, P):
            self.P = P
        def __enter__(self):
            self.es = ExitStack()
            self.es.__enter__()
            self.P.stack = self.es
            self.P.pid += 1
            return self
        def __exit__(self, *a):
            self.P.barrier()
            self.P.stack = self.P.gstack
            return self.es.__exit__(*a)

    def phase(self):
        return Prog._Phase(self)


D = 1024
DIN = 6920
QK0, V0, OG0, IG0, FG0, RW0, CAQ0, GATE0 = 0, 512, 1024, 1536, 1540, 1544, 3336, 3848
NSEQ = 2
MEM = 256
NE = 32
DN_ALPHA = 4.0 ** 0.25
LN_EPS = 1e-5


def bcast_load(P, name, vec_ap, n, eng="sync"):
    t = P.sb(name, [128, n], F32)
    B = Buf(name)
    P.load(eng, t[:], vec_ap.partition_broadcast(128), B)
    return t, B


class LNScratch:
    def __init__(self, P, tag):
        self.st6 = P.sb("ln_st6" + tag, [128, 2, 6], F32)
        self.mv = P.sb("ln_mv" + tag, [128, 2], F32)
        self.rstd = P.sb("ln_rstd" + tag, [128, 1], F32)
        self.B = Buf("ln_scr" + tag)


def layernorm(P, S, xt, Bx, gam, Bg, bet, Bb, eps=LN_EPS):
    for c in range(2):
        P.op("vector", lambda e: e.bn_stats(S.st6[:, c, :], xt[:, c * 512:(c + 1) * 512]), reads=[Bx], writes=[S.B])
    P.op("vector", lambda e: e.bn_aggr(S.mv[:], S.st6[:].rearrange("p a b -> p (a b)")), reads=[S.B], writes=[S.B])
    P.op("scalar", lambda e: e.activation(S.rstd[:], S.mv[:, 1:2], AF.Ln, bias=eps, scale=1.0), reads=[S.B], writes=[S.B])
    P.op("scalar", lambda e: e.activation(S.rstd[:], S.rstd[:], AF.Exp, scale=-0.5), reads=[S.B], writes=[S.B])
    P.op("vector", lambda e: e.tensor_scalar(xt, xt, S.mv[:, 0:1], S.rstd[:], ALU.subtract, ALU.mult), reads=[Bx, S.B], writes=[Bx])
    P.op("vector", lambda e: e.tensor_tensor(xt, xt, gam, ALU.mult), reads=[Bx, Bg], writes=[Bx])
    P.op("vector", lambda e: e.tensor_tensor(xt, xt, bet, ALU.add), reads=[Bx, Bb], writes=[Bx])


def make_ident(P, dt, name):
    t = P.sb(name, [128, 128], dt)
    B = Buf(name)
    P.op("gpsimd", lambda e: e.memset(t[:], 0.0), writes=[B])
    P.op("gpsimd", lambda e: e.affine_select(t[:], t[:], pattern=[[-1, 128]], compare_op=ALU.not_equal,
                                             fill=1.0, base=0, channel_multiplier=1), reads=[B], writes=[B])
    return t, B


def phase_proj(P, l, NT, xin, XRES, U, w):
    nt = NT // 128
    with P.phase():
        wb = P.sb("pa_wb", [128, 8, DIN], BF16)
        Bwb = Buf("pa_wb")
        for k in range(8):
            for c4 in range(4):
                P.load("gpsimd", wb[:, k, c4 * 1730:(c4 + 1) * 1730],
                       w["w_in"][l, k * 128:(k + 1) * 128, c4 * 1730:(c4 + 1) * 1730], Bwb)
        identb, Bidb = make_ident(P, BF16, "pa_identb")
        if l == 0:
            gam, Bg = bcast_load(P, "pa_g", w["ln_in_g"], D)
            bet, Bb = bcast_load(P, "pa_b", w["ln_in_b"], D)
        S = LNScratch(P, "pa")
        xt = [P.sb("pa_xt%d" % i, [128, D], F32) for i in range(2)]
        Bxt = [Buf("pa_xt%d" % i) for i in range(2)]
        xb = P.sb("pa_xb", [128, D], BF16)
        Bxb = Buf("pa_xb")
        xT = P.sb("pa_xT", [128, 8, 128], BF16)
        BxT = Buf("pa_xT")
        pt = P.ps("pa_pt", [128, 8, 128], BF16)
        Bpt = Buf("pa_pt")
        NPY = 3
        py = [P.ps("pa_py%d" % i, [128, 512], F32) for i in range(NPY)]
        Bpy = [Buf("pa_py%d" % i) for i in range(NPY)]
        NU = 4
        us = [P.sb("pa_us%d" % i, [128, 512], F32) for i in range(NU)]
        Bus = [Buf("pa_us%d" % i) for i in range(NU)]
        P.load("sync", xt[0][:], xin[0:128, :], Bxt[0])
        cc = 0
        for i in range(nt):
            X, BX = xt[i % 2], Bxt[i % 2]
            if i + 1 < nt:
                P.load("sync", xt[(i + 1) % 2][:], xin[(i + 1) * 128:(i + 2) * 128, :], Bxt[(i + 1) % 2])
            if l == 0:
                layernorm(P, S, X[:], BX, gam[:], Bg, bet[:], Bb)
                P.store("sync", XRES[i * 128:(i + 1) * 128, :], X[:], BX)
            P.op("scalar", lambda e: e.activation(xb[:], X[:], AF.Copy), reads=[BX], writes=[Bxb])
            for k in range(8):
                P.op("tensor", lambda e: e.transpose(pt[:, k, :], xb[:, k * 128:(k + 1) * 128], identb[:]),
                     reads=[Bxb, Bidb], writes=[Bpt])
            P.op("vector", lambda e: e.tensor_copy(xT[:], pt[:]), reads=[Bpt], writes=[BxT])
            for c in range(14):
                c0 = c * 512
                cw = min(512, DIN - c0)
                pp, Bp = py[cc % NPY], Bpy[cc % NPY]
                uu, Bu = us[cc % NU], Bus[cc % NU]
                for k in range(8):
                    P.op("tensor", lambda e: e.matmul(pp[:, :cw], xT[:, k, :], wb[:, k, c0:c0 + cw],
                                                      start=(k == 0), stop=(k == 7)),
                         reads=[BxT, Bwb], writes=[Bp])
                if cc % 2 == 0:
                    P.op("scalar", lambda e: e.activation(uu[:, :cw], pp[:, :cw], AF.Copy), reads=[Bp], writes=[Bu])
                else:
                    P.op("vector", lambda e: e.tensor_copy(uu[:, :cw], pp[:, :cw]), reads=[Bp], writes=[Bu])
                P.store("sync", U[i * 128:(i + 1) * 128, c0:c0 + cw], uu[:, :cw], Bu)
                cc += 1


def dram_in(nc, name, shape):
    return nc.dram_tensor(name, list(shape), F32, kind="ExternalInput").ap()


W_SHAPES = dict(
    ln_in_g=(D,), ln_in_b=(D,), mem_ln_g=(D,), mem_ln_b=(D,), w_in=(2, D, DIN),
    ml_conv_w=(2, 4, 512), ml_conv_b=(2, 512), ml_ig_b=(2, 4), ml_fg_b=(2, 4), ml_norm_g=(2, 512),
    rw_mu=(2, 1792), rw_w0=(2, 512), rw_w_up=(2, 64, 512), rw_a0=(2, 512), rw_a_up=(2, 64, 512),
    rw_g_up=(2, 128, 512), rw_kk=(2, 512), rw_ka=(2, 512), rw_rk=(2, 8, 64), rw_ln_g=(2, 512), rw_ln_b=(2, 512),
    ca_w_kv=(2, D, 1024), gate_b=(2, 3 * D), w_br_ml=(2, 512, D), w_br_rw=(2, 512, D), w_br_ca=(2, 512, D),
    w_o=(2, D, D), ln1_g=(2, D), ln1_b=(2, D), router_w=(2, D, NE), router_b=(2, NE),
    w_gu=(2, NE, D, 2 * D), b_gu=(2, NE, 2 * D), w_dn=(2, NE, D, D), b_dn=(2, NE, D), ln2_g=(2, D), ln2_b=(2, D),
)


def build(T=4096, nlayers=2, phases=None, dbg=()):
    NT = NSEQ * T
    nc = bass.Bass("TRN2", target_bir_lowering=False)
    x = dram_in(nc, "x", (NT, D))
    mem = dram_in(nc, "mem", (NSEQ * MEM, D))
    w = {k: dram_in(nc, k, s) for k, s in W_SHAPES.items()}
    out = nc.dram_tensor("out", [NT, D], F32, kind="ExternalOutput").ap()

    def scratch(name, shape, dt=F32):
        kind = "ExternalOutput" if name in dbg else "Internal"
        return nc.dram_tensor(name, list(shape), dt, kind=kind).ap()

    XRES = scratch("XRES", (NT, D))
    U = scratch("U", (NT, DIN))
    HCAT = scratch("HCAT", (NT, 1536))
    RWOP = scratch("RWOP", (NT, 6, 512))
    RWG = scratch("RWG", (NT, 512))
    RWB = scratch("RWB", (NT, 8))
    RWO = scratch("RWO", (NT, 512))
    X1 = scratch("X1", (NT, D))
    XRES2 = scratch("XRES2", (NT, D))
    with ExitStack() as st:
        P = Prog(nc, st)
        on = lambda n: phases is None or n in phases
        for l in range(nlayers):
            xin = x if l == 0 else XRES2
            xres = XRES if l == 0 else XRES2
            xout = out if l == nlayers - 1 else XRES2
            if on("proj"):
                phase_proj(P, l, NT, xin, XRES, U, w)
            if on("mlstm"):
                phase_mlstm(P, l, T, U, HCAT, w)
            if on("ca"):
                phase_ca(P, l, T, U, HCAT, mem, w)
            if on("rwpre"):
                phase_rw_pre(P, l, T, U, RWOP, RWG, RWB, w)
            if on("rwscan"):
                phase_rw_scan(P, l, T, RWOP, RWO)
            if on("rwpost"):
                phase_rw_post(P, l, T, RWOP, RWG, RWB, RWO, HCAT, w)
            if on("merge"):
                phase_merge(P, l, NT, U, HCAT, xres, X1, w)
            if on("moe"):
                phase_moe(P, l, NT, X1, xout, w)
        P.barrier()
    print("instructions:", P.ninstr, "dma sems:", len(P.sems))
    return nc


_NC_CACHE = {}


def kernel(**inputs):
    ncores = 8
    T = 4096
    if T not in _NC_CACHE:
        _NC_CACHE[T] = build(T=T)
    nc = _NC_CACHE[T]
    wmap = {k: np.ascontiguousarray(inputs[k], dtype=np.float32) for k in W_SHAPES}
    x = np.asarray(inputs["x"], dtype=np.float32)
    mem = np.asarray(inputs["mem"], dtype=np.float32)
    in_maps = []
    for c in range(ncores):
        m = dict(wmap)
        m["x"] = np.ascontiguousarray(x[NSEQ * c:NSEQ * (c + 1)].reshape(NSEQ * T, D))
        m["mem"] = np.ascontiguousarray(mem[NSEQ * c:NSEQ * (c + 1)].reshape(NSEQ * MEM, D))
        in_maps.append(m)
    res = run_bass_kernel_spmd(nc, in_maps, core_ids=list(range(ncores)))
    outs = [np.asarray(r["out"]).reshape(NSEQ, T, D) for r in res.results]
    return np.concatenate(outs, axis=0).astype(np.float32)


def make_mask_le(P, name):
    t = P.sb(name, [128, 128], F32)
    B = Buf(name)
    P.op("gpsimd", lambda e: e.memset(t[:], 1.0), writes=[B])
    P.op("gpsimd", lambda e: e.affine_select(t[:], t[:], pattern=[[1, 128]], compare_op=ALU.is_ge,
                                             fill=0.0, base=0, channel_multiplier=-1), reads=[B], writes=[B])
    return t, B


def phase_mlstm(P, l, T, U, HCAT, w):
    nchunk = T // 128
    with P.phase():
        identf, Bidf = make_ident(P, F32, "ml_identf")
        identb, Bidb = make_ident(P, BF16, "ml_identb")
        mask, Bmask = make_mask_le(P, "ml_mask")
        sel = P.sb("ml_sel", [4, 4, 128], F32)
        Bsel = Buf("ml_sel")
        P.op("gpsimd", lambda e: e.memset(sel[:], 0.0), writes=[Bsel])
        P.op("gpsimd", lambda e: e.affine_select(sel[:], sel[:], pattern=[[-1, 4], [0, 128]], compare_op=ALU.not_equal,
                                                 fill=1.0, base=0, channel_multiplier=1), reads=[Bsel], writes=[Bsel])
        cw = P.sb("ml_cw", [64, 8, 4], F32)
        Bcw = Buf("ml_cw")
        for jj in range(4):
            P.load("sync", cw[:, :, jj], w["ml_conv_w"][l, jj].rearrange("(blk p) -> p blk", p=64), Bcw,
                   allow_slow_non_contiguous=True)
        cb = P.sb("ml_cb", [64, 8], F32)
        Bcb = Buf("ml_cb")
        P.load("sync", cb[:], w["ml_conv_b"][l].rearrange("(blk p) -> p blk", p=64), Bcb,
               allow_slow_non_contiguous=True)
        gb8 = P.sb("ml_gb8", [128, 8], F32)
        Bgb8 = Buf("ml_gb8")
        P.load("sync", gb8[:, 0:4], w["ml_ig_b"][l].partition_broadcast(128), Bgb8)
        P.load("sync", gb8[:, 4:8], w["ml_fg_b"][l].partition_broadcast(128), Bgb8)
        normg, Bng = bcast_load(P, "ml_normg", w["ml_norm_g"][l], 512)
        ones4 = P.sb("ml_ones4", [4, 128], F32)
        Bones4 = Buf("ml_ones4")
        P.op("vector", lambda e: e.memset(ones4[:], 1.0), writes=[Bones4])
        zeros4 = P.sb("ml_zeros4", [4, 128], F32)
        Bz4 = Buf("ml_zeros4")
        P.op("vector", lambda e: e.memset(zeros4[:], 0.0), writes=[Bz4])
        onesb = P.sb("ml_onesb", [128, 2], BF16)
        Bonesb = Buf("ml_onesb")
        P.op("vector", lambda e: e.memset(onesb[:], 1.0), writes=[Bonesb])

        C32 = P.sb("ml_C32", [64, 4, 132], F32)
        BC32 = Buf("ml_C32")
        Cb = P.sb("ml_Cb", [64, 4, 132], BF16)
        BCb = Buf("ml_Cb")
        uqk = [P.sb("ml_uqk%d" % i, [128, 512], F32) for i in range(2)]
        Buqk = [Buf("ml_uqk%d" % i) for i in range(2)]
        vt = [P.sb("ml_vt%d" % i, [128, 512], F32) for i in range(2)]
        Bvt = [Buf("ml_vt%d" % i) for i in range(2)]
        og = [P.sb("ml_og%d" % i, [128, 512], F32) for i in range(2)]
        Bog = [Buf("ml_og%d" % i) for i in range(2)]
        gt = [P.sb("ml_gt%d" % i, [128, 8], F32) for i in range(2)]
        Bgt = [Buf("ml_gt%d" % i) for i in range(2)]
        uT = [P.sb("ml_uT%d" % i, [64, 8, 131], F32) for i in range(2)]
        BuT = [Buf("ml_uT%d" % i) for i in range(2)]
        Fg = [P.sb("ml_F%d" % i, [4, 5, 128], F32) for i in range(2)]
        BF = [Buf("ml_F%d" % i) for i in range(2)]
        acc = P.sb("ml_acc", [64, 8, 128], F32)
        Bacc = Buf("ml_acc")
        qkb = P.sb("ml_qkb", [64, 8, 128], BF16)
        Bqkb = Buf("ml_qkb")
        ktok = P.sb("ml_ktok", [128, 4, 64], BF16)
        Bktok = Buf("ml_ktok")
        kw = P.sb("ml_kw", [128, 4, 64], BF16)
        Bkw = Buf("ml_kw")
        z8 = P.sb("ml_z8", [128, 8], F32)
        Bz8 = Buf("ml_z8")
        T6 = P.sb("ml_T6", [4, 6, 128], F32)
        BT6 = Buf("ml_T6")
        d41 = P.sb("ml_d41", [4, 1], F32)
        Bd41 = Buf("ml_d41")
        tokA = P.sb("ml_tokA", [128, 4], F32)
        BtokA = Buf("ml_tokA")
        tokE = P.sb("ml_tokE", [128, 4, 4], F32)
        BtokE = Buf("ml_tokE")
        Dm = P.sb("ml_D", [128, 4, 128], F32)
        BD = Buf("ml_D")
        Wt = P.sb("ml_Wt", [128, 4, 128], BF16)
        BWt = Buf("ml_Wt")
        vext = P.sb("ml_vext", [128, 4, 132], BF16)
        Bvext = Buf("ml_vext")
        P.op("vector", lambda e: e.memset(vext[:], 1.0), writes=[Bvext])
        numI = P.sb("ml_numI", [128, 4, 128], F32)
        BnumI = Buf("ml_numI")
        tot = P.sb("ml_tot", [128, 4, 128], F32)
        Btot = Buf("ml_tot")
        sm = P.sb("ml_sm", [128, 8, 4], F32)
        Bsm = Buf("ml_sm")
        st6 = P.sb("ml_st6", [128, 4, 6], F32)
        mv = P.sb("ml_mv", [128, 4, 2], F32)
        Bst = Buf("ml_st")
        sg = P.sb("ml_sg", [128, 512], F32)
        Bsg = Buf("ml_sg")
        hml = [P.sb("ml_hml%d" % i, [128, 512], F32) for i in range(2)]
        Bhml = [Buf("ml_hml%d" % i) for i in range(2)]
        b0 = P.ps("ml_b0", [128, 512], F32); Bb0 = Buf("ml_b0")
        b1 = P.ps("ml_b1", [128, 512], F32); Bb1 = Buf("ml_b1")
        pnG = P.ps("ml_pnG", [128, 4, 128], F32); BpnG = Buf("ml_pnG")
        pS = P.ps("ml_pS", [128, 4, 128], F32); BpS = Buf("ml_pS")
        pI = P.ps("ml_pI", [128, 4, 128], F32); BpI = Buf("ml_pI")
        pC = P.ps("ml_pC", [128, 4, 128], F32); BpC = Buf("ml_pC")
        pK = P.ps("ml_pK", [128, 4, 64], BF16); BpK = Buf("ml_pK")
        b7 = P.ps("ml_b7", [128, 512], F32); Bb7 = Buf("ml_b7")
        ptq = [b0[0:64, :].rearrange("p (a b) -> p a b", a=4), b7[0:64, :].rearrange("p (a b) -> p a b", a=4)]
        Bptq = [Bb0, Bb7]
        pU = [b0[0:64, 0:264].rearrange("p (a b) -> p a b", a=2), b7[0:64, 0:264].rearrange("p (a b) -> p a b", a=2)]
        pg = b1[0:4, 0:256].rearrange("p (a b) -> p a b", a=2)
        pT = b1[:, 256:276].rearrange("p (a b) -> p a b", a=5)
        pD = b1[:, 280:288]

        def loads(b, c, i):
            r0 = b * T + c * 128
            P.load("sync", uqk[i][:], U[r0:r0 + 128, QK0:QK0 + 512], Buqk[i])
            P.load("sync", vt[i][:], U[r0:r0 + 128, V0:V0 + 512], Bvt[i])
            P.load("sync", og[i][:], U[r0:r0 + 128, OG0:OG0 + 512], Bog[i])
            P.load("sync", gt[i][:], U[r0:r0 + 128, IG0:IG0 + 8], Bgt[i])

        it = 0
        loads(0, 0, 0)
        for b in range(NSEQ):
            P.op("gpsimd", lambda e: e.memset(C32[:], 0.0), writes=[BC32])
            P.op("gpsimd", lambda e: e.memset(Cb[:], 0.0), writes=[BCb])
            for c in range(nchunk):
                i = it % 2
                j = 1 - i
                nb, ncn = (b, c + 1) if c + 1 < nchunk else (b + 1, 0)
                if nb < NSEQ:
                    loads(nb, ncn, j)
                first = (c == 0)
                for blk in range(8):
                    P.op("tensor", lambda e: e.transpose(ptq[blk // 4][:, blk % 4, :], uqk[i][:, blk * 64:(blk + 1) * 64], identf[:]),
                         reads=[Buqk[i], Bidf], writes=[Bptq[blk // 4]])
                if first:
                    P.op("gpsimd", lambda e: e.memset(uT[i][:, :, 0:3], 0.0), writes=[BuT[i]])
                else:
                    P.op("gpsimd", lambda e: e.tensor_copy(uT[i][:, :, 0:3], uT[j][:, :, 128:131]), reads=[BuT[j]], writes=[BuT[i]])
                P.op("vector", lambda e: e.tensor_copy(uT[i][:, 0:4, 3:131], ptq[0]), reads=[Bb0], writes=[BuT[i]])
                P.op("vector", lambda e: e.tensor_copy(uT[i][:, 4:8, 3:131], ptq[1]), reads=[Bb7], writes=[BuT[i]])
                for blk in range(8):
                    P.op("scalar", lambda e: e.activation(acc[:, blk, :], uT[i][:, blk, 3:131], AF.Identity,
                                                          bias=cb[:, blk:blk + 1], scale=cw[:, blk, 3:4]),
                         reads=[BuT[i], Bcb, Bcw], writes=[Bacc])
                    for dd in range(1, 4):
                        P.op("vector", lambda e: e.scalar_tensor_tensor(acc[:, blk, :], uT[i][:, blk, 3 - dd:131 - dd],
                                                                        cw[:, blk, 3 - dd:4 - dd], acc[:, blk, :], ALU.mult, ALU.add),
                             reads=[BuT[i], Bcw, Bacc], writes=[Bacc])
                P.op("scalar", lambda e: e.activation(acc[:], acc[:], AF.Silu), reads=[Bacc], writes=[Bacc])
                P.op("vector", lambda e: e.tensor_scalar(qkb[:, 0:4, :], acc[:, 0:4, :], 0.125, None, ALU.mult), reads=[Bacc], writes=[Bqkb])
                P.op("gpsimd", lambda e: e.tensor_copy(qkb[:, 4:8, :], acc[:, 4:8, :]), reads=[Bacc], writes=[Bqkb])
                P.mark(1)
                for blk in range(4):
                    P.op("tensor", lambda e: e.transpose(pK[:, blk, :], qkb[:, 4 + blk, :], identb[0:64, 0:64]), reads=[Bqkb, Bidb], writes=[BpK])
                P.op("scalar", lambda e: e.activation(ktok[:], pK[:], AF.Copy), reads=[BpK], writes=[Bktok])
                P.mark(2)
                P.op("vector", lambda e: e.tensor_tensor(z8[:], gt[i][:], gb8[:], ALU.add), reads=[Bgt[i], Bgb8], writes=[Bz8])
                P.op("scalar", lambda e: e.activation(z8[:, 4:8], z8[:, 4:8], AF.Exp, scale=-1.0), reads=[Bz8], writes=[Bz8])
                P.op("scalar", lambda e: e.activation(z8[:, 4:8], z8[:, 4:8], AF.Ln, bias=1.0, scale=1.0), reads=[Bz8], writes=[Bz8])
                P.op("tensor", lambda e: e.transpose(pg[:, 0, :], z8[:, 0:4], identf[:]), reads=[Bz8, Bidf], writes=[Bb1])
                P.op("tensor", lambda e: e.transpose(pg[:, 1, :], z8[:, 4:8], identf[:]), reads=[Bz8, Bidf], writes=[Bb1])
                F = Fg[i]
                Fp = Fg[j]
                P.op("vector", lambda e: e.tensor_copy(F[:, 0, :], pg[:, 0, :]), reads=[Bb1], writes=[BF[i]])
                P.op("vector", lambda e: e.tensor_scalar(F[:, 1, :], pg[:, 1, :], -1.0, None, ALU.mult), reads=[Bb1], writes=[BF[i]])
                P.op("vector", lambda e: e.tensor_tensor_scan(F[:, 2, :], ones4[:], F[:, 1, :],
                                                              0.0 if first else Fp[:, 2, 127:128], ALU.mult, ALU.add),
                     reads=[BF[i], BF[j], Bones4], writes=[BF[i]])
                P.op("vector", lambda e: e.tensor_tensor(F[:, 3, :], F[:, 0, :], F[:, 2, :], ALU.subtract), reads=[BF[i]], writes=[BF[i]])
                P.op("vector", lambda e: e.tensor_tensor_scan(F[:, 4, :], F[:, 3, :], F[:, 3, :],
                                                              0.0 if first else Fp[:, 4, 127:128], ALU.max, ALU.max),
                     reads=[BF[i], BF[j]], writes=[BF[i]])
                gprev = zeros4[:, 0:1] if first else Fp[:, 4, 127:128]
                P.op("vector", lambda e: e.tensor_copy(T6[:, 0, :], F[:, 3, :]), reads=[BF[i]], writes=[BT6])
                P.op("vector", lambda e: e.scalar_tensor_tensor(T6[:, 1, :], F[:, 2, :], -1.0, F[:, 4, :], ALU.mult, ALU.subtract),
                     reads=[BF[i]], writes=[BT6])
                P.op("vector", lambda e: e.tensor_scalar(T6[:, 2, :], F[:, 4, :], -1.0, gprev, ALU.mult, ALU.add),
                     reads=[BF[i], BF[j], Bz4], writes=[BT6])
                P.op("vector", lambda e: e.tensor_scalar(T6[:, 3, :], F[:, 3, :], F[:, 4, 127:128], None, ALU.subtract),
                     reads=[BF[i]], writes=[BT6])
                P.op("vector", lambda e: e.tensor_tensor(d41[:], gprev, F[:, 4, 127:128], ALU.subtract),
                     reads=[BF[i], BF[j], Bz4], writes=[Bd41])
                P.op("vector", lambda e: e.tensor_scalar(T6[:, 4, :], zeros4[:], d41[:, 0:1], None, ALU.add),
                     reads=[Bz4, Bd41], writes=[BT6])
                P.op("vector", lambda e: e.tensor_scalar(T6[:, 5, :], F[:, 4, :], -1.0, None, ALU.mult), reads=[BF[i]], writes=[BT6])
                for r in range(5):
                    P.op("tensor", lambda e: e.transpose(pT[:, r, :], T6[:, r, :], identf[0:4, 0:4]), reads=[BT6, Bidf], writes=[Bb1])
                P.op("vector", lambda e: e.tensor_copy(tokA[:], pT[:, 0, :]), reads=[Bb1], writes=[BtokA])
                P.op("scalar", lambda e: e.activation(tokE[:], pT[:, 1:5, :], AF.Exp), reads=[Bb1], writes=[BtokE])
                P.mark(3)
                for h in range(4):
                    P.op("tensor", lambda e: e.matmul(pnG[:, h, :], sel[:, h, :], T6[:, 5, :], start=True, stop=True),
                         reads=[Bsel, BT6], writes=[BpnG])
                for h in range(4):
                    P.op("tensor", lambda e: e.matmul(pS[:, h, :], qkb[:, 4 + h, :], qkb[:, h, :],
                                                      start=True, stop=True), reads=[Bqkb], writes=[BpS])
                P.mark(3.1)
                for h in range(4):
                    P.op("vector", lambda e: e.tensor_scalar(Dm[:, h, :], pnG[:, h, :], tokA[:, h:h + 1], 0.0, ALU.add, ALU.min),
                         reads=[BpnG, BtokA], writes=[BD])
                P.mark(3.2)
                P.op("scalar", lambda e: e.activation(Dm[:], Dm[:], AF.Exp), reads=[BD], writes=[BD])
                P.mark(3.3)
                P.op("gpsimd", lambda e: e.tensor_tensor(Dm[:], Dm[:], mask[:].unsqueeze(1).to_broadcast([128, 4, 128]), ALU.mult),
                     reads=[BD, Bmask], writes=[BD])
                P.mark(3.4)
                P.op("vector", lambda e: e.tensor_tensor(Wt[:], Dm[:], pS[:], ALU.mult), reads=[BD, BpS], writes=[BWt])
                P.mark(3.5)
                P.op("gpsimd", lambda e: e.tensor_copy(vext[:, :, 0:128], vt[i][:].rearrange("p (h v) -> p h v", h=4)),
                     reads=[Bvt[i]], writes=[Bvext])
                P.mark(4)
                for h in range(4):
                    P.op("tensor", lambda e: e.matmul(pI[:, h, :], Wt[:, h, :], vext[:, h, 0:128], start=True, stop=True),
                         reads=[BWt, Bvext], writes=[BpI])
                    P.op("tensor", lambda e: e.matmul(pC[:, h, :], qkb[:, h, :], Cb[:, h, 0:128],
                                                      start=True, stop=True), reads=[Bqkb, BCb], writes=[BpC])
                    P.op("tensor", lambda e: e.matmul(pD[:, h:h + 1], Wt[:, h, :], onesb[:, 0:1], start=True, stop=True),
                         reads=[BWt, Bonesb], writes=[Bb1])
                    P.op("tensor", lambda e: e.matmul(pD[:, 4 + h:5 + h], qkb[:, h, :], Cb[:, h, 128:129],
                                                      start=True, stop=True), reads=[Bqkb, BCb], writes=[Bb1])
                P.op("scalar", lambda e: e.activation(numI[:], pI[:], AF.Copy), reads=[BpI], writes=[BnumI])
                for h in range(4):
                    P.op("vector", lambda e: e.scalar_tensor_tensor(tot[:, h, :], pC[:, h, :], tokE[:, 1, h:h + 1], numI[:, h, :],
                                                                    ALU.mult, ALU.add), reads=[BpC, BtokE, BnumI], writes=[Btot])
                P.op("vector", lambda e: e.tensor_tensor(sm[:, 0, :], pD[:, 4:8], tokE[:, 1, :], ALU.mult), reads=[Bb1, BtokE], writes=[Bsm])
                P.op("vector", lambda e: e.tensor_tensor(sm[:, 1, :], pD[:, 0:4], sm[:, 0, :], ALU.add), reads=[Bb1, Bsm], writes=[Bsm])
                P.op("vector", lambda e: e.tensor_scalar(sm[:, 2, :], sm[:, 1, :], -1.0, None, ALU.mult), reads=[Bsm], writes=[Bsm])
                P.op("vector", lambda e: e.tensor_tensor(sm[:, 2, :], sm[:, 2, :], sm[:, 1, :], ALU.max), reads=[Bsm], writes=[Bsm])
                P.op("vector", lambda e: e.tensor_tensor(sm[:, 3, :], sm[:, 2, :], tokE[:, 0, :], ALU.max), reads=[Bsm, BtokE], writes=[Bsm])
                P.op("vector", lambda e: e.reciprocal(sm[:, 4, :], sm[:, 3, :]), reads=[Bsm], writes=[Bsm])
                P.op("vector", lambda e: e.tensor_tensor(tot[:], tot[:], sm[:, 4, :].unsqueeze(2).to_broadcast([128, 4, 128]), ALU.mult),
                     reads=[Btot, Bsm], writes=[Btot])
                P.mark(5)
                for h in range(4):
                    P.op("vector", lambda e: e.bn_stats(st6[:, h, :], tot[:, h, :]), reads=[Btot], writes=[Bst])
                for h in range(4):
                    P.op("vector", lambda e: e.bn_aggr(mv[:, h, :], st6[:, h, :]), reads=[Bst], writes=[Bst])
                P.op("scalar", lambda e: e.activation(sm[:, 5, :], mv[:, :, 1], AF.Ln, bias=LN_EPS, scale=1.0), reads=[Bst], writes=[Bsm])
                P.op("scalar", lambda e: e.activation(sm[:, 5, :], sm[:, 5, :], AF.Exp, scale=-0.5), reads=[Bsm], writes=[Bsm])
                for h in range(4):
                    P.op("vector", lambda e: e.tensor_scalar(tot[:, h, :], tot[:, h, :], mv[:, h, 0:1], sm[:, 5, h:h + 1],
                                                             ALU.subtract, ALU.mult), reads=[Btot, Bst, Bsm], writes=[Btot])
                H, BH = hml[i], Bhml[i]
                P.op("scalar", lambda e: e.activation(sg[:], og[i][:], AF.Sigmoid), reads=[Bog[i]], writes=[Bsg])
                P.op("vector", lambda e: e.tensor_tensor(H[:], tot[:].rearrange("p h v -> p (h v)"), normg[:], ALU.mult),
                     reads=[Btot, Bng], writes=[BH])
                P.op("vector", lambda e: e.tensor_tensor(H[:], H[:], sg[:], ALU.mult), reads=[BH, Bsg], writes=[BH])
                r0 = b * T + c * 128
                P.store("sync", HCAT[r0:r0 + 128, 0:512], H[:], BH)
                P.mark(6)
                for h in range(4):
                    P.op("vector", lambda e: e.tensor_scalar(kw[:, h, :], ktok[:, h, :],
                                                             tokE[:, 2, h:h + 1], None, ALU.mult), reads=[Bktok, BtokE], writes=[Bkw])
                for h in range(4):
                    P.op("tensor", lambda e: e.matmul(pU[h // 2][:, h % 2, 0:129], kw[:, h, :], vext[:, h, 0:129], start=True, stop=True),
                         reads=[Bkw, Bvext], writes=[Bptq[h // 2]])
                for h in range(4):
                    P.op("vector", lambda e: e.scalar_tensor_tensor(C32[:, h, 0:129], C32[:, h, 0:129],
                                                                    tokE[0:64, 3, h:h + 1], pU[h // 2][:, h % 2, 0:129],
                                                                    ALU.mult, ALU.add), reads=[BC32, BtokE, Bptq[h // 2]], writes=[BC32])
                P.op("scalar", lambda e: e.activation(Cb[:], C32[:], AF.Copy), reads=[BC32], writes=[BCb])
                P.mute = False
                it += 1


def phase_ca(P, l, T, U, HCAT, mem, w):
    ntile = T // 128
    with P.phase():
        identb, Bidb = make_ident(P, BF16, "ca_identb")
        wkv = P.sb("ca_wkv", [128, 8, 1024], BF16)
        Bwkv = Buf("ca_wkv")
        for k in range(8):
            P.load("gpsimd", wkv[:, k, :], w["ca_w_kv"][l, k * 128:(k + 1) * 128, :], Bwkv)
        gam, Bg = bcast_load(P, "ca_g", w["mem_ln_g"], D)
        bet, Bb = bcast_load(P, "ca_b", w["mem_ln_b"], D)
        S = LNScratch(P, "ca")
        mt_ = P.sb("ca_mt", [128, D], F32); Bmt = Buf("ca_mt")
        mb = P.sb("ca_mb", [128, D], BF16); Bmb = Buf("ca_mb")
        memT = P.sb("ca_memT", [128, 8, 256], BF16); BmemT = Buf("ca_memT")
        kT = P.sb("ca_kT", [128, 4, 256], BF16); BkT = Buf("ca_kT")
        Vv = P.sb("ca_V", [128, 2, 512], BF16); BV = Buf("ca_V")
        qt = [P.sb("ca_qt%d" % i, [128, 512], F32) for i in range(2)]
        Bqt = [Buf("ca_qt%d" % i) for i in range(2)]
        qb = P.sb("ca_qb", [128, 512], BF16); Bqb = Buf("ca_qb")
        qT = P.sb("ca_qT", [128, 4, 128], BF16); BqT = Buf("ca_qT")
        mx = P.sb("ca_mx", [128, 4], F32); Bmx = Buf("ca_mx")
        rs = P.sb("ca_rs", [128, 4], F32); Brs = Buf("ca_rs")
        Pm = P.sb("ca_P", [128, 4, 256], BF16); BPm = Buf("ca_P")
        PT = P.sb("ca_PT", [128, 4, 2, 128], BF16); BPT = Buf("ca_PT")
        ho = [P.sb("ca_ho%d" % i, [128, 512], F32) for i in range(2)]
        Bho = [Buf("ca_ho%d" % i) for i in range(2)]
        pmt = P.ps("ca_pmt", [128, 8, 128], BF16); Bpmt = Buf("ca_pmt")
        pkv = P.ps("ca_pkv", [128, 512], F32); Bpkv = Buf("ca_pkv")
        pSc = P.ps("ca_pSc", [128, 4, 256], F32); BpSc = Buf("ca_pSc")
        pPT = P.ps("ca_pPT", [128, 4, 2, 128], BF16); BpPT = Buf("ca_pPT")
        pO = P.ps("ca_pO", [128, 4, 128], F32); BpO = Buf("ca_pO")
        pq = pmt[:, 0:4, :]
        it = 0
        for b in range(NSEQ):
            for mtile in range(2):
                r0 = b * MEM + mtile * 128
                P.load("sync", mt_[:], mem[r0:r0 + 128, :], Bmt)
                layernorm(P, S, mt_[:], Bmt, gam[:], Bg, bet[:], Bb)
                P.op("scalar", lambda e: e.activation(mb[:], mt_[:], AF.Copy), reads=[Bmt], writes=[Bmb])
                for k in range(8):
                    P.op("tensor", lambda e: e.transpose(pmt[:, k, :], mb[:, k * 128:(k + 1) * 128], identb[:]),
                         reads=[Bmb, Bidb], writes=[Bpmt])
                P.op("vector", lambda e: e.tensor_copy(memT[:, :, mtile * 128:(mtile + 1) * 128], pmt[:]), reads=[Bpmt], writes=[BmemT])
            for h in range(4):
                for k in range(8):
                    P.op("tensor", lambda e: e.matmul(pkv[:, 0:256], wkv[:, k, h * 128:(h + 1) * 128], memT[:, k, :],
                                                      start=(k == 0), stop=(k == 7)), reads=[Bwkv, BmemT], writes=[Bpkv])
                P.op("vector", lambda e: e.tensor_copy(kT[:, h, :], pkv[:, 0:256]), reads=[Bpkv], writes=[BkT])
            for mtile in range(2):
                for k in range(8):
                    P.op("tensor", lambda e: e.matmul(pkv[:], memT[:, k, mtile * 128:(mtile + 1) * 128], wkv[:, k, 512:1024],
                                                      start=(k == 0), stop=(k == 7)), reads=[Bwkv, BmemT], writes=[Bpkv])
                P.op("vector", lambda e: e.tensor_copy(Vv[:, mtile, :], pkv[:]), reads=[Bpkv], writes=[BV])
            P.load("sync", qt[it % 2][:], U[b * T:b * T + 128, CAQ0:CAQ0 + 512], Bqt[it % 2])
            for c in range(ntile):
                i = it % 2
                if c + 1 < ntile:
                    r1 = b * T + (c + 1) * 128
                    P.load("sync", qt[1 - i][:], U[r1:r1 + 128, CAQ0:CAQ0 + 512], Bqt[1 - i])
                P.op("scalar", lambda e: e.activation(qb[:], qt[i][:], AF.Copy, scale=128.0 ** -0.5), reads=[Bqt[i]], writes=[Bqb])
                for h in range(4):
                    P.op("tensor", lambda e: e.transpose(pq[:, h, :], qb[:, h * 128:(h + 1) * 128], identb[:]),
                         reads=[Bqb, Bidb], writes=[Bpmt])
                P.op("vector", lambda e: e.tensor_copy(qT[:], pq), reads=[Bpmt], writes=[BqT])
                for h in range(4):
                    P.op("tensor", lambda e: e.matmul(pSc[:, h, :], qT[:, h, :], kT[:, h, :], start=True, stop=True),
                         reads=[BqT, BkT], writes=[BpSc])
                P.op("vector", lambda e: e.tensor_reduce(mx[:], pSc[:], AX.X, ALU.max), reads=[BpSc], writes=[Bmx])
                P.op("vector", lambda e: e.tensor_scalar(mx[:], mx[:], -1.0, None, ALU.mult), reads=[Bmx], writes=[Bmx])
                for h in range(4):
                    P.op("scalar", lambda e: e.activation(Pm[:, h, :], pSc[:, h, :], AF.Exp, bias=mx[:, h:h + 1], scale=1.0,
                                                          accum_out=rs[:, h:h + 1]), reads=[BpSc, Bmx], writes=[BPm, Brs])
                for h in range(4):
                    for mtile in range(2):
                        P.op("tensor", lambda e: e.transpose(pPT[:, h, mtile, :], Pm[:, h, mtile * 128:(mtile + 1) * 128], identb[:]),
                             reads=[BPm, Bidb], writes=[BpPT])
                P.op("vector", lambda e: e.tensor_copy(PT[:], pPT[:]), reads=[BpPT], writes=[BPT])
                for h in range(4):
                    for mtile in range(2):
                        P.op("tensor", lambda e: e.matmul(pO[:, h, :], PT[:, h, mtile, :], Vv[:, mtile, h * 128:(h + 1) * 128],
                                                          start=(mtile == 0), stop=(mtile == 1)), reads=[BPT, BV], writes=[BpO])
                P.op("vector", lambda e: e.reciprocal(rs[:], rs[:]), reads=[Brs], writes=[Brs])
                H, BH = ho[i], Bho[i]
                P.op("vector", lambda e: e.tensor_tensor(H[:].rearrange("p (h v) -> p h v", h=4), pO[:],
                                                         rs[:].unsqueeze(2).to_broadcast([128, 4, 128]), ALU.mult),
                     reads=[BpO, Brs], writes=[BH])
                r0 = b * T + c * 128
                P.store("sync", HCAT[r0:r0 + 128, 1024:1536], H[:], BH)
                it += 1


def phase_rw_pre(P, l, T, U, RWOP, RWG, RWB, w):
    ntile = T // 128
    with P.phase():
        identb, Bidb = make_ident(P, BF16, "rp_identb")
        mu, Bmu = bcast_load(P, "rp_mu", w["rw_mu"][l], 1792)
        w0, Bw0 = bcast_load(P, "rp_w0", w["rw_w0"][l], 512)
        a0, Ba0 = bcast_load(P, "rp_a0", w["rw_a0"][l], 512)
        kkb, Bkkb = bcast_load(P, "rp_kk", w["rw_kk"][l], 512)
        kab, Bkab = bcast_load(P, "rp_ka", w["rw_ka"][l], 512)
        rkb, Brkb = bcast_load(P, "rp_rk", w["rw_rk"][l].rearrange("h k -> (h k)"), 512)
        wup = P.sb("rp_wup", [64, 512], BF16); Bwup = Buf("rp_wup")
        aup = P.sb("rp_aup", [64, 512], BF16); Baup = Buf("rp_aup")
        gup = P.sb("rp_gup", [128, 512], BF16); Bgup = Buf("rp_gup")
        P.load("gpsimd", wup[:], w["rw_w_up"][l], Bwup)
        P.load("gpsimd", aup[:], w["rw_a_up"][l], Baup)
        P.load("gpsimd", gup[:], w["rw_g_up"][l], Bgup)
        ucur = [P.sb("rp_ucur%d" % i, [128, 1792], F32) for i in range(2)]
        Bucur = [Buf("rp_ucur%d" % i) for i in range(2)]
        uprev = [P.sb("rp_uprev%d" % i, [128, 1792], F32) for i in range(2)]
        Buprev = [Buf("rp_uprev%d" % i) for i in range(2)]
        xs = P.sb("rp_xs", [128, 1792], F32); Bxs = Buf("rp_xs")
        lb = P.sb("rp_lb", [128, 256], BF16); Blb = Buf("rp_lb")
        lT = P.sb("rp_lT", [128, 3, 128], BF16); BlT = Buf("rp_lT")
        zt = P.sb("rp_zt", [128, 512], F32); Bzt = Buf("rp_zt")
        nz = P.sb("rp_nz", [128, 512], F32); Bnz = Buf("rp_nz")
        az = P.sb("rp_az", [128, 512], F32); Baz = Buf("rp_az")
        av = P.sb("rp_av", [128, 512], F32); Bav = Buf("rp_av")
        t1 = P.sb("rp_t1", [128, 512], F32); Bt1 = Buf("rp_t1")
        ss = P.sb("rp_ss", [128, 8], F32); Bss = Buf("rp_ss")
        ops = [P.sb("rp_ops%d" % i, [128, 6, 512], F32) for i in range(2)]
        Bops = [Buf("rp_ops%d" % i) for i in range(2)]
        gg = [P.sb("rp_g%d" % i, [128, 512], F32) for i in range(2)]
        Bgg = [Buf("rp_g%d" % i) for i in range(2)]
        bc = [P.sb("rp_bc%d" % i, [128, 8], F32) for i in range(2)]
        Bbc = [Buf("rp_bc%d" % i) for i in range(2)]
        plT = P.ps("rp_plT", [128, 3, 128], BF16); BplT = Buf("rp_plT")
        pw = P.ps("rp_pw", [128, 512], F32); Bpw = Buf("rp_pw")
        pa = P.ps("rp_pa", [128, 512], F32); Bpa = Buf("rp_pa")
        pg = P.ps("rp_pg", [128, 512], F32); Bpg = Buf("rp_pg")

        def loads(b, c, i):
            r0 = b * T + c * 128
            P.load("sync", ucur[i][:], U[r0:r0 + 128, RW0:RW0 + 1792], Bucur[i])
            if c == 0:
                P.op("vector", lambda e: e.memset(uprev[i][0:1, :], 0.0), writes=[Buprev[i]])
                P.load("sync", uprev[i][1:128, :], U[r0:r0 + 127, RW0:RW0 + 1792], Buprev[i])
            else:
                P.load("sync", uprev[i][:], U[r0 - 1:r0 + 127, RW0:RW0 + 1792], Buprev[i])

        tiles = [(b, c) for b in range(NSEQ) for c in range(ntile)]
        loads(0, 0, 0)
        for it, (b, c) in enumerate(tiles):
            i = it % 2
            if it + 1 < len(tiles):
                loads(tiles[it + 1][0], tiles[it + 1][1], 1 - i)
            O, BO = ops[i], Bops[i]
            P.op("vector", lambda e: e.tensor_tensor(xs[:], uprev[i][:], ucur[i][:], ALU.subtract), reads=[Buprev[i], Bucur[i]], writes=[Bxs])
            P.op("gpsimd", lambda e: e.tensor_tensor(xs[:], xs[:], mu[:], ALU.mult), reads=[Bxs, Bmu], writes=[Bxs])
            P.op("vector", lambda e: e.tensor_tensor(xs[:], xs[:], ucur[i][:], ALU.add), reads=[Bxs, Bucur[i]], writes=[Bxs])
            P.op("scalar", lambda e: e.activation(O[:, 0, :], xs[:, 0:512], AF.Copy), reads=[Bxs], writes=[BO])
            P.op("scalar", lambda e: e.activation(O[:, 5, :], xs[:, 1024:1536], AF.Copy), reads=[Bxs], writes=[BO])
            P.op("scalar", lambda e: e.activation(lb[:, 0:64], xs[:, 1536:1600], AF.Tanh), reads=[Bxs], writes=[Blb])
            P.op("scalar", lambda e: e.activation(lb[:, 128:256], xs[:, 1664:1792], AF.Sigmoid), reads=[Bxs], writes=[Blb])
            P.op("vector", lambda e: e.tensor_copy(lb[:, 64:128], xs[:, 1600:1664]), reads=[Bxs], writes=[Blb])
            P.op("tensor", lambda e: e.transpose(plT[0:64, 0, :], lb[:, 0:64], identb[:]), reads=[Blb, Bidb], writes=[BplT])
            P.op("tensor", lambda e: e.transpose(plT[0:64, 1, :], lb[:, 64:128], identb[:]), reads=[Blb, Bidb], writes=[BplT])
            P.op("tensor", lambda e: e.transpose(plT[:, 2, :], lb[:, 128:256], identb[:]), reads=[Blb, Bidb], writes=[BplT])
            P.op("vector", lambda e: e.tensor_copy(lT[0:64, 0:2, :], plT[0:64, 0:2, :]), reads=[BplT], writes=[BlT])
            P.op("vector", lambda e: e.tensor_copy(lT[:, 2, :], plT[:, 2, :]), reads=[BplT], writes=[BlT])
            P.op("tensor", lambda e: e.matmul(pw[:], lT[0:64, 0, :], wup[:], start=True, stop=True), reads=[BlT, Bwup], writes=[Bpw])
            P.op("tensor", lambda e: e.matmul(pa[:], lT[0:64, 1, :], aup[:], start=True, stop=True), reads=[BlT, Baup], writes=[Bpa])
            P.op("tensor", lambda e: e.matmul(pg[:], lT[:, 2, :], gup[:], start=True, stop=True), reads=[BlT, Bgup], writes=[Bpg])
            P.op("vector", lambda e: e.tensor_tensor(zt[:], pw[:], w0[:], ALU.add), reads=[Bpw, Bw0], writes=[Bzt])
            P.op("vector", lambda e: e.tensor_scalar(nz[:], zt[:], -1.0, None, ALU.mult), reads=[Bzt], writes=[Bnz])
            P.op("vector", lambda e: e.tensor_tensor(az[:], zt[:], nz[:], ALU.max), reads=[Bzt, Bnz], writes=[Baz])
            P.op("scalar", lambda e: e.activation(az[:], az[:], AF.Exp, scale=-1.0), reads=[Baz], writes=[Baz])
            P.op("scalar", lambda e: e.activation(az[:], az[:], AF.Ln, bias=1.0, scale=1.0), reads=[Baz], writes=[Baz])
            P.op("vector", lambda e: e.scalar_tensor_tensor(az[:], nz[:], 0.0, az[:], ALU.max, ALU.add), reads=[Bnz, Baz], writes=[Baz])
            P.op("scalar", lambda e: e.activation(az[:], az[:], AF.Exp, bias=-0.5, scale=-1.0), reads=[Baz], writes=[Baz])
            P.op("scalar", lambda e: e.activation(O[:, 1, :], az[:], AF.Exp, scale=-1.0), reads=[Baz], writes=[BO])
            P.op("vector", lambda e: e.tensor_tensor(av[:], pa[:], a0[:], ALU.add), reads=[Bpa, Ba0], writes=[Bav])
            P.op("scalar", lambda e: e.activation(av[:], av[:], AF.Sigmoid), reads=[Bav], writes=[Bav])
            P.op("scalar", lambda e: e.activation(gg[i][:], pg[:], AF.Copy), reads=[Bpg], writes=[Bgg[i]])
            kr = xs[:, 512:1024]
            P.op("vector", lambda e: e.tensor_tensor(O[:, 3, :], kr, kkb[:], ALU.mult), reads=[Bxs, Bkkb], writes=[BO])
            P.op("gpsimd", lambda e: e.tensor_tensor(t1[:], O[:, 3, :], O[:, 3, :], ALU.mult), reads=[BO], writes=[Bt1])
            P.op("vector", lambda e: e.tensor_reduce(ss[:], t1[:].rearrange("p (h k) -> p h k", h=8), AX.X, ALU.add), reads=[Bt1], writes=[Bss])
            P.op("vector", lambda e: e.tensor_scalar(ss[:], ss[:], 1e-24, None, ALU.max), reads=[Bss], writes=[Bss])
            P.op("scalar", lambda e: e.activation(ss[:], ss[:], AF.Sqrt), reads=[Bss], writes=[Bss])
            P.op("vector", lambda e: e.reciprocal(ss[:], ss[:]), reads=[Bss], writes=[Bss])
            P.op("vector", lambda e: e.tensor_tensor(O[:, 3, :].rearrange("p (h k) -> p h k", h=8), O[:, 3, :].rearrange("p (h k) -> p h k", h=8),
                                                     ss[:].unsqueeze(2).to_broadcast([128, 8, 64]), ALU.mult), reads=[BO, Bss], writes=[BO])
            P.op("vector", lambda e: e.scalar_tensor_tensor(t1[:], av[:], -1.0, kab[:], ALU.add, ALU.mult), reads=[Bav, Bkab], writes=[Bt1])
            P.op("vector", lambda e: e.scalar_tensor_tensor(O[:, 2, :], t1[:], 1.0, kr, ALU.add, ALU.mult), reads=[Bt1, Bxs], writes=[BO])
            P.op("gpsimd", lambda e: e.tensor_tensor(O[:, 4, :], O[:, 3, :], av[:], ALU.mult), reads=[BO, Bav], writes=[BO])
            P.op("vector", lambda e: e.tensor_tensor(t1[:], O[:, 0, :], O[:, 2, :], ALU.mult), reads=[BO], writes=[Bt1])
            P.op("gpsimd", lambda e: e.tensor_tensor(t1[:], t1[:], rkb[:], ALU.mult), reads=[Bt1, Brkb], writes=[Bt1])
            P.op("vector", lambda e: e.tensor_reduce(bc[i][:], t1[:].rearrange("p (h k) -> p h k", h=8), AX.X, ALU.add), reads=[Bt1], writes=[Bbc[i]])
            r0 = b * T + c * 128
            P.store("sync", RWOP[r0:r0 + 128, :, :], O[:], BO)
            P.store("sync", RWG[r0:r0 + 128, :], gg[i][:], Bgg[i])
            P.store("sync", RWB[r0:r0 + 128, :], bc[i][:], Bbc[i])


def phase_rw_scan(P, l, T, RWOP, RWO):
    nblk = T // 128
    with P.phase():
        identf, Bidf = make_ident(P, F32, "rs_identf")
        E = P.sb("rs_E", [128, 64, 128], F32); BE = Buf("rs_E")
        P.op("gpsimd", lambda e: e.memset(E[:], 0.0), writes=[BE])
        for j in range(2):
            v_ = E[j * 64:(j + 1) * 64, :, j * 64:(j + 1) * 64]
            P.op("gpsimd", lambda e: e.affine_select(v_, v_, pattern=[[-1, 64], [0, 64]], compare_op=ALU.not_equal,
                                                     fill=1.0, base=0, channel_multiplier=1), reads=[BE], writes=[BE])
        S = P.sb("rs_S", [128, 512], F32); BS = Buf("rs_S")
        P.op("vector", lambda e: e.memset(S[:], 0.0), writes=[BS])
        Xs = [P.sb("rs_X%d" % i, [128, 5, 2, 256], F32) for i in range(2)]
        BX = [Buf("rs_X%d" % i) for i in range(2)]
        vt = [P.sb("rs_vt%d" % i, [128, 2, 4, 128], F32) for i in range(2)]
        Bvt = [Buf("rs_vt%d" % i) for i in range(2)]
        Vc = [P.sb("rs_Vc%d" % i, [128, 8, 128], F32) for i in range(2)]
        BVc = [Buf("rs_Vc%d" % i) for i in range(2)]
        Ob = [P.sb("rs_Ob%d" % i, [128, 128, 8], F32) for i in range(2)]
        BOb = [Buf("rs_Ob%d" % i) for i in range(2)]
        otok = [P.sb("rs_ot%d" % i, [128, 8, 128], F32) for i in range(2)]
        Bot = [Buf("rs_ot%d" % i) for i in range(2)]
        NR = 3
        R = [P.sb("rs_R%d" % i, [128, 5, 512], F32) for i in range(NR)]
        BR = [[Buf("rs_R%d_%d" % (i, o)) for o in range(5)] for i in range(NR)]
        vk = [P.sb("rs_vk%d" % i, [128, 512], F32) for i in range(2)]
        Bvk = [Buf("rs_vk%d" % i) for i in range(2)]
        tmp = P.sb("rs_tmp", [128, 512], F32); Btmp = Buf("rs_tmp")
        sa = P.sb("rs_sa", [128, 8], F32); Bsa = Buf("rs_sa")
        NPOP = 4
        pop = [P.ps("rs_pop%d" % i, [128, 512], F32) for i in range(NPOP)]
        Bpop = [Buf("rs_pop%d" % i) for i in range(NPOP)]
        pv = P.ps("rs_pv", [128, 8, 128], F32); Bpv = Buf("rs_pv")
        po = P.ps("rs_po", [128, 8, 128], F32); Bpo = Buf("rs_po")

        def g3(ap):
            return ap.rearrange("p (g k) -> p g k", g=8)

        def load_seg(s, i):
            for half in range(2):
                for b in range(NSEQ):
                    r0 = b * T + s * 64
                    P.load("sync", Xs[i][half * 64:(half + 1) * 64, :, b, :],
                           RWOP[r0:r0 + 64, 0:5, half * 256:(half + 1) * 256], BX[i])

        def load_v(blk, i):
            for b in range(NSEQ):
                r0 = b * T + blk * 128
                for g in range(4):
                    P.load("sync", vt[i][:, b, g, :].rearrange("p (j v) -> p j v", j=2),
                           RWOP[r0:r0 + 128, 5, :].rearrange("t (j g v) -> t g j v", j=2, g=4)[:, g], Bvt[i])

        load_seg(0, 0)
        load_v(0, 0)
        pc = 0
        step = 0
        for blk in range(nblk):
            bi = blk % 2
            if blk + 1 < nblk:
                load_v(blk + 1, 1 - bi)
            for bg in range(8):
                P.op("tensor", lambda e: e.transpose(pv[:, bg, :], vt[bi][:, bg // 4, bg % 4, :], identf[:]),
                     reads=[Bvt[bi], Bidf], writes=[Bpv])
            P.op("scalar", lambda e: e.activation(Vc[bi][:], pv[:], AF.Copy), reads=[Bpv], writes=[BVc[bi]])
            for sg in range(2):
                s = blk * 2 + sg
                xi = s % 2
                if s + 1 < 2 * nblk:
                    load_seg(s + 1, 1 - xi)
                for t in range(64):
                    tt = sg * 64 + t
                    ri = step % NR
                    for o in range(5):
                        pp, Bp = pop[pc % NPOP], Bpop[pc % NPOP]
                        pc += 1
                        P.op("tensor", lambda e: e.matmul(pp[:], E[:, t, :], Xs[xi][:, o, :, :].rearrange("p b c -> p (b c)"),
                                                          start=True, stop=True), reads=[BE, BX[xi]], writes=[Bp])
                        P.op("scalar", lambda e: e.activation(R[ri][:, o, :], pp[:], AF.Copy), reads=[Bp], writes=[BR[ri][o]])
                    P.mark(11)
                    r_b, w_b, k_b, kk_b, kka_b = (R[ri][:, o, :] for o in range(5))
                    Br, Bw, Bk, Bkk, Bkka = BR[ri]
                    vi = step % 2
                    P.op("gpsimd", lambda e: e.tensor_tensor(g3(vk[vi][:]), g3(k_b), Vc[bi][:, :, tt].unsqueeze(2).to_broadcast([128, 8, 64]), ALU.mult),
                         reads=[Bk, BVc[bi]], writes=[Bvk[vi]])
                    P.mark(12)
                    P.op("vector", lambda e: e.tensor_tensor(tmp[:], S[:], kk_b, ALU.mult), reads=[BS, Bkk], writes=[Btmp])
                    P.op("vector", lambda e: e.tensor_reduce(sa[:], g3(tmp[:]), AX.X, ALU.add), reads=[Btmp], writes=[Bsa])
                    P.mark(13)
                    P.op("vector", lambda e: e.tensor_tensor(S[:], S[:], w_b, ALU.mult), reads=[BS, Bw], writes=[BS])
                    P.op("vector", lambda e: e.tensor_tensor(g3(tmp[:]), g3(kka_b), sa[:].unsqueeze(2).to_broadcast([128, 8, 64]), ALU.mult),
                         reads=[Bkka, Bsa], writes=[Btmp])
                    P.op("vector", lambda e: e.tensor_tensor(S[:], S[:], tmp[:], ALU.subtract), reads=[BS, Btmp], writes=[BS])
                    P.op("vector", lambda e: e.tensor_tensor(S[:], S[:], vk[vi][:], ALU.add), reads=[BS, Bvk[vi]], writes=[BS])
                    P.op("vector", lambda e: e.tensor_tensor(tmp[:], S[:], r_b, ALU.mult), reads=[BS, Br], writes=[Btmp])
                    P.op("vector", lambda e: e.tensor_reduce(Ob[bi][:, tt, :], g3(tmp[:]), AX.X, ALU.add), reads=[Btmp], writes=[BOb[bi]])
                    P.mute = False
                    step += 1
            P.mark(14)
            for bg in range(8):
                P.op("tensor", lambda e: e.transpose(po[:, bg, :], Ob[bi][:, :, bg], identf[:]), reads=[BOb[bi], Bidf], writes=[Bpo])
            P.op("scalar", lambda e: e.activation(otok[bi][:], po[:], AF.Copy), reads=[Bpo], writes=[Bot[bi]])
            for b in range(NSEQ):
                r0 = b * T + blk * 128
                for g in range(4):
                    P.store("sync", RWO[r0:r0 + 128, :].rearrange("t (j g v) -> t g j v", j=2, g=4)[:, g],
                            otok[bi][:, b * 4 + g, :].rearrange("p (j v) -> p j v", j=2), Bot[bi])
            P.mute = False


def phase_rw_post(P, l, T, RWOP, RWG, RWB, RWO, HCAT, w):
    ntile = NSEQ * T // 128
    with P.phase():
        lng, Blng = bcast_load(P, "rq_lng", w["rw_ln_g"][l], 512)
        lnb, Blnb = bcast_load(P, "rq_lnb", w["rw_ln_b"][l], 512)
        ot = [P.sb("rq_o%d" % i, [128, 512], F32) for i in range(2)]
        Bot = [Buf("rq_o%d" % i) for i in range(2)]
        vv = [P.sb("rq_v%d" % i, [128, 512], F32) for i in range(2)]
        Bvv = [Buf("rq_v%d" % i) for i in range(2)]
        gg = [P.sb("rq_g%d" % i, [128, 512], F32) for i in range(2)]
        Bgg = [Buf("rq_g%d" % i) for i in range(2)]
        bc = [P.sb("rq_bc%d" % i, [128, 8], F32) for i in range(2)]
        Bbc = [Buf("rq_bc%d" % i) for i in range(2)]
        st6 = P.sb("rq_st6", [128, 8, 6], F32)
        mv = P.sb("rq_mv", [128, 8, 2], F32)
        rstd = P.sb("rq_rstd", [128, 8], F32)
        Bst = Buf("rq_st")
        hh = [P.sb("rq_h%d" % i, [128, 512], F32) for i in range(2)]
        Bhh = [Buf("rq_h%d" % i) for i in range(2)]

        def g3(ap):
            return ap.rearrange("p (g k) -> p g k", g=8)

        def loads(it, i):
            r0 = it * 128
            P.load("sync", ot[i][:], RWO[r0:r0 + 128, :], Bot[i])
            P.load("sync", vv[i][:], RWOP[r0:r0 + 128, 5, :], Bvv[i])
            P.load("sync", gg[i][:], RWG[r0:r0 + 128, :], Bgg[i])
            P.load("sync", bc[i][:], RWB[r0:r0 + 128, :], Bbc[i])

        loads(0, 0)
        for it in range(ntile):
            i = it % 2
            if it + 1 < ntile:
                loads(it + 1, 1 - i)
            O, BO = ot[i], Bot[i]
            for h in range(8):
                P.op("vector", lambda e: e.bn_stats(st6[:, h, :], O[:, h * 64:(h + 1) * 64]), reads=[BO], writes=[Bst])
            for h in range(8):
                P.op("vector", lambda e: e.bn_aggr(mv[:, h, :], st6[:, h, :]), reads=[Bst], writes=[Bst])
            P.op("scalar", lambda e: e.activation(rstd[:], mv[:, :, 1], AF.Ln, bias=64e-5, scale=1.0), reads=[Bst], writes=[Bst])
            P.op("scalar", lambda e: e.activation(rstd[:], rstd[:], AF.Exp, scale=-0.5), reads=[Bst], writes=[Bst])
            H, BH = hh[i], Bhh[i]
            P.op("vector", lambda e: e.tensor_tensor(g3(H[:]), g3(O[:]), mv[:, :, 0].unsqueeze(2).to_broadcast([128, 8, 64]), ALU.subtract),
                 reads=[BO, Bst], writes=[BH])
            P.op("vector", lambda e: e.tensor_tensor(g3(H[:]), g3(H[:]), rstd[:].unsqueeze(2).to_broadcast([128, 8, 64]), ALU.mult),
                 reads=[BH, Bst], writes=[BH])
            P.op("gpsimd", lambda e: e.tensor_tensor(H[:], H[:], lng[:], ALU.mult), reads=[BH, Blng], writes=[BH])
            P.op("gpsimd", lambda e: e.tensor_tensor(H[:], H[:], lnb[:], ALU.add), reads=[BH, Blnb], writes=[BH])
            P.op("vector", lambda e: e.tensor_tensor(g3(vv[i][:]), g3(vv[i][:]), bc[i][:].unsqueeze(2).to_broadcast([128, 8, 64]), ALU.mult),
                 reads=[Bvv[i], Bbc[i]], writes=[Bvv[i]])
            P.op("vector", lambda e: e.tensor_tensor(H[:], H[:], vv[i][:], ALU.add), reads=[BH, Bvv[i]], writes=[BH])
            P.op("vector", lambda e: e.tensor_tensor(H[:], H[:], gg[i][:], ALU.mult), reads=[BH, Bgg[i]], writes=[BH])
            r0 = it * 128
            P.store("sync", HCAT[r0:r0 + 128, 512:1024], H[:], BH)


def phase_merge(P, l, NT, U, HCAT, XRES, X1, w):
    ntile = NT // 128
    with P.phase():
        identb, Bidb = make_ident(P, BF16, "mg_identb")
        wbr = P.sb("mg_wbr", [128, 12, D], BF16); Bwbr = Buf("mg_wbr")
        for bi, nm in enumerate(("w_br_ml", "w_br_rw", "w_br_ca")):
            for k in range(4):
                P.load("gpsimd", wbr[:, bi * 4 + k, :], w[nm][l, k * 128:(k + 1) * 128, :], Bwbr)
        wo = P.sb("mg_wo", [128, 8, D], BF16); Bwo = Buf("mg_wo")
        for k in range(8):
            P.load("gpsimd", wo[:, k, :], w["w_o"][l, k * 128:(k + 1) * 128, :], Bwo)
        gbias, Bgb = bcast_load(P, "mg_gb", w["gate_b"][l], 3 * D)
        gam, Bg = bcast_load(P, "mg_g", w["ln1_g"][l], D)
        bet, Bb = bcast_load(P, "mg_b", w["ln1_b"][l], D)
        S = LNScratch(P, "mg")
        hc = [P.sb("mg_hc%d" % i, [128, 1536], F32) for i in range(2)]
        Bhc = [Buf("mg_hc%d" % i) for i in range(2)]
        ug = [P.sb("mg_ug%d" % i, [128, 3 * D], F32) for i in range(2)]
        Bug = [Buf("mg_ug%d" % i) for i in range(2)]
        xr = [P.sb("mg_xr%d" % i, [128, D], F32) for i in range(2)]
        Bxr = [Buf("mg_xr%d" % i) for i in range(2)]
        hb = P.sb("mg_hb", [128, 1536], BF16); Bhb = Buf("mg_hb")
        hT = P.sb("mg_hT", [128, 12, 128], BF16); BhT = Buf("mg_hT")
        ym = P.sb("mg_ym", [128, D], F32); Bym = Buf("mg_ym")
        tmp = P.sb("mg_tmp", [128, 512], F32); Btmp = Buf("mg_tmp")
        ymb = P.sb("mg_ymb", [128, D], BF16); Bymb = Buf("mg_ymb")
        yT = P.sb("mg_yT", [128, 8, 128], BF16); ByT = Buf("mg_yT")
        pt = P.ps("mg_pt", [128, 16, 128], BF16); Bpt = Buf("mg_pt")
        pb = [P.ps("mg_pb%d" % i, [128, 512], F32) for i in range(3)]
        Bpb = [Buf("mg_pb%d" % i) for i in range(3)]

        def loads(it, i):
            r0 = it * 128
            P.load("sync", hc[i][:], HCAT[r0:r0 + 128, :], Bhc[i])
            P.load("sync", ug[i][:], U[r0:r0 + 128, GATE0:GATE0 + 3 * D], Bug[i])
            P.load("sync", xr[i][:], XRES[r0:r0 + 128, :], Bxr[i])

        loads(0, 0)
        pc = 0
        for it in range(ntile):
            i = it % 2
            if it + 1 < ntile:
                loads(it + 1, 1 - i)
            P.op("scalar", lambda e: e.activation(hb[:], hc[i][:], AF.Copy), reads=[Bhc[i]], writes=[Bhb])
            for k in range(12):
                P.op("tensor", lambda e: e.transpose(pt[:, k, :], hb[:, k * 128:(k + 1) * 128], identb[:]), reads=[Bhb, Bidb], writes=[Bpt])
            P.op("vector", lambda e: e.tensor_copy(hT[:], pt[:, 0:12, :]), reads=[Bpt], writes=[BhT])
            P.op("gpsimd", lambda e: e.tensor_tensor(ug[i][:], ug[i][:], gbias[:], ALU.add), reads=[Bug[i], Bgb], writes=[Bug[i]])
            P.op("scalar", lambda e: e.activation(ug[i][:], ug[i][:], AF.Sigmoid), reads=[Bug[i]], writes=[Bug[i]])
            for br in range(3):
                for half in range(2):
                    pp, Bp = pb[pc % 3], Bpb[pc % 3]
                    pc += 1
                    for k in range(4):
                        P.op("tensor", lambda e: e.matmul(pp[:], hT[:, br * 4 + k, :], wbr[:, br * 4 + k, half * 512:(half + 1) * 512],
                                                          start=(k == 0), stop=(k == 3)), reads=[BhT, Bwbr], writes=[Bp])
                    gsl = ug[i][:, br * D + half * 512:br * D + (half + 1) * 512]
                    ysl = ym[:, half * 512:(half + 1) * 512]
                    if br == 0:
                        P.op("vector", lambda e: e.tensor_tensor(ysl, pp[:], gsl, ALU.mult), reads=[Bp, Bug[i]], writes=[Bym])
                    else:
                        P.op("vector", lambda e: e.tensor_tensor(tmp[:], pp[:], gsl, ALU.mult), reads=[Bp, Bug[i]], writes=[Btmp])
                        P.op("gpsimd", lambda e: e.tensor_tensor(ysl, ysl, tmp[:], ALU.add), reads=[Bym, Btmp], writes=[Bym])
            P.op("scalar", lambda e: e.activation(ymb[:], ym[:], AF.Copy), reads=[Bym], writes=[Bymb])
            for k in range(8):
                P.op("tensor", lambda e: e.transpose(pt[:, k, :], ymb[:, k * 128:(k + 1) * 128], identb[:]), reads=[Bymb, Bidb], writes=[Bpt])
            P.op("vector", lambda e: e.tensor_copy(yT[:], pt[:, 0:8, :]), reads=[Bpt], writes=[ByT])
            X, BX = xr[i], Bxr[i]
            for half in range(2):
                pp, Bp = pb[pc % 3], Bpb[pc % 3]
                pc += 1
                for k in range(8):
                    P.op("tensor", lambda e: e.matmul(pp[:], yT[:, k, :], wo[:, k, half * 512:(half + 1) * 512],
                                                      start=(k == 0), stop=(k == 7)), reads=[ByT, Bwo], writes=[Bp])
                xsl = X[:, half * 512:(half + 1) * 512]
                P.op("vector", lambda e: e.scalar_tensor_tensor(xsl, xsl, DN_ALPHA, pp[:], ALU.mult, ALU.add), reads=[BX, Bp], writes=[BX])
            layernorm(P, S, X[:], BX, gam[:], Bg, bet[:], Bb)
            P.store("sync", X1[it * 128:(it + 1) * 128, :], X[:], BX)


def phase_moe(P, l, NT, X1, XOUT, w):
    NTS = min(1024, NT)
    QT = min(512, NTS)
    nsup = NT // NTS
    tps = NTS // 128
    with P.phase():
        identf, Bidf = make_ident(P, F32, "me_identf")
        rwf = P.sb("me_rw", [128, 8, NE], F32); Brwf = Buf("me_rw")
        P.load("sync", rwf[:], w["router_w"][l].rearrange("(k p) e -> p k e", p=128), Brwf)
        rbias, Brb = bcast_load(P, "me_rb", w["router_b"][l], NE)
        bdn = P.sb("me_bdn", [NE, D], F32); Bbdn = Buf("me_bdn")
        P.load("sync", bdn[:], w["b_dn"][l], Bbdn)
        bgu = P.sb("me_bgu", [128, NE, 8, 2], F32); Bbgu = Buf("me_bgu")
        for e_ in range(NE):
            P.load("sync", bgu[:, e_, :, :], w["b_gu"][l, e_].rearrange("(i p two) -> p i two", p=128, two=2), Bbgu)
        gam, Bg = bcast_load(P, "me_g", w["ln2_g"][l], D)
        bet, Bb = bcast_load(P, "me_b", w["ln2_b"][l], D)
        S = LNScratch(P, "me")
        xt = [P.sb("me_xt%d" % i, [128, D], F32) for i in range(2)]
        Bxt = [Buf("me_xt%d" % i) for i in range(2)]
        xTf = P.sb("me_xTf", [128, 8, 128], F32); BxTf = Buf("me_xTf")
        xT = P.sb("me_xT", [128, 8, NTS], BF16); BxT = Buf("me_xT")
        lg = P.sb("me_lg", [128, NE], F32); Blg = Buf("me_lg")
        top8 = P.sb("me_top8", [128, 8], F32); Btop8 = Buf("me_top8")
        msk = P.sb("me_msk", [128, NE], F32); Bmsk = Buf("me_msk")
        ssum = P.sb("me_ssum", [128, 1], F32); Bssum = Buf("me_ssum")
        G = P.sb("me_G", [128, tps, NE], F32); BG = Buf("me_G")
        GT = P.sb("me_GT", [NE, tps, 128], F32); BGT = Buf("me_GT")
        acc = P.sb("me_acc", [128, tps, D], F32); Bacc = Buf("me_acc")
        wgu = [P.sb("me_wgu%d" % i, [128, 8, 2 * D], BF16) for i in range(2)]
        Bwgu = [Buf("me_wgu%d" % i) for i in range(2)]
        wdn = [P.sb("me_wdn%d" % i, [128, 8, D], BF16) for i in range(2)]
        Bwdn = [Buf("me_wdn%d" % i) for i in range(2)]
        actT = [P.sb("me_actT%d" % i, [128, 8, QT], BF16) for i in range(2)]
        BactT = [Buf("me_actT%d" % i) for i in range(2)]
        glu = P.sb("me_glu", [128, QT], F32); Bglu = Buf("me_glu")
        sgm = P.sb("me_sgm", [128, QT], F32); Bsgm = Buf("me_sgm")
        lin = P.sb("me_lin", [128, QT], F32); Blin = Buf("me_lin")
        pg = [P.ps("me_pg%d" % i, [128, 512], F32) for i in range(2)]
        Bpg = [Buf("me_pg%d" % i) for i in range(2)]
        pl = [P.ps("me_pl%d" % i, [128, 512], F32) for i in range(2)]
        Bpl = [Buf("me_pl%d" % i) for i in range(2)]
        pd = [P.ps("me_pd%d" % i, [128, 512], F32) for i in range(2)]
        Bpd = [Buf("me_pd%d" % i) for i in range(2)]
        px = P.ps("me_px", [128, 8, 128], F32); Bpx = Buf("me_px")

        def load_w(e_, i):
            for k in range(8):
                P.load("gpsimd", wgu[i][:, k, :], w["w_gu"][l, e_, k * 128:(k + 1) * 128, :], Bwgu[i])
            for k in range(8):
                P.load("gpsimd", wdn[i][:, k, :], w["w_dn"][l, e_, k * 128:(k + 1) * 128, :], Bwdn[i])

        wi = 0
        pc = 0
        ai = 0
        for sp in range(nsup):
            t0 = sp * NTS
            load_w(0, wi % 2)
            P.load("sync", xt[0][:], X1[t0:t0 + 128, :], Bxt[0])
            for tl in range(tps):
                i = tl % 2
                if tl + 1 < tps:
                    P.load("sync", xt[1 - i][:], X1[t0 + (tl + 1) * 128:t0 + (tl + 2) * 128, :], Bxt[1 - i])
                for k in range(8):
                    P.op("tensor", lambda e: e.transpose(px[:, k, :], xt[i][:, k * 128:(k + 1) * 128], identf[:]), reads=[Bxt[i], Bidf], writes=[Bpx])
                P.op("vector", lambda e: e.tensor_copy(xTf[:], px[:]), reads=[Bpx], writes=[BxTf])
                P.op("scalar", lambda e: e.activation(xT[:, :, tl * 128:(tl + 1) * 128], xTf[:], AF.Copy), reads=[BxTf], writes=[BxT])
                for k in range(8):
                    P.op("tensor", lambda e: e.matmul(px[:, 0, 0:NE], xTf[:, k, :], rwf[:, k, :], start=(k == 0), stop=(k == 7)),
                         reads=[BxTf, Brwf], writes=[Bpx])
                P.op("vector", lambda e: e.tensor_tensor(lg[:], px[:, 0, 0:NE], rbias[:], ALU.add), reads=[Bpx, Brb], writes=[Blg])
                P.op("vector", lambda e: e.max(out=top8[:], in_=lg[:]), reads=[Blg], writes=[Btop8])
                P.op("vector", lambda e: e.tensor_scalar(msk[:], lg[:], top8[:, 3:4], None, ALU.is_ge), reads=[Blg, Btop8], writes=[Bmsk])
                P.op("vector", lambda e: e.tensor_scalar(top8[:, 0:1], top8[:, 0:1], -1.0, None, ALU.mult), reads=[Btop8], writes=[Btop8])
                P.op("scalar", lambda e: e.activation(lg[:], lg[:], AF.Exp, bias=top8[:, 0:1], scale=1.0), reads=[Blg, Btop8], writes=[Blg])
                P.op("vector", lambda e: e.tensor_tensor(lg[:], lg[:], msk[:], ALU.mult), reads=[Blg, Bmsk], writes=[Blg])
                P.op("vector", lambda e: e.tensor_reduce(ssum[:], lg[:], AX.X, ALU.add), reads=[Blg], writes=[Bssum])
                P.op("vector", lambda e: e.reciprocal(ssum[:], ssum[:]), reads=[Bssum], writes=[Bssum])
                P.op("vector", lambda e: e.tensor_scalar(G[:, tl, :], lg[:], ssum[:, 0:1], None, ALU.mult), reads=[Blg, Bssum], writes=[BG])
                P.op("tensor", lambda e: e.transpose(px[0:NE, 1, :], G[:, tl, :], identf[:]), reads=[BG, Bidf], writes=[Bpx])
                P.op("vector", lambda e: e.tensor_copy(GT[:, tl, :], px[0:NE, 1, :]), reads=[Bpx], writes=[BGT])
            for e_ in range(NE):
                ww = wi % 2
                if e_ + 1 < NE:
                    load_w(e_ + 1, (wi + 1) % 2)
                WG, BWG, WD, BWD = wgu[ww], Bwgu[ww], wdn[ww], Bwdn[ww]
                for q in range(NTS // QT):
                    A, BA = actT[ai % 2], BactT[ai % 2]
                    ai += 1
                    for fc in range(8):
                        pgg, Bpgg = pg[pc % 2], Bpg[pc % 2]
                        pll, Bpll = pl[pc % 2], Bpl[pc % 2]
                        pc += 1
                        for k in range(8):
                            P.op("tensor", lambda e: e.matmul(pgg[:, 0:QT], WG[:, k, fc * 256:(fc + 1) * 256:2], xT[:, k, q * QT:(q + 1) * QT],
                                                              start=(k == 0), stop=(k == 7)), reads=[BWG, BxT], writes=[Bpgg])
                        for k in range(8):
                            P.op("tensor", lambda e: e.matmul(pll[:, 0:QT], WG[:, k, fc * 256 + 1:(fc + 1) * 256:2], xT[:, k, q * QT:(q + 1) * QT],
                                                              start=(k == 0), stop=(k == 7)), reads=[BWG, BxT], writes=[Bpll])
                        P.op("vector", lambda e: e.tensor_scalar(glu[:], pgg[:, 0:QT], bgu[:, e_, fc, 0:1], 7.0, ALU.add, ALU.min),
                             reads=[Bpgg, Bbgu], writes=[Bglu])
                        P.op("scalar", lambda e: e.activation(sgm[:], glu[:], AF.Sigmoid, scale=1.702), reads=[Bglu], writes=[Bsgm])
                        P.op("vector", lambda e: e.tensor_scalar(lin[:], pll[:, 0:QT], bgu[:, e_, fc, 1:2], -7.0, ALU.add, ALU.max),
                             reads=[Bpll, Bbgu], writes=[Blin])
                        P.op("gpsimd", lambda e: e.tensor_scalar(lin[:], lin[:], 7.0, 1.0, ALU.min, ALU.add), reads=[Blin], writes=[Blin])
                        P.op("gpsimd", lambda e: e.tensor_tensor(glu[:], glu[:], sgm[:], ALU.mult), reads=[Bglu, Bsgm], writes=[Bglu])
                        P.op("vector", lambda e: e.tensor_tensor(A[:, fc, :], glu[:], lin[:], ALU.mult), reads=[Bglu, Blin], writes=[BA])
                    for t4 in range(QT // 128):
                        tl = q * (QT // 128) + t4
                        for half in range(2):
                            pdd, Bpdd = pd[pc % 2], Bpd[pc % 2]
                            pc += 1
                            for fc in range(8):
                                P.op("tensor", lambda e: e.matmul(pdd[:], A[:, fc, t4 * 128:(t4 + 1) * 128], WD[:, fc, half * 512:(half + 1) * 512],
                                                                  start=(fc == 0), stop=(fc == 7)), reads=[BA, BWD], writes=[Bpdd])
                            asl = acc[:, tl, half * 512:(half + 1) * 512]
                            if e_ == 0:
                                P.op("vector", lambda e: e.tensor_scalar(asl, pdd[:], G[:, tl, e_:e_ + 1], None, ALU.mult),
                                     reads=[Bpdd, BG], writes=[Bacc])
                            else:
                                P.op("vector", lambda e: e.scalar_tensor_tensor(asl, pdd[:], G[:, tl, e_:e_ + 1], asl, ALU.mult, ALU.add),
                                     reads=[Bpdd, BG, Bacc], writes=[Bacc])
                wi += 1
            for tl in range(tps):
                i = tl % 2
                r0 = t0 + tl * 128
                P.load("sync", xt[i][:], X1[r0:r0 + 128, :], Bxt[i])
                X, BX = xt[i], Bxt[i]
                for half in range(2):
                    P.op("tensor", lambda e: e.matmul(px[:, half * 4:(half + 1) * 4, :].rearrange("p a b -> p (a b)"), GT[:, tl, :],
                                                      bdn[:, half * 512:(half + 1) * 512], start=True, stop=True), reads=[BGT, Bbdn], writes=[Bpx])
                P.op("vector", lambda e: e.tensor_tensor(acc[:, tl, :], acc[:, tl, :], px[:].rearrange("p a b -> p (a b)"), ALU.add),
                     reads=[Bacc, Bpx], writes=[Bacc])
                P.op("vector", lambda e: e.scalar_tensor_tensor(X[:], X[:], DN_ALPHA, acc[:, tl, :], ALU.mult, ALU.add), reads=[BX, Bacc], writes=[BX])
                layernorm(P, S, X[:], BX, gam[:], Bg, bet[:], Bb)
                P.store("sync", XOUT[r0:r0 + 128, :], X[:], BX)
```

```python
import numpy as np
import concourse.bass as bass
import concourse.mybir as mybir
from concourse.bass_utils import run_bass_kernel_spmd
from contextlib import ExitStack

F32 = mybir.dt.float32
BF16 = mybir.dt.bfloat16
U32 = mybir.dt.uint32
ALU = mybir.AluOpType
AF = mybir.ActivationFunctionType
AX = mybir.AxisListType


class Buf:
    __slots__ = ("name", "lw", "rd", "wsem", "rsem")

    def __init__(self, name):
        self.name = name
        self.lw = None
        self.rd = []
        self.wsem = None
        self.rsem = None


class Prog:
    def __init__(self, nc, stack):
        self.nc = nc
        self.gstack = stack
        self.stack = stack
        self.engs = {}
        for n in ("tensor", "vector", "scalar", "gpsimd", "sync"):
            e = getattr(nc, n)
            sem = stack.enter_context(nc.semaphore("es_" + n))
            self.engs[n] = dict(h=e, sem=sem, cnt=0, seen={}, seen_d={})
        self.sems = []
        self.free_sems = {"hw": [], "sw": []}
        self.sem_kind = {}
        self.phase_bufs = []
        self.ninstr = 0
        self.mute = False
        self.pid = 0
        import os
        self.cut = float(os.environ.get('ML_CUT', '99'))

    def sb(self, name, shape, dt):
        return self.stack.enter_context(self.nc.sbuf_tensor("%s_p%d" % (name, self.pid), list(shape), dt))

    def ps(self, name, shape, dt=F32):
        return self.stack.enter_context(self.nc.psum_tensor("%s_p%d" % (name, self.pid), list(shape), dt))

    def _getsem(self, en):
        kind = "sw" if en == "gpsimd" else "hw"
        if self.free_sems[kind]:
            return self.free_sems[kind].pop()
        h = self.gstack.enter_context(self.nc.semaphore("ds%d" % len(self.sems)))
        self.sems.append([h, 0])
        self.sem_kind[len(self.sems) - 1] = kind
        return len(self.sems) - 1

    def _need(self, en, dep):
        E = self.engs[en]
        if dep[0] == 'e':
            _, pn, cnt = dep
            if pn == en and en == "tensor":
                return
            if E["seen"].get(pn, 0) >= cnt:
                return
            E["seen"][pn] = cnt
            E["h"].wait_ge(self.engs[pn]["sem"], cnt)
        else:
            _, k, val = dep
            if E["seen_d"].get(k, 0) >= val:
                return
            E["seen_d"][k] = val
            E["h"].wait_ge(self.sems[k][0], val)

    def _deps(self, en, reads, writes):
        for b in reads:
            if b.lw is not None:
                self._need(en, b.lw)
        for b in writes:
            if b.lw is not None:
                self._need(en, b.lw)
            for r in b.rd:
                self._need(en, r)

    def mark(self, k):
        if k > self.cut:
            self.mute = True

    def op(self, en, fn, reads=(), writes=()):
        if self.mute:
            return None
        self._deps(en, reads, writes)
        E = self.engs[en]
        ins = fn(E["h"])
        E["cnt"] += 1
        ins.then_inc(E["sem"], 1)
        tag = ('e', en, E["cnt"])
        for b in reads:
            b.rd.append(tag)
        for b in writes:
            b.lw = tag
            b.rd = []
        self.ninstr += 1
        return ins

    def load(self, en, out, in_, sbuf, extra_reads=(), **kw):
        if self.mute:
            return None
        self._deps(en, extra_reads, [sbuf])
        if sbuf.wsem is None:
            sbuf.wsem = self._getsem(en)
            self.phase_bufs.append(sbuf)
        s = self.sems[sbuf.wsem]
        s[1] += 16
        ins = self.engs[en]["h"].dma_start(out=out, in_=in_, **kw)
        ins.then_inc(s[0], 16)
        sbuf.lw = ('d', sbuf.wsem, s[1])
        sbuf.rd = []
        self.ninstr += 1
        return ins

    def store(self, en, out, in_, sbuf, **kw):
        if self.mute:
            return None
        self._deps(en, [sbuf], [])
        if sbuf.rsem is None:
            sbuf.rsem = self._getsem(en)
            self.phase_bufs.append(sbuf)
        s = self.sems[sbuf.rsem]
        s[1] += 16
        ins = self.engs[en]["h"].dma_start(out=out, in_=in_, **kw)
        ins.then_inc(s[0], 16)
        sbuf.rd.append(('d', sbuf.rsem, s[1]))
        self.ninstr += 1
        return ins

    def barrier(self):
        S = self.engs["sync"]
        for n, E in self.engs.items():
            if n != "sync" and E["cnt"] > 0:
                self._need("sync", ('e', n, E["cnt"]))
        for k, (h, v) in enumerate(self.sems):
            if v > 0:
                self._need("sync", ('d', k, v))
        ins = S["h"].nop()
        S["cnt"] += 1
        ins.then_inc(S["sem"], 1)
        for n in self.engs:
            if n != "sync":
                self._need(n, ('e', "sync", S["cnt"]))
        for n, E in self.engs.items():
            for m, F in self.engs.items():
                if m != n:
                    E["seen"][m] = max(E["seen"].get(m, 0), F["cnt"])
            for k, (h, v) in enumerate(self.sems):
                E["seen_d"][k] = max(E["seen_d"].get(k, 0), v)
        for b in self.phase_bufs:
            for k in (b.wsem, b.rsem):
                if k is not None:
                    self.free_sems[self.sem_kind[k]].append(k)
            b.wsem = b.rsem = None
        self.phase_bufs = []

    class _Phase:
        def __init__(self, P):
            self.P = P
        def __enter__(self):
            self.es = ExitStack()
            self.es.__enter__()
            self.P.stack = self.es
            self.P.pid += 1
            return self
        def __exit__(self, *a):
            self.P.barrier()
            self.P.stack = self.P.gstack
            return self.es.__exit__(*a)

    def phase(self):
        return Prog._Phase(self)


D = 1024
DIN = 6920
QK0, V0, OG0, IG0, FG0, RW0, CAQ0, GATE0 = 0, 512, 1024, 1536, 1540, 1544, 3336, 3848
NSEQ = 2
MEM = 256
NE = 32
DN_ALPHA = 4.0 ** 0.25
LN_EPS = 1e-5


def bcast_load(P, name, vec_ap, n, eng="sync"):
    t = P.sb(name, [128, n], F32)
    B = Buf(name)
    P.load(eng, t[:], vec_ap.partition_broadcast(128), B)
    return t, B


class LNScratch:
    def __init__(self, P, tag):
        self.st6 = P.sb("ln_st6" + tag, [128, 2, 6], F32)
        self.mv = P.sb("ln_mv" + tag, [128, 2], F32)
        self.rstd = P.sb("ln_rstd" + tag, [128, 1], F32)
        self.B = Buf("ln_scr" + tag)


def layernorm(P, S, xt, Bx, gam, Bg, bet, Bb, eps=LN_EPS):
    for c in range(2):
        P.op("vector", lambda e: e.bn_stats(S.st6[:, c, :], xt[:, c * 512:(c + 1) * 512]), reads=[Bx], writes=[S.B])
    P.op("vector", lambda e: e.bn_aggr(S.mv[:], S.st6[:].rearrange("p a b -> p (a b)")), reads=[S.B], writes=[S.B])
    P.op("scalar", lambda e: e.activation(S.rstd[:], S.mv[:, 1:2], AF.Ln, bias=eps, scale=1.0), reads=[S.B], writes=[S.B])
    P.op("scalar", lambda e: e.activation(S.rstd[:], S.rstd[:], AF.Exp, scale=-0.5), reads=[S.B], writes=[S.B])
    P.op("vector", lambda e: e.tensor_scalar(xt, xt, S.mv[:, 0:1], S.rstd[:], ALU.subtract, ALU.mult), reads=[Bx, S.B], writes=[Bx])
    P.op("vector", lambda e: e.tensor_tensor(xt, xt, gam, ALU.mult), reads=[Bx, Bg], writes=[Bx])
    P.op("vector", lambda e: e.tensor_tensor(xt, xt, bet, ALU.add), reads=[Bx, Bb], writes=[Bx])


def make_ident(P, dt, name):
    t = P.sb(name, [128, 128], dt)
    B = Buf(name)
    P.op("gpsimd", lambda e: e.memset(t[:], 0.0), writes=[B])
    P.op("gpsimd", lambda e: e.affine_select(t[:], t[:], pattern=[[-1, 128]], compare_op=ALU.not_equal,
                                             fill=1.0, base=0, channel_multiplier=1), reads=[B], writes=[B])
    return t, B


def phase_proj(P, l, NT, xin, XRES, U, w):
    nt = NT // 128
    with P.phase():
        wb = P.sb("pa_wb", [128, 8, DIN], BF16)
        Bwb = Buf("pa_wb")
        for k in range(8):
            for c4 in range(4):
                P.load("gpsimd", wb[:, k, c4 * 1730:(c4 + 1) * 1730],
                       w["w_in"][l, k * 128:(k + 1) * 128, c4 * 1730:(c4 + 1) * 1730], Bwb)
        identb, Bidb = make_ident(P, BF16, "pa_identb")
        if l == 0:
            gam, Bg = bcast_load(P, "pa_g", w["ln_in_g"], D)
            bet, Bb = bcast_load(P, "pa_b", w["ln_in_b"], D)
        S = LNScratch(P, "pa")
        xt = [P.sb("pa_xt%d" % i, [128, D], F32) for i in range(2)]
        Bxt = [Buf("pa_xt%d" % i) for i in range(2)]
        xb = P.sb("pa_xb", [128, D], BF16)
        Bxb = Buf("pa_xb")
        xT = P.sb("pa_xT", [128, 8, 128], BF16)
        BxT = Buf("pa_xT")
        pt = P.ps("pa_pt", [128, 8, 128], BF16)
        Bpt = Buf("pa_pt")
        NPY = 3
        py = [P.ps("pa_py%d" % i, [128, 512], F32) for i in range(NPY)]
        Bpy = [Buf("pa_py%d" % i) for i in range(NPY)]
        NU = 4
        us = [P.sb("pa_us%d" % i, [128, 512], F32) for i in range(NU)]
        Bus = [Buf("pa_us%d" % i) for i in range(NU)]
        P.load("sync", xt[0][:], xin[0:128, :], Bxt[0])
        cc = 0
        for i in range(nt):
            X, BX = xt[i % 2], Bxt[i % 2]
            if i + 1 < nt:
                P.load("sync", xt[(i + 1) % 2][:], xin[(i + 1) * 128:(i + 2) * 128, :], Bxt[(i + 1) % 2])
            if l == 0:
                layernorm(P, S, X[:], BX, gam[:], Bg, bet[:], Bb)
                P.store("sync", XRES[i * 128:(i + 1) * 128, :], X[:], BX)
            P.op("scalar", lambda e: e.activation(xb[:], X[:], AF.Copy), reads=[BX], writes=[Bxb])
            for k in range(8):
                P.op("tensor", lambda e: e.transpose(pt[:, k, :], xb[:, k * 128:(k + 1) * 128], identb[:]),
                     reads=[Bxb, Bidb], writes=[Bpt])
            P.op("vector", lambda e: e.tensor_copy(xT[:], pt[:]), reads=[Bpt], writes=[BxT])
            for c in range(14):
                c0 = c * 512
                cw = min(512, DIN - c0)
                pp, Bp = py[cc % NPY], Bpy[cc % NPY]
                uu, Bu = us[cc % NU], Bus[cc % NU]
                for k in range(8):
                    P.op("tensor", lambda e: e.matmul(pp[:, :cw], xT[:, k, :], wb[:, k, c0:c0 + cw],
                                                      start=(k == 0), stop=(k == 7)),
                         reads=[BxT, Bwb], writes=[Bp])
                if cc % 2 == 0:
                    P.op("scalar", lambda e: e.activation(uu[:, :cw], pp[:, :cw], AF.Copy), reads=[Bp], writes=[Bu])
                else:
                    P.op("vector", lambda e: e.tensor_copy(uu[:, :cw], pp[:, :cw]), reads=[Bp], writes=[Bu])
                P.store("sync", U[i * 128:(i + 1) * 128, c0:c0 + cw], uu[:, :cw], Bu)
                cc += 1


def dram_in(nc, name, shape):
    return nc.dram_tensor(name, list(shape), F32, kind="ExternalInput").ap()


W_SHAPES = dict(
    ln_in_g=(D,), ln_in_b=(D,), mem_ln_g=(D,), mem_ln_b=(D,), w_in=(2, D, DIN),
    ml_conv_w=(2, 4, 512), ml_conv_b=(2, 512), ml_ig_b=(2, 4), ml_fg_b=(2, 4), ml_norm_g=(2, 512),
    rw_mu=(2, 1792), rw_w0=(2, 512), rw_w_up=(2, 64, 512), rw_a0=(2, 512), rw_a_up=(2, 64, 512),
    rw_g_up=(2, 128, 512), rw_kk=(2, 512), rw_ka=(2, 512), rw_rk=(2, 8, 64), rw_ln_g=(2, 512), rw_ln_b=(2, 512),
    ca_w_kv=(2, D, 1024), gate_b=(2, 3 * D), w_br_ml=(2, 512, D), w_br_rw=(2, 512, D), w_br_ca=(2, 512, D),
    w_o=(2, D, D), ln1_g=(2, D), ln1_b=(2, D), router_w=(2, D, NE), router_b=(2, NE),
    w_gu=(2, NE, D, 2 * D), b_gu=(2, NE, 2 * D), w_dn=(2, NE, D, D), b_dn=(2, NE, D), ln2_g=(2, D), ln2_b=(2, D),
)


def build(T=4096, nlayers=2, phases=None, dbg=()):
    NT = NSEQ * T
    nc = bass.Bass("TRN2", target_bir_lowering=False)
    x = dram_in(nc, "x", (NT, D))
    mem = dram_in(nc, "mem", (NSEQ * MEM, D))
    w = {k: dram_in(nc, k, s) for k, s in W_SHAPES.items()}
    out = nc.dram_tensor("out", [NT, D], F32, kind="ExternalOutput").ap()

    def scratch(name, shape, dt=F32):
        kind = "ExternalOutput" if name in dbg else "Internal"
        return nc.dram_tensor(name, list(shape), dt, kind=kind).ap()

    XRES = scratch("XRES", (NT, D))
    U = scratch("U", (NT, DIN))
    HCAT = scratch("HCAT", (NT, 1536))
    RWOP = scratch("RWOP", (NT, 6, 512))
    RWOPB = scratch("RWOPB", (NT, 5, 3, 512), BF16)
    RWG = scratch("RWG", (NT, 512))
    RWB = scratch("RWB", (NT, 8))
    RWO = scratch("RWO", (NT, 512))
    X1 = scratch("X1", (NT, D))
    XRES2 = scratch("XRES2", (NT, D))
    WGUB = scratch("WGUB", (NE, D, 2 * D), BF16)
    WDNB = scratch("WDNB", (NE, D, D), BF16)
    with ExitStack() as st:
        P = Prog(nc, st)
        on = lambda n: phases is None or n in phases
        for l in range(nlayers):
            xin = x if l == 0 else XRES2
            xres = XRES if l == 0 else XRES2
            xout = out if l == nlayers - 1 else XRES2
            if on("proj"):
                phase_proj(P, l, NT, xin, XRES, U, w)
            if on("mlstm"):
                phase_mlstm(P, l, T, U, HCAT, w)
            if on("ca"):
                phase_ca(P, l, T, U, HCAT, mem, w)
            if on("rwpre"):
                phase_rw_pre(P, l, T, U, RWOP, RWOPB, RWG, RWB, w)
            if on("rwscan"):
                phase_rw_scan(P, l, T, RWOP, RWOPB, RWO,
                              side_factory=(lambda l=l: wcast_pieces(P, l, WGUB, WDNB, w, engs=("scalar",))) if on("moe") else None)
            if on("rwpost"):
                phase_rw_post(P, l, T, RWOP, RWG, RWB, RWO, HCAT, w)
            if on("merge"):
                phase_merge(P, l, NT, U, HCAT, xres, X1, w)
            if on("moe"):
                if not on("rwscan"):
                    phase_wcast(P, l, WGUB, WDNB, w)
                phase_moe(P, l, NT, X1, xout, WGUB, WDNB, w)
        P.barrier()
    print("instructions:", P.ninstr, "dma sems:", len(P.sems))
    return nc


_NC_CACHE = {}


def kernel(**inputs):
    ncores = 8
    T = 4096
    if T not in _NC_CACHE:
        _NC_CACHE[T] = build(T=T)
    nc = _NC_CACHE[T]
    wmap = {k: np.ascontiguousarray(inputs[k], dtype=np.float32) for k in W_SHAPES}
    x = np.asarray(inputs["x"], dtype=np.float32)
    mem = np.asarray(inputs["mem"], dtype=np.float32)
    in_maps = []
    for c in range(ncores):
        m = dict(wmap)
        m["x"] = np.ascontiguousarray(x[NSEQ * c:NSEQ * (c + 1)].reshape(NSEQ * T, D))
        m["mem"] = np.ascontiguousarray(mem[NSEQ * c:NSEQ * (c + 1)].reshape(NSEQ * MEM, D))
        in_maps.append(m)
    res = run_bass_kernel_spmd(nc, in_maps, core_ids=list(range(ncores)))
    outs = [np.asarray(r["out"]).reshape(NSEQ, T, D) for r in res.results]
    return np.concatenate(outs, axis=0).astype(np.float32)


def make_mask_le(P, name):
    t = P.sb(name, [128, 128], F32)
    B = Buf(name)
    P.op("gpsimd", lambda e: e.memset(t[:], 1.0), writes=[B])
    P.op("gpsimd", lambda e: e.affine_select(t[:], t[:], pattern=[[1, 128]], compare_op=ALU.is_ge,
                                             fill=0.0, base=0, channel_multiplier=-1), reads=[B], writes=[B])
    return t, B


def phase_mlstm(P, l, T, U, HCAT, w):
    nchunk = T // 128
    with P.phase():
        identf, Bidf = make_ident(P, F32, "ml_identf")
        identb, Bidb = make_ident(P, BF16, "ml_identb")
        mask, Bmask = make_mask_le(P, "ml_mask")
        sel = P.sb("ml_sel", [4, 4, 128], F32)
        Bsel = Buf("ml_sel")
        P.op("gpsimd", lambda e: e.memset(sel[:], 0.0), writes=[Bsel])
        P.op("gpsimd", lambda e: e.affine_select(sel[:], sel[:], pattern=[[-1, 4], [0, 128]], compare_op=ALU.not_equal,
                                                 fill=1.0, base=0, channel_multiplier=1), reads=[Bsel], writes=[Bsel])
        cw = P.sb("ml_cw", [64, 8, 4], F32)
        Bcw = Buf("ml_cw")
        for jj in range(4):
            P.load("sync", cw[:, :, jj], w["ml_conv_w"][l, jj].rearrange("(blk p) -> p blk", p=64), Bcw,
                   allow_slow_non_contiguous=True)
        cb = P.sb("ml_cb", [64, 8], F32)
        Bcb = Buf("ml_cb")
        P.load("sync", cb[:], w["ml_conv_b"][l].rearrange("(blk p) -> p blk", p=64), Bcb,
               allow_slow_non_contiguous=True)
        gb8 = P.sb("ml_gb8", [128, 8], F32)
        Bgb8 = Buf("ml_gb8")
        P.load("sync", gb8[:, 0:4], w["ml_ig_b"][l].partition_broadcast(128), Bgb8)
        P.load("sync", gb8[:, 4:8], w["ml_fg_b"][l].partition_broadcast(128), Bgb8)
        normg, Bng = bcast_load(P, "ml_normg", w["ml_norm_g"][l], 512)
        ones4 = P.sb("ml_ones4", [4, 128], F32)
        Bones4 = Buf("ml_ones4")
        P.op("vector", lambda e: e.memset(ones4[:], 1.0), writes=[Bones4])
        zeros4 = P.sb("ml_zeros4", [4, 128], F32)
        Bz4 = Buf("ml_zeros4")
        P.op("vector", lambda e: e.memset(zeros4[:], 0.0), writes=[Bz4])
        onesb = P.sb("ml_onesb", [128, 2], BF16)
        Bonesb = Buf("ml_onesb")
        P.op("vector", lambda e: e.memset(onesb[:], 1.0), writes=[Bonesb])

        C32 = P.sb("ml_C32", [64, 4, 132], F32)
        BC32 = Buf("ml_C32")
        Cb = P.sb("ml_Cb", [64, 4, 132], BF16)
        BCb = Buf("ml_Cb")
        uqk = [P.sb("ml_uqk%d" % i, [128, 512], F32) for i in range(2)]
        Buqk = [Buf("ml_uqk%d" % i) for i in range(2)]
        vt = [P.sb("ml_vt%d" % i, [128, 512], F32) for i in range(2)]
        Bvt = [Buf("ml_vt%d" % i) for i in range(2)]
        og = [P.sb("ml_og%d" % i, [128, 512], F32) for i in range(2)]
        Bog = [Buf("ml_og%d" % i) for i in range(2)]
        gt = [P.sb("ml_gt%d" % i, [128, 8], F32) for i in range(2)]
        Bgt = [Buf("ml_gt%d" % i) for i in range(2)]
        uT = [P.sb("ml_uT%d" % i, [64, 8, 131], F32) for i in range(2)]
        BuT = [Buf("ml_uT%d" % i) for i in range(2)]
        Fg = [P.sb("ml_F%d" % i, [4, 5, 128], F32) for i in range(2)]
        BF = [Buf("ml_F%d" % i) for i in range(2)]
        acc = P.sb("ml_acc", [64, 8, 128], F32)
        Bacc = Buf("ml_acc")
        qkb = P.sb("ml_qkb", [64, 8, 128], BF16)
        Bqkb = Buf("ml_qkb")
        ktok = P.sb("ml_ktok", [128, 4, 64], BF16)
        Bktok = Buf("ml_ktok")
        kw = P.sb("ml_kw", [128, 4, 64], BF16)
        Bkw = Buf("ml_kw")
        z8 = P.sb("ml_z8", [128, 8], F32)
        Bz8 = Buf("ml_z8")
        T6 = P.sb("ml_T6", [4, 6, 128], F32)
        BT6 = Buf("ml_T6")
        d41 = P.sb("ml_d41", [4, 1], F32)
        Bd41 = Buf("ml_d41")
        tokA = P.sb("ml_tokA", [128, 4], F32)
        BtokA = Buf("ml_tokA")
        tokE = P.sb("ml_tokE", [128, 4, 4], F32)
        BtokE = Buf("ml_tokE")
        Dm = P.sb("ml_D", [128, 4, 128], F32)
        BD = Buf("ml_D")
        Wt = P.sb("ml_Wt", [128, 4, 128], BF16)
        BWt = Buf("ml_Wt")
        vext = P.sb("ml_vext", [128, 4, 132], BF16)
        Bvext = Buf("ml_vext")
        P.op("vector", lambda e: e.memset(vext[:], 1.0), writes=[Bvext])
        numI = P.sb("ml_numI", [128, 4, 128], F32)
        BnumI = Buf("ml_numI")
        tot = P.sb("ml_tot", [128, 4, 128], F32)
        Btot = Buf("ml_tot")
        sm = P.sb("ml_sm", [128, 8, 4], F32)
        Bsm = Buf("ml_sm")
        st6 = P.sb("ml_st6", [128, 4, 6], F32)
        mv = P.sb("ml_mv", [128, 4, 2], F32)
        Bst = Buf("ml_st")
        sg = P.sb("ml_sg", [128, 512], F32)
        Bsg = Buf("ml_sg")
        hml = [P.sb("ml_hml%d" % i, [128, 512], F32) for i in range(2)]
        Bhml = [Buf("ml_hml%d" % i) for i in range(2)]
        b0 = P.ps("ml_b0", [128, 512], F32); Bb0 = Buf("ml_b0")
        b1 = P.ps("ml_b1", [128, 512], F32); Bb1 = Buf("ml_b1")
        pnG = P.ps("ml_pnG", [128, 4, 128], F32); BpnG = Buf("ml_pnG")
        pS = P.ps("ml_pS", [128, 4, 128], F32); BpS = Buf("ml_pS")
        pI = P.ps("ml_pI", [128, 4, 128], F32); BpI = Buf("ml_pI")
        pC = P.ps("ml_pC", [128, 4, 128], F32); BpC = Buf("ml_pC")
        pK = P.ps("ml_pK", [128, 4, 64], BF16); BpK = Buf("ml_pK")
        b7 = P.ps("ml_b7", [128, 512], F32); Bb7 = Buf("ml_b7")
        ptq = [b0[0:64, :].rearrange("p (a b) -> p a b", a=4), b7[0:64, :].rearrange("p (a b) -> p a b", a=4)]
        Bptq = [Bb0, Bb7]
        pU = [b0[0:64, 0:264].rearrange("p (a b) -> p a b", a=2), b7[0:64, 0:264].rearrange("p (a b) -> p a b", a=2)]
        pg = b1[0:4, 0:256].rearrange("p (a b) -> p a b", a=2)
        pT = b1[:, 256:276].rearrange("p (a b) -> p a b", a=5)
        pD = b1[:, 280:288]

        def loads(b, c, i):
            r0 = b * T + c * 128
            P.load("sync", uqk[i][:], U[r0:r0 + 128, QK0:QK0 + 512], Buqk[i])
            P.load("sync", vt[i][:], U[r0:r0 + 128, V0:V0 + 512], Bvt[i])
            P.load("sync", og[i][:], U[r0:r0 + 128, OG0:OG0 + 512], Bog[i])
            P.load("sync", gt[i][:], U[r0:r0 + 128, IG0:IG0 + 8], Bgt[i])

        it = 0
        loads(0, 0, 0)
        for b in range(NSEQ):
            P.op("gpsimd", lambda e: e.memset(C32[:], 0.0), writes=[BC32])
            P.op("gpsimd", lambda e: e.memset(Cb[:], 0.0), writes=[BCb])
            for c in range(nchunk):
                i = it % 2
                j = 1 - i
                nb, ncn = (b, c + 1) if c + 1 < nchunk else (b + 1, 0)
                if nb < NSEQ:
                    loads(nb, ncn, j)
                first = (c == 0)
                for blk in range(8):
                    P.op("tensor", lambda e: e.transpose(ptq[blk // 4][:, blk % 4, :], uqk[i][:, blk * 64:(blk + 1) * 64], identf[:]),
                         reads=[Buqk[i], Bidf], writes=[Bptq[blk // 4]])
                if first:
                    P.op("gpsimd", lambda e: e.memset(uT[i][:, :, 0:3], 0.0), writes=[BuT[i]])
                else:
                    P.op("gpsimd", lambda e: e.tensor_copy(uT[i][:, :, 0:3], uT[j][:, :, 128:131]), reads=[BuT[j]], writes=[BuT[i]])
                P.op("vector", lambda e: e.tensor_copy(uT[i][:, 0:4, 3:131], ptq[0]), reads=[Bb0], writes=[BuT[i]])
                P.op("vector", lambda e: e.tensor_copy(uT[i][:, 4:8, 3:131], ptq[1]), reads=[Bb7], writes=[BuT[i]])
                for blk in range(8):
                    P.op("scalar", lambda e: e.activation(acc[:, blk, :], uT[i][:, blk, 3:131], AF.Identity,
                                                          bias=cb[:, blk:blk + 1], scale=cw[:, blk, 3:4]),
                         reads=[BuT[i], Bcb, Bcw], writes=[Bacc])
                    for dd in range(1, 4):
                        P.op("vector", lambda e: e.scalar_tensor_tensor(acc[:, blk, :], uT[i][:, blk, 3 - dd:131 - dd],
                                                                        cw[:, blk, 3 - dd:4 - dd], acc[:, blk, :], ALU.mult, ALU.add),
                             reads=[BuT[i], Bcw, Bacc], writes=[Bacc])
                P.op("scalar", lambda e: e.activation(acc[:], acc[:], AF.Silu), reads=[Bacc], writes=[Bacc])
                P.op("vector", lambda e: e.tensor_scalar(qkb[:, 0:4, :], acc[:, 0:4, :], 0.125, None, ALU.mult), reads=[Bacc], writes=[Bqkb])
                P.op("gpsimd", lambda e: e.tensor_copy(qkb[:, 4:8, :], acc[:, 4:8, :]), reads=[Bacc], writes=[Bqkb])
                P.mark(1)
                for blk in range(4):
                    P.op("tensor", lambda e: e.transpose(pK[:, blk, :], qkb[:, 4 + blk, :], identb[0:64, 0:64]), reads=[Bqkb, Bidb], writes=[BpK])
                P.op("scalar", lambda e: e.activation(ktok[:], pK[:], AF.Copy), reads=[BpK], writes=[Bktok])
                P.mark(2)
                P.op("vector", lambda e: e.tensor_tensor(z8[:], gt[i][:], gb8[:], ALU.add), reads=[Bgt[i], Bgb8], writes=[Bz8])
                P.op("scalar", lambda e: e.activation(z8[:, 4:8], z8[:, 4:8], AF.Exp, scale=-1.0), reads=[Bz8], writes=[Bz8])
                P.op("scalar", lambda e: e.activation(z8[:, 4:8], z8[:, 4:8], AF.Ln, bias=1.0, scale=1.0), reads=[Bz8], writes=[Bz8])
                P.op("tensor", lambda e: e.transpose(pg[:, 0, :], z8[:, 0:4], identf[:]), reads=[Bz8, Bidf], writes=[Bb1])
                P.op("tensor", lambda e: e.transpose(pg[:, 1, :], z8[:, 4:8], identf[:]), reads=[Bz8, Bidf], writes=[Bb1])
                F = Fg[i]
                Fp = Fg[j]
                P.op("vector", lambda e: e.tensor_copy(F[:, 0, :], pg[:, 0, :]), reads=[Bb1], writes=[BF[i]])
                P.op("vector", lambda e: e.tensor_scalar(F[:, 1, :], pg[:, 1, :], -1.0, None, ALU.mult), reads=[Bb1], writes=[BF[i]])
                P.op("vector", lambda e: e.tensor_tensor_scan(F[:, 2, :], ones4[:], F[:, 1, :],
                                                              0.0 if first else Fp[:, 2, 127:128], ALU.mult, ALU.add),
                     reads=[BF[i], BF[j], Bones4], writes=[BF[i]])
                P.op("vector", lambda e: e.tensor_tensor(F[:, 3, :], F[:, 0, :], F[:, 2, :], ALU.subtract), reads=[BF[i]], writes=[BF[i]])
                P.op("vector", lambda e: e.tensor_tensor_scan(F[:, 4, :], F[:, 3, :], F[:, 3, :],
                                                              0.0 if first else Fp[:, 4, 127:128], ALU.max, ALU.max),
                     reads=[BF[i], BF[j]], writes=[BF[i]])
                gprev = zeros4[:, 0:1] if first else Fp[:, 4, 127:128]
                P.op("vector", lambda e: e.tensor_copy(T6[:, 0, :], F[:, 3, :]), reads=[BF[i]], writes=[BT6])
                P.op("vector", lambda e: e.scalar_tensor_tensor(T6[:, 1, :], F[:, 2, :], -1.0, F[:, 4, :], ALU.mult, ALU.subtract),
                     reads=[BF[i]], writes=[BT6])
                P.op("vector", lambda e: e.tensor_scalar(T6[:, 2, :], F[:, 4, :], -1.0, gprev, ALU.mult, ALU.add),
                     reads=[BF[i], BF[j], Bz4], writes=[BT6])
                P.op("vector", lambda e: e.tensor_scalar(T6[:, 3, :], F[:, 3, :], F[:, 4, 127:128], None, ALU.subtract),
                     reads=[BF[i]], writes=[BT6])
                P.op("vector", lambda e: e.tensor_tensor(d41[:], gprev, F[:, 4, 127:128], ALU.subtract),
                     reads=[BF[i], BF[j], Bz4], writes=[Bd41])
                P.op("vector", lambda e: e.tensor_scalar(T6[:, 4, :], zeros4[:], d41[:, 0:1], None, ALU.add),
                     reads=[Bz4, Bd41], writes=[BT6])
                P.op("vector", lambda e: e.tensor_scalar(T6[:, 5, :], F[:, 4, :], -1.0, None, ALU.mult), reads=[BF[i]], writes=[BT6])
                for r in range(5):
                    P.op("tensor", lambda e: e.transpose(pT[:, r, :], T6[:, r, :], identf[0:4, 0:4]), reads=[BT6, Bidf], writes=[Bb1])
                P.op("vector", lambda e: e.tensor_copy(tokA[:], pT[:, 0, :]), reads=[Bb1], writes=[BtokA])
                P.op("scalar", lambda e: e.activation(tokE[:], pT[:, 1:5, :], AF.Exp), reads=[Bb1], writes=[BtokE])
                P.mark(3)
                for h in range(4):
                    P.op("tensor", lambda e: e.matmul(pnG[:, h, :], sel[:, h, :], T6[:, 5, :], start=True, stop=True),
                         reads=[Bsel, BT6], writes=[BpnG])
                for h in range(4):
                    P.op("tensor", lambda e: e.matmul(pS[:, h, :], qkb[:, 4 + h, :], qkb[:, h, :],
                                                      start=True, stop=True), reads=[Bqkb], writes=[BpS])
                P.mark(3.1)
                for h in range(4):
                    P.op("vector", lambda e: e.tensor_scalar(Dm[:, h, :], pnG[:, h, :], tokA[:, h:h + 1], 0.0, ALU.add, ALU.min),
                         reads=[BpnG, BtokA], writes=[BD])
                P.mark(3.2)
                P.op("scalar", lambda e: e.activation(Dm[:], Dm[:], AF.Exp), reads=[BD], writes=[BD])
                P.mark(3.3)
                P.op("gpsimd", lambda e: e.tensor_tensor(Dm[:], Dm[:], mask[:].unsqueeze(1).to_broadcast([128, 4, 128]), ALU.mult),
                     reads=[BD, Bmask], writes=[BD])
                P.mark(3.4)
                P.op("vector", lambda e: e.tensor_tensor(Wt[:], Dm[:], pS[:], ALU.mult), reads=[BD, BpS], writes=[BWt])
                P.mark(3.5)
                P.op("gpsimd", lambda e: e.tensor_copy(vext[:, :, 0:128], vt[i][:].rearrange("p (h v) -> p h v", h=4)),
                     reads=[Bvt[i]], writes=[Bvext])
                P.mark(4)
                for h in range(4):
                    P.op("tensor", lambda e: e.matmul(pI[:, h, :], Wt[:, h, :], vext[:, h, 0:128], start=True, stop=True),
                         reads=[BWt, Bvext], writes=[BpI])
                    P.op("tensor", lambda e: e.matmul(pC[:, h, :], qkb[:, h, :], Cb[:, h, 0:128],
                                                      start=True, stop=True), reads=[Bqkb, BCb], writes=[BpC])
                    P.op("tensor", lambda e: e.matmul(pD[:, h:h + 1], Wt[:, h, :], onesb[:, 0:1], start=True, stop=True),
                         reads=[BWt, Bonesb], writes=[Bb1])
                    P.op("tensor", lambda e: e.matmul(pD[:, 4 + h:5 + h], qkb[:, h, :], Cb[:, h, 128:129],
                                                      start=True, stop=True), reads=[Bqkb, BCb], writes=[Bb1])
                P.op("scalar", lambda e: e.activation(numI[:], pI[:], AF.Copy), reads=[BpI], writes=[BnumI])
                for h in range(4):
                    P.op("vector", lambda e: e.scalar_tensor_tensor(tot[:, h, :], pC[:, h, :], tokE[:, 1, h:h + 1], numI[:, h, :],
                                                                    ALU.mult, ALU.add), reads=[BpC, BtokE, BnumI], writes=[Btot])
                P.op("vector", lambda e: e.tensor_tensor(sm[:, 0, :], pD[:, 4:8], tokE[:, 1, :], ALU.mult), reads=[Bb1, BtokE], writes=[Bsm])
                P.op("vector", lambda e: e.tensor_tensor(sm[:, 1, :], pD[:, 0:4], sm[:, 0, :], ALU.add), reads=[Bb1, Bsm], writes=[Bsm])
                P.op("vector", lambda e: e.tensor_scalar(sm[:, 2, :], sm[:, 1, :], -1.0, None, ALU.mult), reads=[Bsm], writes=[Bsm])
                P.op("vector", lambda e: e.tensor_tensor(sm[:, 2, :], sm[:, 2, :], sm[:, 1, :], ALU.max), reads=[Bsm], writes=[Bsm])
                P.op("vector", lambda e: e.tensor_tensor(sm[:, 3, :], sm[:, 2, :], tokE[:, 0, :], ALU.max), reads=[Bsm, BtokE], writes=[Bsm])
                P.op("vector", lambda e: e.reciprocal(sm[:, 4, :], sm[:, 3, :]), reads=[Bsm], writes=[Bsm])
                P.op("vector", lambda e: e.tensor_tensor(tot[:], tot[:], sm[:, 4, :].unsqueeze(2).to_broadcast([128, 4, 128]), ALU.mult),
                     reads=[Btot, Bsm], writes=[Btot])
                P.mark(5)
                for h in range(4):
                    P.op("vector", lambda e: e.bn_stats(st6[:, h, :], tot[:, h, :]), reads=[Btot], writes=[Bst])
                for h in range(4):
                    P.op("vector", lambda e: e.bn_aggr(mv[:, h, :], st6[:, h, :]), reads=[Bst], writes=[Bst])
                P.op("scalar", lambda e: e.activation(sm[:, 5, :], mv[:, :, 1], AF.Ln, bias=LN_EPS, scale=1.0), reads=[Bst], writes=[Bsm])
                P.op("scalar", lambda e: e.activation(sm[:, 5, :], sm[:, 5, :], AF.Exp, scale=-0.5), reads=[Bsm], writes=[Bsm])
                for h in range(4):
                    P.op("vector", lambda e: e.tensor_scalar(tot[:, h, :], tot[:, h, :], mv[:, h, 0:1], sm[:, 5, h:h + 1],
                                                             ALU.subtract, ALU.mult), reads=[Btot, Bst, Bsm], writes=[Btot])
                H, BH = hml[i], Bhml[i]
                P.op("scalar", lambda e: e.activation(sg[:], og[i][:], AF.Sigmoid), reads=[Bog[i]], writes=[Bsg])
                P.op("vector", lambda e: e.tensor_tensor(H[:], tot[:].rearrange("p h v -> p (h v)"), normg[:], ALU.mult),
                     reads=[Btot, Bng], writes=[BH])
                P.op("vector", lambda e: e.tensor_tensor(H[:], H[:], sg[:], ALU.mult), reads=[BH, Bsg], writes=[BH])
                r0 = b * T + c * 128
                P.store("sync", HCAT[r0:r0 + 128, 0:512], H[:], BH)
                P.mark(6)
                for h in range(4):
                    P.op("vector", lambda e: e.tensor_scalar(kw[:, h, :], ktok[:, h, :],
                                                             tokE[:, 2, h:h + 1], None, ALU.mult), reads=[Bktok, BtokE], writes=[Bkw])
                for h in range(4):
                    P.op("tensor", lambda e: e.matmul(pU[h // 2][:, h % 2, 0:129], kw[:, h, :], vext[:, h, 0:129], start=True, stop=True),
                         reads=[Bkw, Bvext], writes=[Bptq[h // 2]])
                for h in range(4):
                    P.op("vector", lambda e: e.scalar_tensor_tensor(C32[:, h, 0:129], C32[:, h, 0:129],
                                                                    tokE[0:64, 3, h:h + 1], pU[h // 2][:, h % 2, 0:129],
                                                                    ALU.mult, ALU.add), reads=[BC32, BtokE, Bptq[h // 2]], writes=[BC32])
                P.op("scalar", lambda e: e.activation(Cb[:], C32[:], AF.Copy), reads=[BC32], writes=[BCb])
                P.mute = False
                it += 1


def phase_ca(P, l, T, U, HCAT, mem, w):
    ntile = T // 128
    with P.phase():
        identb, Bidb = make_ident(P, BF16, "ca_identb")
        wkv = P.sb("ca_wkv", [128, 8, 1024], BF16)
        Bwkv = Buf("ca_wkv")
        for k in range(8):
            P.load("gpsimd", wkv[:, k, :], w["ca_w_kv"][l, k * 128:(k + 1) * 128, :], Bwkv)
        gam, Bg = bcast_load(P, "ca_g", w["mem_ln_g"], D)
        bet, Bb = bcast_load(P, "ca_b", w["mem_ln_b"], D)
        S = LNScratch(P, "ca")
        mt_ = P.sb("ca_mt", [128, D], F32); Bmt = Buf("ca_mt")
        mb = P.sb("ca_mb", [128, D], BF16); Bmb = Buf("ca_mb")
        memT = P.sb("ca_memT", [128, 8, 256], BF16); BmemT = Buf("ca_memT")
        kT = P.sb("ca_kT", [128, 4, 256], BF16); BkT = Buf("ca_kT")
        Vv = P.sb("ca_V", [128, 2, 512], BF16); BV = Buf("ca_V")
        qt = [P.sb("ca_qt%d" % i, [128, 512], F32) for i in range(2)]
        Bqt = [Buf("ca_qt%d" % i) for i in range(2)]
        qb = P.sb("ca_qb", [128, 512], BF16); Bqb = Buf("ca_qb")
        qT = P.sb("ca_qT", [128, 4, 128], BF16); BqT = Buf("ca_qT")
        mx = P.sb("ca_mx", [128, 4], F32); Bmx = Buf("ca_mx")
        rs = P.sb("ca_rs", [128, 4], F32); Brs = Buf("ca_rs")
        Pm = P.sb("ca_P", [128, 4, 256], BF16); BPm = Buf("ca_P")
        PT = P.sb("ca_PT", [128, 4, 2, 128], BF16); BPT = Buf("ca_PT")
        ho = [P.sb("ca_ho%d" % i, [128, 512], F32) for i in range(2)]
        Bho = [Buf("ca_ho%d" % i) for i in range(2)]
        pmt = P.ps("ca_pmt", [128, 8, 128], BF16); Bpmt = Buf("ca_pmt")
        pkv = P.ps("ca_pkv", [128, 512], F32); Bpkv = Buf("ca_pkv")
        pSc = P.ps("ca_pSc", [128, 4, 256], F32); BpSc = Buf("ca_pSc")
        pPT = P.ps("ca_pPT", [128, 4, 2, 128], BF16); BpPT = Buf("ca_pPT")
        pO = P.ps("ca_pO", [128, 4, 128], F32); BpO = Buf("ca_pO")
        pq = pmt[:, 0:4, :]
        it = 0
        for b in range(NSEQ):
            for mtile in range(2):
                r0 = b * MEM + mtile * 128
                P.load("sync", mt_[:], mem[r0:r0 + 128, :], Bmt)
                layernorm(P, S, mt_[:], Bmt, gam[:], Bg, bet[:], Bb)
                P.op("scalar", lambda e: e.activation(mb[:], mt_[:], AF.Copy), reads=[Bmt], writes=[Bmb])
                for k in range(8):
                    P.op("tensor", lambda e: e.transpose(pmt[:, k, :], mb[:, k * 128:(k + 1) * 128], identb[:]),
                         reads=[Bmb, Bidb], writes=[Bpmt])
                P.op("vector", lambda e: e.tensor_copy(memT[:, :, mtile * 128:(mtile + 1) * 128], pmt[:]), reads=[Bpmt], writes=[BmemT])
            for h in range(4):
                for k in range(8):
                    P.op("tensor", lambda e: e.matmul(pkv[:, 0:256], wkv[:, k, h * 128:(h + 1) * 128], memT[:, k, :],
                                                      start=(k == 0), stop=(k == 7)), reads=[Bwkv, BmemT], writes=[Bpkv])
                P.op("vector", lambda e: e.tensor_copy(kT[:, h, :], pkv[:, 0:256]), reads=[Bpkv], writes=[BkT])
            for mtile in range(2):
                for k in range(8):
                    P.op("tensor", lambda e: e.matmul(pkv[:], memT[:, k, mtile * 128:(mtile + 1) * 128], wkv[:, k, 512:1024],
                                                      start=(k == 0), stop=(k == 7)), reads=[Bwkv, BmemT], writes=[Bpkv])
                P.op("vector", lambda e: e.tensor_copy(Vv[:, mtile, :], pkv[:]), reads=[Bpkv], writes=[BV])
            P.load("sync", qt[it % 2][:], U[b * T:b * T + 128, CAQ0:CAQ0 + 512], Bqt[it % 2])
            for c in range(ntile):
                i = it % 2
                if c + 1 < ntile:
                    r1 = b * T + (c + 1) * 128
                    P.load("sync", qt[1 - i][:], U[r1:r1 + 128, CAQ0:CAQ0 + 512], Bqt[1 - i])
                P.op("scalar", lambda e: e.activation(qb[:], qt[i][:], AF.Copy, scale=128.0 ** -0.5), reads=[Bqt[i]], writes=[Bqb])
                for h in range(4):
                    P.op("tensor", lambda e: e.transpose(pq[:, h, :], qb[:, h * 128:(h + 1) * 128], identb[:]),
                         reads=[Bqb, Bidb], writes=[Bpmt])
                P.op("vector", lambda e: e.tensor_copy(qT[:], pq), reads=[Bpmt], writes=[BqT])
                for h in range(4):
                    P.op("tensor", lambda e: e.matmul(pSc[:, h, :], qT[:, h, :], kT[:, h, :], start=True, stop=True),
                         reads=[BqT, BkT], writes=[BpSc])
                P.op("vector", lambda e: e.tensor_reduce(mx[:], pSc[:], AX.X, ALU.max), reads=[BpSc], writes=[Bmx])
                P.op("vector", lambda e: e.tensor_scalar(mx[:], mx[:], -1.0, None, ALU.mult), reads=[Bmx], writes=[Bmx])
                for h in range(4):
                    P.op("scalar", lambda e: e.activation(Pm[:, h, :], pSc[:, h, :], AF.Exp, bias=mx[:, h:h + 1], scale=1.0,
                                                          accum_out=rs[:, h:h + 1]), reads=[BpSc, Bmx], writes=[BPm, Brs])
                for h in range(4):
                    for mtile in range(2):
                        P.op("tensor", lambda e: e.transpose(pPT[:, h, mtile, :], Pm[:, h, mtile * 128:(mtile + 1) * 128], identb[:]),
                             reads=[BPm, Bidb], writes=[BpPT])
                P.op("vector", lambda e: e.tensor_copy(PT[:], pPT[:]), reads=[BpPT], writes=[BPT])
                for h in range(4):
                    for mtile in range(2):
                        P.op("tensor", lambda e: e.matmul(pO[:, h, :], PT[:, h, mtile, :], Vv[:, mtile, h * 128:(h + 1) * 128],
                                                          start=(mtile == 0), stop=(mtile == 1)), reads=[BPT, BV], writes=[BpO])
                P.op("vector", lambda e: e.reciprocal(rs[:], rs[:]), reads=[Brs], writes=[Brs])
                H, BH = ho[i], Bho[i]
                P.op("vector", lambda e: e.tensor_tensor(H[:].rearrange("p (h v) -> p h v", h=4), pO[:],
                                                         rs[:].unsqueeze(2).to_broadcast([128, 4, 128]), ALU.mult),
                     reads=[BpO, Brs], writes=[BH])
                r0 = b * T + c * 128
                P.store("sync", HCAT[r0:r0 + 128, 1024:1536], H[:], BH)
                it += 1


def phase_rw_pre(P, l, T, U, RWOP, RWOPB, RWG, RWB, w):
    ntile = T // 128
    with P.phase():
        identb, Bidb = make_ident(P, BF16, "rp_identb")
        mu, Bmu = bcast_load(P, "rp_mu", w["rw_mu"][l], 1792)
        w0, Bw0 = bcast_load(P, "rp_w0", w["rw_w0"][l], 512)
        a0, Ba0 = bcast_load(P, "rp_a0", w["rw_a0"][l], 512)
        kkb, Bkkb = bcast_load(P, "rp_kk", w["rw_kk"][l], 512)
        kab, Bkab = bcast_load(P, "rp_ka", w["rw_ka"][l], 512)
        rkb, Brkb = bcast_load(P, "rp_rk", w["rw_rk"][l].rearrange("h k -> (h k)"), 512)
        wup = P.sb("rp_wup", [64, 512], BF16); Bwup = Buf("rp_wup")
        aup = P.sb("rp_aup", [64, 512], BF16); Baup = Buf("rp_aup")
        gup = P.sb("rp_gup", [128, 512], BF16); Bgup = Buf("rp_gup")
        P.load("gpsimd", wup[:], w["rw_w_up"][l], Bwup)
        P.load("gpsimd", aup[:], w["rw_a_up"][l], Baup)
        P.load("gpsimd", gup[:], w["rw_g_up"][l], Bgup)
        ucur = [P.sb("rp_ucur%d" % i, [128, 1792], F32) for i in range(2)]
        Bucur = [Buf("rp_ucur%d" % i) for i in range(2)]
        uprev = [P.sb("rp_uprev%d" % i, [128, 1792], F32) for i in range(2)]
        Buprev = [Buf("rp_uprev%d" % i) for i in range(2)]
        xs = P.sb("rp_xs", [128, 1792], F32); Bxs = Buf("rp_xs")
        lb = P.sb("rp_lb", [128, 256], BF16); Blb = Buf("rp_lb")
        lT = P.sb("rp_lT", [128, 3, 128], BF16); BlT = Buf("rp_lT")
        zt = P.sb("rp_zt", [128, 512], F32); Bzt = Buf("rp_zt")
        nz = P.sb("rp_nz", [128, 512], F32); Bnz = Buf("rp_nz")
        az = P.sb("rp_az", [128, 512], F32); Baz = Buf("rp_az")
        av = P.sb("rp_av", [128, 512], F32); Bav = Buf("rp_av")
        t1 = P.sb("rp_t1", [128, 512], F32); Bt1 = Buf("rp_t1")
        ss = P.sb("rp_ss", [128, 8], F32); Bss = Buf("rp_ss")
        ops = [P.sb("rp_ops%d" % i, [128, 6, 512], F32) for i in range(2)]
        Bops = [Buf("rp_ops%d" % i) for i in range(2)]
        gg = [P.sb("rp_g%d" % i, [128, 512], F32) for i in range(2)]
        Bgg = [Buf("rp_g%d" % i) for i in range(2)]
        bc = [P.sb("rp_bc%d" % i, [128, 8], F32) for i in range(2)]
        Bbc = [Buf("rp_bc%d" % i) for i in range(2)]
        ob = [P.sb("rp_ob%d" % i, [128, 5, 3, 512], BF16) for i in range(2)]
        Bob = [Buf("rp_ob%d" % i) for i in range(2)]
        rres = P.sb("rp_rres", [128, 5, 512], F32); Brres = Buf("rp_rres")
        plT = P.ps("rp_plT", [128, 3, 128], BF16); BplT = Buf("rp_plT")
        pw = P.ps("rp_pw", [128, 512], F32); Bpw = Buf("rp_pw")
        pa = P.ps("rp_pa", [128, 512], F32); Bpa = Buf("rp_pa")
        pg = P.ps("rp_pg", [128, 512], F32); Bpg = Buf("rp_pg")

        def loads(b, c, i):
            r0 = b * T + c * 128
            P.load("sync", ucur[i][:], U[r0:r0 + 128, RW0:RW0 + 1792], Bucur[i])
            if c == 0:
                P.op("vector", lambda e: e.memset(uprev[i][0:1, :], 0.0), writes=[Buprev[i]])
                P.load("sync", uprev[i][1:128, :], U[r0:r0 + 127, RW0:RW0 + 1792], Buprev[i])
            else:
                P.load("sync", uprev[i][:], U[r0 - 1:r0 + 127, RW0:RW0 + 1792], Buprev[i])

        tiles = [(b, c) for b in range(NSEQ) for c in range(ntile)]
        loads(0, 0, 0)
        for it, (b, c) in enumerate(tiles):
            i = it % 2
            if it + 1 < len(tiles):
                loads(tiles[it + 1][0], tiles[it + 1][1], 1 - i)
            O, BO = ops[i], Bops[i]
            P.op("vector", lambda e: e.tensor_tensor(xs[:], uprev[i][:], ucur[i][:], ALU.subtract), reads=[Buprev[i], Bucur[i]], writes=[Bxs])
            P.op("gpsimd", lambda e: e.tensor_tensor(xs[:], xs[:], mu[:], ALU.mult), reads=[Bxs, Bmu], writes=[Bxs])
            P.op("vector", lambda e: e.tensor_tensor(xs[:], xs[:], ucur[i][:], ALU.add), reads=[Bxs, Bucur[i]], writes=[Bxs])
            P.op("scalar", lambda e: e.activation(O[:, 0, :], xs[:, 0:512], AF.Copy), reads=[Bxs], writes=[BO])
            P.op("scalar", lambda e: e.activation(O[:, 5, :], xs[:, 1024:1536], AF.Copy), reads=[Bxs], writes=[BO])
            P.op("scalar", lambda e: e.activation(lb[:, 0:64], xs[:, 1536:1600], AF.Tanh), reads=[Bxs], writes=[Blb])
            P.op("scalar", lambda e: e.activation(lb[:, 128:256], xs[:, 1664:1792], AF.Sigmoid), reads=[Bxs], writes=[Blb])
            P.op("vector", lambda e: e.tensor_copy(lb[:, 64:128], xs[:, 1600:1664]), reads=[Bxs], writes=[Blb])
            P.op("tensor", lambda e: e.transpose(plT[0:64, 0, :], lb[:, 0:64], identb[:]), reads=[Blb, Bidb], writes=[BplT])
            P.op("tensor", lambda e: e.transpose(plT[0:64, 1, :], lb[:, 64:128], identb[:]), reads=[Blb, Bidb], writes=[BplT])
            P.op("tensor", lambda e: e.transpose(plT[:, 2, :], lb[:, 128:256], identb[:]), reads=[Blb, Bidb], writes=[BplT])
            P.op("vector", lambda e: e.tensor_copy(lT[0:64, 0:2, :], plT[0:64, 0:2, :]), reads=[BplT], writes=[BlT])
            P.op("vector", lambda e: e.tensor_copy(lT[:, 2, :], plT[:, 2, :]), reads=[BplT], writes=[BlT])
            P.op("tensor", lambda e: e.matmul(pw[:], lT[0:64, 0, :], wup[:], start=True, stop=True), reads=[BlT, Bwup], writes=[Bpw])
            P.op("tensor", lambda e: e.matmul(pa[:], lT[0:64, 1, :], aup[:], start=True, stop=True), reads=[BlT, Baup], writes=[Bpa])
            P.op("tensor", lambda e: e.matmul(pg[:], lT[:, 2, :], gup[:], start=True, stop=True), reads=[BlT, Bgup], writes=[Bpg])
            P.op("vector", lambda e: e.tensor_tensor(zt[:], pw[:], w0[:], ALU.add), reads=[Bpw, Bw0], writes=[Bzt])
            P.op("vector", lambda e: e.tensor_scalar(nz[:], zt[:], -1.0, None, ALU.mult), reads=[Bzt], writes=[Bnz])
            P.op("vector", lambda e: e.tensor_tensor(az[:], zt[:], nz[:], ALU.max), reads=[Bzt, Bnz], writes=[Baz])
            P.op("scalar", lambda e: e.activation(az[:], az[:], AF.Exp, scale=-1.0), reads=[Baz], writes=[Baz])
            P.op("scalar", lambda e: e.activation(az[:], az[:], AF.Ln, bias=1.0, scale=1.0), reads=[Baz], writes=[Baz])
            P.op("vector", lambda e: e.scalar_tensor_tensor(az[:], nz[:], 0.0, az[:], ALU.max, ALU.add), reads=[Bnz, Baz], writes=[Baz])
            P.op("scalar", lambda e: e.activation(az[:], az[:], AF.Exp, bias=-0.5, scale=-1.0), reads=[Baz], writes=[Baz])
            P.op("scalar", lambda e: e.activation(O[:, 1, :], az[:], AF.Exp, scale=-1.0), reads=[Baz], writes=[BO])
            P.op("vector", lambda e: e.tensor_tensor(av[:], pa[:], a0[:], ALU.add), reads=[Bpa, Ba0], writes=[Bav])
            P.op("scalar", lambda e: e.activation(av[:], av[:], AF.Sigmoid), reads=[Bav], writes=[Bav])
            P.op("scalar", lambda e: e.activation(gg[i][:], pg[:], AF.Copy), reads=[Bpg], writes=[Bgg[i]])
            kr = xs[:, 512:1024]
            P.op("vector", lambda e: e.tensor_tensor(O[:, 3, :], kr, kkb[:], ALU.mult), reads=[Bxs, Bkkb], writes=[BO])
            P.op("gpsimd", lambda e: e.tensor_tensor(t1[:], O[:, 3, :], O[:, 3, :], ALU.mult), reads=[BO], writes=[Bt1])
            P.op("vector", lambda e: e.tensor_reduce(ss[:], t1[:].rearrange("p (h k) -> p h k", h=8), AX.X, ALU.add), reads=[Bt1], writes=[Bss])
            P.op("vector", lambda e: e.tensor_scalar(ss[:], ss[:], 1e-24, None, ALU.max), reads=[Bss], writes=[Bss])
            P.op("scalar", lambda e: e.activation(ss[:], ss[:], AF.Sqrt), reads=[Bss], writes=[Bss])
            P.op("vector", lambda e: e.reciprocal(ss[:], ss[:]), reads=[Bss], writes=[Bss])
            P.op("vector", lambda e: e.tensor_tensor(O[:, 3, :].rearrange("p (h k) -> p h k", h=8), O[:, 3, :].rearrange("p (h k) -> p h k", h=8),
                                                     ss[:].unsqueeze(2).to_broadcast([128, 8, 64]), ALU.mult), reads=[BO, Bss], writes=[BO])
            P.op("vector", lambda e: e.scalar_tensor_tensor(t1[:], av[:], -1.0, kab[:], ALU.add, ALU.mult), reads=[Bav, Bkab], writes=[Bt1])
            P.op("vector", lambda e: e.scalar_tensor_tensor(O[:, 2, :], t1[:], 1.0, kr, ALU.add, ALU.mult), reads=[Bt1, Bxs], writes=[BO])
            P.op("gpsimd", lambda e: e.tensor_tensor(O[:, 4, :], O[:, 3, :], av[:], ALU.mult), reads=[BO, Bav], writes=[BO])
            P.op("vector", lambda e: e.tensor_tensor(t1[:], O[:, 0, :], O[:, 2, :], ALU.mult), reads=[BO], writes=[Bt1])
            P.op("gpsimd", lambda e: e.tensor_tensor(t1[:], t1[:], rkb[:], ALU.mult), reads=[Bt1, Brkb], writes=[Bt1])
            P.op("vector", lambda e: e.tensor_reduce(bc[i][:], t1[:].rearrange("p (h k) -> p h k", h=8), AX.X, ALU.add), reads=[Bt1], writes=[Bbc[i]])
            r0 = b * T + c * 128
            OB, BOB = ob[i], Bob[i]
            P.op("scalar", lambda e: e.activation(OB[:, :, 0, :], O[:, 0:5, :], AF.Copy), reads=[BO], writes=[BOB])
            P.op("gpsimd", lambda e: e.tensor_tensor(rres[:], O[:, 0:5, :], OB[:, :, 0, :], ALU.subtract), reads=[BO, BOB], writes=[Brres])
            P.op("scalar", lambda e: e.activation(OB[:, :, 1, :], rres[:], AF.Copy), reads=[Brres], writes=[BOB])
            P.op("gpsimd", lambda e: e.tensor_tensor(rres[:], rres[:], OB[:, :, 1, :], ALU.subtract), reads=[Brres, BOB], writes=[Brres])
            P.op("scalar", lambda e: e.activation(OB[:, :, 2, :], rres[:], AF.Copy), reads=[Brres], writes=[BOB])
            P.store("sync", RWOPB[r0:r0 + 128, :, :, :], OB[:], BOB)
            P.store("sync", RWOP[r0:r0 + 128, :, :], O[:], BO)
            P.store("sync", RWG[r0:r0 + 128, :], gg[i][:], Bgg[i])
            P.store("sync", RWB[r0:r0 + 128, :], bc[i][:], Bbc[i])


def phase_rw_scan(P, l, T, RWOP, RWOPB, RWO, side_factory=None):
    nblk = T // 128
    with P.phase():
        identf, Bidf = make_ident(P, F32, "rs_identf")
        E = P.sb("rs_E", [128, 64, 128], BF16); BE = Buf("rs_E")
        P.op("gpsimd", lambda e: e.memset(E[:], 0.0), writes=[BE])
        for j in range(2):
            v_ = E[j * 64:(j + 1) * 64, :, j * 64:(j + 1) * 64]
            P.op("gpsimd", lambda e: e.affine_select(v_, v_, pattern=[[-1, 64], [0, 64]], compare_op=ALU.not_equal,
                                                     fill=1.0, base=0, channel_multiplier=1), reads=[BE], writes=[BE])
        Sp = [P.sb("rs_S%d" % i, [128, 512], F32) for i in range(2)]
        BSp = [Buf("rs_S%d" % i) for i in range(2)]
        P.op("vector", lambda e: e.memset(Sp[0][:], 0.0), writes=[BSp[0]])
        NB = 8
        t4 = [P.sb("rs_t4_%d" % i, [128, NB, 512], F32) for i in range(2)]
        Bt4 = [Buf("rs_t4_%d" % i) for i in range(2)]
        Xs = [P.sb("rs_X%d" % i, [128, 15, 2, 256], BF16) for i in range(2)]
        BX = [Buf("rs_X%d" % i) for i in range(2)]
        vt = [P.sb("rs_vt%d" % i, [128, 2, 4, 128], F32) for i in range(2)]
        Bvt = [Buf("rs_vt%d" % i) for i in range(2)]
        Vc = [P.sb("rs_Vc%d" % i, [128, 8, 128], F32) for i in range(2)]
        BVc = [Buf("rs_Vc%d" % i) for i in range(2)]
        Ob = [P.sb("rs_Ob%d" % i, [128, 128, 8], F32) for i in range(2)]
        BOb = [Buf("rs_Ob%d" % i) for i in range(2)]
        otok = [P.sb("rs_ot%d" % i, [128, 8, 128], F32) for i in range(2)]
        Bot = [Buf("rs_ot%d" % i) for i in range(2)]
        NR = 3
        R = [P.sb("rs_R%d" % i, [128, 5, 512], F32) for i in range(NR)]
        BR = [[Buf("rs_R%d_%d" % (i, o)) for o in range(5)] for i in range(NR)]
        vk = [P.sb("rs_vk%d" % i, [128, 512], F32) for i in range(2)]
        Bvk = [Buf("rs_vk%d" % i) for i in range(2)]
        tmp = P.sb("rs_tmp", [128, 512], F32); Btmp = Buf("rs_tmp")
        sa = P.sb("rs_sa", [128, 8], F32); Bsa = Buf("rs_sa")
        pdir = {3: P.ps("rs_pkk", [128, 512], F32), 1: P.ps("rs_pw", [128, 512], F32), 4: P.ps("rs_pkka", [128, 512], F32)}
        Bpdir = {3: Buf("rs_pkk"), 1: Buf("rs_pw"), 4: Buf("rs_pkka")}
        pkr = P.ps("rs_pkr", [128, 512], F32); Bpkr = Buf("rs_pkr")
        pv = P.ps("rs_pv", [128, 8, 128], F32); Bpv = Buf("rs_pv")
        po = P.ps("rs_po", [128, 8, 128], F32); Bpo = Buf("rs_po")

        def g3(ap):
            return ap.rearrange("p (g k) -> p g k", g=8)

        def load_seg(s, i):
            for half in range(2):
                for b in range(NSEQ):
                    r0 = b * T + s * 64
                    P.load("sync", Xs[i][half * 64:(half + 1) * 64, :, b, :],
                           RWOPB[r0:r0 + 64, :, :, half * 256:(half + 1) * 256].rearrange("t o q c -> t (o q) c"), BX[i])

        def load_v(blk, i):
            for b in range(NSEQ):
                r0 = b * T + blk * 128
                for g in range(4):
                    P.load("sync", vt[i][:, b, g, :].rearrange("p (j v) -> p j v", j=2),
                           RWOP[r0:r0 + 128, 5, :].rearrange("t (j g v) -> t g j v", j=2, g=4)[:, g], Bvt[i])

        side = list(side_factory()) if side_factory is not None else []
        side_every = max(1, (T - 8) // max(1, len(side))) if side else 0
        load_seg(0, 0)
        load_v(0, 0)
        pc = 0
        step = 0
        for blk in range(nblk):
            bi = blk % 2
            if blk + 1 < nblk:
                load_v(blk + 1, 1 - bi)
            for bg in range(8):
                P.op("tensor", lambda e: e.transpose(pv[:, bg, :], vt[bi][:, bg // 4, bg % 4, :], identf[:]),
                     reads=[Bvt[bi], Bidf], writes=[Bpv])
            P.op("scalar", lambda e: e.activation(Vc[bi][:], pv[:], AF.Copy), reads=[Bpv], writes=[BVc[bi]])
            for sg in range(2):
                s = blk * 2 + sg
                xi = s % 2
                if s + 1 < 2 * nblk:
                    load_seg(s + 1, 1 - xi)
                for t in range(64):
                    tt = sg * 64 + t
                    ri = step % NR
                    for o in (3, 2, 1, 4, 0):
                        if o in pdir:
                            pp, Bp = pdir[o], Bpdir[o]
                        else:
                            pp, Bp = pkr, Bpkr
                        for q in range(3):
                            P.op("tensor", lambda e: e.matmul(pp[:], E[:, t, :], Xs[xi][:, o * 3 + q, :, :].rearrange("p b c -> p (b c)"),
                                                              start=(q == 0), stop=(q == 2)), reads=[BE, BX[xi]], writes=[Bp])
                        if o not in pdir:
                            P.op("scalar", lambda e: e.activation(R[ri][:, o, :], pp[:], AF.Copy), reads=[Bp], writes=[BR[ri][o]])
                    P.mark(11)
                    r_b, k_b = R[ri][:, 0, :], R[ri][:, 2, :]
                    Br, Bk = BR[ri][0], BR[ri][2]
                    w_b, kk_b, kka_b = pdir[1][:], pdir[3][:], pdir[4][:]
                    Bw, Bkk, Bkka = Bpdir[1], Bpdir[3], Bpdir[4]
                    vi = step % 2
                    P.op("gpsimd", lambda e: e.tensor_tensor(g3(vk[vi][:]), g3(k_b), Vc[bi][:, :, tt].unsqueeze(2).to_broadcast([128, 8, 64]), ALU.mult),
                         reads=[Bk, BVc[bi]], writes=[Bvk[vi]])
                    P.mark(12)
                    So, BSo = Sp[step % 2], BSp[step % 2]
                    Sn, BSn = Sp[(step + 1) % 2], BSp[(step + 1) % 2]
                    P.op("vector", lambda e: e.tensor_tensor(tmp[:], So[:], kk_b, ALU.mult), reads=[BSo, Bkk], writes=[Btmp])
                    P.op("vector", lambda e: e.tensor_reduce(sa[:], g3(tmp[:]), AX.X, ALU.add), reads=[Btmp], writes=[Bsa])
                    P.mark(13)
                    P.op("vector", lambda e: e.tensor_tensor(Sn[:], So[:], w_b, ALU.mult), reads=[BSo, Bw], writes=[BSn])
                    P.op("vector", lambda e: e.tensor_tensor(g3(tmp[:]), g3(kka_b), sa[:].unsqueeze(2).to_broadcast([128, 8, 64]), ALU.mult),
                         reads=[Bkka, Bsa], writes=[Btmp])
                    P.op("vector", lambda e: e.tensor_tensor(Sn[:], Sn[:], tmp[:], ALU.subtract), reads=[BSn, Btmp], writes=[BSn])
                    P.op("vector", lambda e: e.tensor_tensor(Sn[:], Sn[:], vk[vi][:], ALU.add), reads=[BSn, Bvk[vi]], writes=[BSn])
                    ti, tj = (step // NB) % 2, step % NB
                    P.op("gpsimd", lambda e: e.tensor_tensor(t4[ti][:, tj, :], Sn[:], r_b, ALU.mult), reads=[BSn, Br], writes=[Bt4[ti]])
                    if tj == NB - 1:
                        P.op("vector", lambda e: e.tensor_reduce(Ob[bi][:, tt - NB + 1:tt + 1, :], t4[ti][:].rearrange("p n (g k) -> p n g k", g=8),
                                                                 AX.X, ALU.add), reads=[Bt4[ti]], writes=[BOb[bi]])
                    P.mute = False
                    step += 1
                    if side and step % side_every == 0:
                        side.pop(0)()
            P.mark(14)
            for bg in range(8):
                P.op("tensor", lambda e: e.transpose(po[:, bg, :], Ob[bi][:, :, bg], identf[:]), reads=[BOb[bi], Bidf], writes=[Bpo])
            P.op("scalar", lambda e: e.activation(otok[bi][:], po[:], AF.Copy), reads=[Bpo], writes=[Bot[bi]])
            for b in range(NSEQ):
                r0 = b * T + blk * 128
                for g in range(4):
                    P.store("sync", RWO[r0:r0 + 128, :].rearrange("t (j g v) -> t g j v", j=2, g=4)[:, g],
                            otok[bi][:, b * 4 + g, :].rearrange("p (j v) -> p j v", j=2), Bot[bi])
            P.mute = False
        while side:
            side.pop(0)()


def phase_rw_post(P, l, T, RWOP, RWG, RWB, RWO, HCAT, w):
    ntile = NSEQ * T // 128
    with P.phase():
        lng, Blng = bcast_load(P, "rq_lng", w["rw_ln_g"][l], 512)
        lnb, Blnb = bcast_load(P, "rq_lnb", w["rw_ln_b"][l], 512)
        ot = [P.sb("rq_o%d" % i, [128, 512], F32) for i in range(2)]
        Bot = [Buf("rq_o%d" % i) for i in range(2)]
        vv = [P.sb("rq_v%d" % i, [128, 512], F32) for i in range(2)]
        Bvv = [Buf("rq_v%d" % i) for i in range(2)]
        gg = [P.sb("rq_g%d" % i, [128, 512], F32) for i in range(2)]
        Bgg = [Buf("rq_g%d" % i) for i in range(2)]
        bc = [P.sb("rq_bc%d" % i, [128, 8], F32) for i in range(2)]
        Bbc = [Buf("rq_bc%d" % i) for i in range(2)]
        st6 = P.sb("rq_st6", [128, 8, 6], F32)
        mv = P.sb("rq_mv", [128, 8, 2], F32)
        rstd = P.sb("rq_rstd", [128, 8], F32)
        Bst = Buf("rq_st")
        hh = [P.sb("rq_h%d" % i, [128, 512], F32) for i in range(2)]
        Bhh = [Buf("rq_h%d" % i) for i in range(2)]

        def g3(ap):
            return ap.rearrange("p (g k) -> p g k", g=8)

        def loads(it, i):
            r0 = it * 128
            P.load("sync", ot[i][:], RWO[r0:r0 + 128, :], Bot[i])
            P.load("sync", vv[i][:], RWOP[r0:r0 + 128, 5, :], Bvv[i])
            P.load("sync", gg[i][:], RWG[r0:r0 + 128, :], Bgg[i])
            P.load("sync", bc[i][:], RWB[r0:r0 + 128, :], Bbc[i])

        loads(0, 0)
        for it in range(ntile):
            i = it % 2
            if it + 1 < ntile:
                loads(it + 1, 1 - i)
            O, BO = ot[i], Bot[i]
            for h in range(8):
                P.op("vector", lambda e: e.bn_stats(st6[:, h, :], O[:, h * 64:(h + 1) * 64]), reads=[BO], writes=[Bst])
            for h in range(8):
                P.op("vector", lambda e: e.bn_aggr(mv[:, h, :], st6[:, h, :]), reads=[Bst], writes=[Bst])
            P.op("scalar", lambda e: e.activation(rstd[:], mv[:, :, 1], AF.Ln, bias=64e-5, scale=1.0), reads=[Bst], writes=[Bst])
            P.op("scalar", lambda e: e.activation(rstd[:], rstd[:], AF.Exp, scale=-0.5), reads=[Bst], writes=[Bst])
            H, BH = hh[i], Bhh[i]
            P.op("vector", lambda e: e.tensor_tensor(g3(H[:]), g3(O[:]), mv[:, :, 0].unsqueeze(2).to_broadcast([128, 8, 64]), ALU.subtract),
                 reads=[BO, Bst], writes=[BH])
            P.op("vector", lambda e: e.tensor_tensor(g3(H[:]), g3(H[:]), rstd[:].unsqueeze(2).to_broadcast([128, 8, 64]), ALU.mult),
                 reads=[BH, Bst], writes=[BH])
            P.op("gpsimd", lambda e: e.tensor_tensor(H[:], H[:], lng[:], ALU.mult), reads=[BH, Blng], writes=[BH])
            P.op("gpsimd", lambda e: e.tensor_tensor(H[:], H[:], lnb[:], ALU.add), reads=[BH, Blnb], writes=[BH])
            P.op("vector", lambda e: e.tensor_tensor(g3(vv[i][:]), g3(vv[i][:]), bc[i][:].unsqueeze(2).to_broadcast([128, 8, 64]), ALU.mult),
                 reads=[Bvv[i], Bbc[i]], writes=[Bvv[i]])
            P.op("vector", lambda e: e.tensor_tensor(H[:], H[:], vv[i][:], ALU.add), reads=[BH, Bvv[i]], writes=[BH])
            P.op("vector", lambda e: e.tensor_tensor(H[:], H[:], gg[i][:], ALU.mult), reads=[BH, Bgg[i]], writes=[BH])
            r0 = it * 128
            P.store("sync", HCAT[r0:r0 + 128, 512:1024], H[:], BH)


def phase_merge(P, l, NT, U, HCAT, XRES, X1, w):
    ntile = NT // 128
    with P.phase():
        identb, Bidb = make_ident(P, BF16, "mg_identb")
        wbr = P.sb("mg_wbr", [128, 12, D], BF16); Bwbr = Buf("mg_wbr")
        for bi, nm in enumerate(("w_br_ml", "w_br_rw", "w_br_ca")):
            for k in range(4):
                P.load("gpsimd", wbr[:, bi * 4 + k, :], w[nm][l, k * 128:(k + 1) * 128, :], Bwbr)
        wo = P.sb("mg_wo", [128, 8, D], BF16); Bwo = Buf("mg_wo")
        for k in range(8):
            P.load("gpsimd", wo[:, k, :], w["w_o"][l, k * 128:(k + 1) * 128, :], Bwo)
        gbias, Bgb = bcast_load(P, "mg_gb", w["gate_b"][l], 3 * D)
        gam, Bg = bcast_load(P, "mg_g", w["ln1_g"][l], D)
        bet, Bb = bcast_load(P, "mg_b", w["ln1_b"][l], D)
        S = LNScratch(P, "mg")
        hc = [P.sb("mg_hc%d" % i, [128, 1536], F32) for i in range(2)]
        Bhc = [Buf("mg_hc%d" % i) for i in range(2)]
        ug = [P.sb("mg_ug%d" % i, [128, 3 * D], F32) for i in range(2)]
        Bug = [Buf("mg_ug%d" % i) for i in range(2)]
        xr = [P.sb("mg_xr%d" % i, [128, D], F32) for i in range(2)]
        Bxr = [Buf("mg_xr%d" % i) for i in range(2)]
        hb = P.sb("mg_hb", [128, 1536], BF16); Bhb = Buf("mg_hb")
        hT = P.sb("mg_hT", [128, 12, 128], BF16); BhT = Buf("mg_hT")
        ym = P.sb("mg_ym", [128, D], F32); Bym = Buf("mg_ym")
        tmp = P.sb("mg_tmp", [128, 512], F32); Btmp = Buf("mg_tmp")
        ymb = P.sb("mg_ymb", [128, D], BF16); Bymb = Buf("mg_ymb")
        yT = P.sb("mg_yT", [128, 8, 128], BF16); ByT = Buf("mg_yT")
        pt = P.ps("mg_pt", [128, 16, 128], BF16); Bpt = Buf("mg_pt")
        pb = [P.ps("mg_pb%d" % i, [128, 512], F32) for i in range(3)]
        Bpb = [Buf("mg_pb%d" % i) for i in range(3)]

        def loads(it, i):
            r0 = it * 128
            P.load("sync", hc[i][:], HCAT[r0:r0 + 128, :], Bhc[i])
            P.load("sync", ug[i][:], U[r0:r0 + 128, GATE0:GATE0 + 3 * D], Bug[i])
            P.load("sync", xr[i][:], XRES[r0:r0 + 128, :], Bxr[i])

        loads(0, 0)
        pc = 0
        for it in range(ntile):
            i = it % 2
            if it + 1 < ntile:
                loads(it + 1, 1 - i)
            P.op("scalar", lambda e: e.activation(hb[:], hc[i][:], AF.Copy), reads=[Bhc[i]], writes=[Bhb])
            for k in range(12):
                P.op("tensor", lambda e: e.transpose(pt[:, k, :], hb[:, k * 128:(k + 1) * 128], identb[:]), reads=[Bhb, Bidb], writes=[Bpt])
            P.op("vector", lambda e: e.tensor_copy(hT[:], pt[:, 0:12, :]), reads=[Bpt], writes=[BhT])
            P.op("gpsimd", lambda e: e.tensor_tensor(ug[i][:], ug[i][:], gbias[:], ALU.add), reads=[Bug[i], Bgb], writes=[Bug[i]])
            P.op("scalar", lambda e: e.activation(ug[i][:], ug[i][:], AF.Sigmoid), reads=[Bug[i]], writes=[Bug[i]])
            for br in range(3):
                for half in range(2):
                    pp, Bp = pb[pc % 3], Bpb[pc % 3]
                    pc += 1
                    for k in range(4):
                        P.op("tensor", lambda e: e.matmul(pp[:], hT[:, br * 4 + k, :], wbr[:, br * 4 + k, half * 512:(half + 1) * 512],
                                                          start=(k == 0), stop=(k == 3)), reads=[BhT, Bwbr], writes=[Bp])
                    gsl = ug[i][:, br * D + half * 512:br * D + (half + 1) * 512]
                    ysl = ym[:, half * 512:(half + 1) * 512]
                    if br == 0:
                        P.op("vector", lambda e: e.tensor_tensor(ysl, pp[:], gsl, ALU.mult), reads=[Bp, Bug[i]], writes=[Bym])
                    else:
                        P.op("vector", lambda e: e.tensor_tensor(tmp[:], pp[:], gsl, ALU.mult), reads=[Bp, Bug[i]], writes=[Btmp])
                        P.op("gpsimd", lambda e: e.tensor_tensor(ysl, ysl, tmp[:], ALU.add), reads=[Bym, Btmp], writes=[Bym])
            P.op("scalar", lambda e: e.activation(ymb[:], ym[:], AF.Copy), reads=[Bym], writes=[Bymb])
            for k in range(8):
                P.op("tensor", lambda e: e.transpose(pt[:, k, :], ymb[:, k * 128:(k + 1) * 128], identb[:]), reads=[Bymb, Bidb], writes=[Bpt])
            P.op("vector", lambda e: e.tensor_copy(yT[:], pt[:, 0:8, :]), reads=[Bpt], writes=[ByT])
            X, BX = xr[i], Bxr[i]
            for half in range(2):
                pp, Bp = pb[pc % 3], Bpb[pc % 3]
                pc += 1
                for k in range(8):
                    P.op("tensor", lambda e: e.matmul(pp[:], yT[:, k, :], wo[:, k, half * 512:(half + 1) * 512],
                                                      start=(k == 0), stop=(k == 7)), reads=[ByT, Bwo], writes=[Bp])
                xsl = X[:, half * 512:(half + 1) * 512]
                P.op("vector", lambda e: e.scalar_tensor_tensor(xsl, xsl, DN_ALPHA, pp[:], ALU.mult, ALU.add), reads=[BX, Bp], writes=[BX])
            layernorm(P, S, X[:], BX, gam[:], Bg, bet[:], Bb)
            P.store("sync", X1[it * 128:(it + 1) * 128, :], X[:], BX)


def phase_moe(P, l, NT, X1, XOUT, WGUB, WDNB, w):
    NTS = min(1024, NT)
    QT = min(512, NTS)
    nsup = NT // NTS
    tps = NTS // 128
    with P.phase():
        identf, Bidf = make_ident(P, F32, "me_identf")
        rwf = P.sb("me_rw", [128, 8, NE], F32); Brwf = Buf("me_rw")
        P.load("sync", rwf[:], w["router_w"][l].rearrange("(k p) e -> p k e", p=128), Brwf)
        rbias, Brb = bcast_load(P, "me_rb", w["router_b"][l], NE)
        bdn = P.sb("me_bdn", [NE, D], F32); Bbdn = Buf("me_bdn")
        P.load("sync", bdn[:], w["b_dn"][l], Bbdn)
        bgu = P.sb("me_bgu", [128, NE, 8, 2], F32); Bbgu = Buf("me_bgu")
        for e_ in range(NE):
            P.load("sync", bgu[:, e_, :, :], w["b_gu"][l, e_].rearrange("(i p two) -> p i two", p=128, two=2), Bbgu)
        P.op("vector", lambda e: e.tensor_scalar(bgu[:, :, :, 1], bgu[:, :, :, 1], 1.0, None, ALU.add), reads=[Bbgu], writes=[Bbgu])
        gam, Bg = bcast_load(P, "me_g", w["ln2_g"][l], D)
        bet, Bb = bcast_load(P, "me_b", w["ln2_b"][l], D)
        S = LNScratch(P, "me")
        xt = [P.sb("me_xt%d" % i, [128, D], F32) for i in range(2)]
        Bxt = [Buf("me_xt%d" % i) for i in range(2)]
        xTf = P.sb("me_xTf", [128, 8, 128], F32); BxTf = Buf("me_xTf")
        xT = P.sb("me_xT", [128, 8, NTS], BF16); BxT = Buf("me_xT")
        lg = P.sb("me_lg", [128, NE], F32); Blg = Buf("me_lg")
        top8 = P.sb("me_top8", [128, 8], F32); Btop8 = Buf("me_top8")
        msk = P.sb("me_msk", [128, NE], F32); Bmsk = Buf("me_msk")
        ssum = P.sb("me_ssum", [128, 1], F32); Bssum = Buf("me_ssum")
        G = P.sb("me_G", [128, tps, NE], F32); BG = Buf("me_G")
        Gs = P.sb("me_Gs", [128, tps, NE], F32); BGs = Buf("me_Gs")
        GT = P.sb("me_GT", [NE, tps, 128], F32); BGT = Buf("me_GT")
        acc = P.sb("me_acc", [128, tps, D], F32); Bacc = Buf("me_acc")
        wgu = [P.sb("me_wgu%d" % i, [128, 8, 2 * D], BF16) for i in range(2)]
        Bwgu = [Buf("me_wgu%d" % i) for i in range(2)]
        wdn = [P.sb("me_wdn%d" % i, [128, 8, D], BF16) for i in range(2)]
        Bwdn = [Buf("me_wdn%d" % i) for i in range(2)]
        actT = [P.sb("me_actT%d" % i, [128, 8, QT], BF16) for i in range(2)]
        BactT = [Buf("me_actT%d" % i) for i in range(2)]
        glu = P.sb("me_glu", [128, QT], F32); Bglu = Buf("me_glu")
        sgm = P.sb("me_sgm", [128, QT], F32); Bsgm = Buf("me_sgm")
        lin = P.sb("me_lin", [128, QT], F32); Blin = Buf("me_lin")
        pg = [P.ps("me_pg%d" % i, [128, 512], F32) for i in range(2)]
        Bpg = [Buf("me_pg%d" % i) for i in range(2)]
        pl = [P.ps("me_pl%d" % i, [128, 512], F32) for i in range(2)]
        Bpl = [Buf("me_pl%d" % i) for i in range(2)]
        pd = [P.ps("me_pd%d" % i, [128, 512], F32) for i in range(2)]
        Bpd = [Buf("me_pd%d" % i) for i in range(2)]
        px = P.ps("me_px", [128, 8, 128], F32); Bpx = Buf("me_px")

        def w_pieces(e_, i):
            def f1():
                P.load("sync", wgu[i][:], WGUB[e_].rearrange("(k p) n -> p k n", p=128), Bwgu[i])
            def f2():
                P.load("sync", wdn[i][:], WDNB[e_].rearrange("(k p) n -> p k n", p=128), Bwdn[i])
            yield f1
            yield f2

        def load_w(e_, i):
            for f in w_pieces(e_, i):
                f()

        wi = 0
        pc = 0
        ai = 0
        for sp in range(nsup):
            t0 = sp * NTS
            load_w(0, wi % 2)
            P.load("sync", xt[0][:], X1[t0:t0 + 128, :], Bxt[0])
            for tl in range(tps):
                i = tl % 2
                if tl + 1 < tps:
                    P.load("sync", xt[1 - i][:], X1[t0 + (tl + 1) * 128:t0 + (tl + 2) * 128, :], Bxt[1 - i])
                for k in range(8):
                    P.op("tensor", lambda e: e.transpose(px[:, k, :], xt[i][:, k * 128:(k + 1) * 128], identf[:]), reads=[Bxt[i], Bidf], writes=[Bpx])
                P.op("vector", lambda e: e.tensor_copy(xTf[:], px[:]), reads=[Bpx], writes=[BxTf])
                P.op("scalar", lambda e: e.activation(xT[:, :, tl * 128:(tl + 1) * 128], xTf[:], AF.Copy), reads=[BxTf], writes=[BxT])
                for k in range(8):
                    P.op("tensor", lambda e: e.matmul(px[:, 0, 0:NE], xTf[:, k, :], rwf[:, k, :], start=(k == 0), stop=(k == 7)),
                         reads=[BxTf, Brwf], writes=[Bpx])
                P.op("vector", lambda e: e.tensor_tensor(lg[:], px[:, 0, 0:NE], rbias[:], ALU.add), reads=[Bpx, Brb], writes=[Blg])
                P.op("vector", lambda e: e.max(out=top8[:], in_=lg[:]), reads=[Blg], writes=[Btop8])
                P.op("vector", lambda e: e.tensor_scalar(msk[:], lg[:], top8[:, 3:4], None, ALU.is_ge), reads=[Blg, Btop8], writes=[Bmsk])
                P.op("vector", lambda e: e.tensor_scalar(top8[:, 0:1], top8[:, 0:1], -1.0, None, ALU.mult), reads=[Btop8], writes=[Btop8])
                P.op("scalar", lambda e: e.activation(lg[:], lg[:], AF.Exp, bias=top8[:, 0:1], scale=1.0), reads=[Blg, Btop8], writes=[Blg])
                P.op("vector", lambda e: e.tensor_tensor(lg[:], lg[:], msk[:], ALU.mult), reads=[Blg, Bmsk], writes=[Blg])
                P.op("vector", lambda e: e.tensor_reduce(ssum[:], lg[:], AX.X, ALU.add), reads=[Blg], writes=[Bssum])
                P.op("vector", lambda e: e.reciprocal(ssum[:], ssum[:]), reads=[Bssum], writes=[Bssum])
                P.op("vector", lambda e: e.tensor_scalar(G[:, tl, :], lg[:], ssum[:, 0:1], None, ALU.mult), reads=[Blg, Bssum], writes=[BG])
                P.op("vector", lambda e: e.tensor_scalar(Gs[:, tl, :], lg[:], ssum[:, 0:1], 1.0 / 1.702, ALU.mult, ALU.mult), reads=[Blg, Bssum], writes=[BGs])
                P.op("tensor", lambda e: e.transpose(px[0:NE, 1, :], G[:, tl, :], identf[:]), reads=[BG, Bidf], writes=[Bpx])
                P.op("vector", lambda e: e.tensor_copy(GT[:, tl, :], px[0:NE, 1, :]), reads=[Bpx], writes=[BGT])
            for e_ in range(NE):
                ww = wi % 2
                pieces = list(w_pieces(e_ + 1, (wi + 1) % 2)) if e_ + 1 < NE else []
                nslots = (NTS // QT) * 8
                pps = -(-len(pieces) // nslots) if pieces else 0
                WG, BWG, WD, BWD = wgu[ww], Bwgu[ww], wdn[ww], Bwdn[ww]
                for q in range(NTS // QT):
                    A, BA = actT[ai % 2], BactT[ai % 2]
                    ai += 1
                    for fc in range(8):
                        pgg, Bpgg = pg[pc % 2], Bpg[pc % 2]
                        pll, Bpll = pl[pc % 2], Bpl[pc % 2]
                        pc += 1
                        for k in range(8):
                            P.op("tensor", lambda e: e.matmul(pgg[:, 0:QT], WG[:, k, fc * 256:(fc + 1) * 256:2], xT[:, k, q * QT:(q + 1) * QT],
                                                              start=(k == 0), stop=(k == 7)), reads=[BWG, BxT], writes=[Bpgg])
                        for k in range(8):
                            P.op("tensor", lambda e: e.matmul(pll[:, 0:QT], WG[:, k, fc * 256 + 1:(fc + 1) * 256:2], xT[:, k, q * QT:(q + 1) * QT],
                                                              start=(k == 0), stop=(k == 7)), reads=[BWG, BxT], writes=[Bpll])
                        P.op("vector", lambda e: e.tensor_scalar(glu[:], pgg[:, 0:QT], bgu[:, e_, fc, 0:1], 7.0, ALU.add, ALU.min),
                             reads=[Bpgg, Bbgu], writes=[Bglu])
                        P.op("scalar", lambda e: e.activation(sgm[:], glu[:], AF.Silu, scale=1.702), reads=[Bglu], writes=[Bsgm])
                        P.op("vector", lambda e: e.tensor_scalar(lin[:], pll[:, 0:QT], bgu[:, e_, fc, 1:2], -6.0, ALU.add, ALU.max),
                             reads=[Bpll, Bbgu], writes=[Blin])
                        P.op("vector", lambda e: e.scalar_tensor_tensor(A[:, fc, :], lin[:], 8.0, sgm[:], ALU.min, ALU.mult),
                             reads=[Blin, Bsgm], writes=[BA])
                        for _ in range(pps):
                            if pieces:
                                pieces.pop(0)()
                    for t4 in range(QT // 128):
                        tl = q * (QT // 128) + t4
                        for half in range(2):
                            pdd, Bpdd = pd[pc % 2], Bpd[pc % 2]
                            pc += 1
                            for fc in range(8):
                                P.op("tensor", lambda e: e.matmul(pdd[:], A[:, fc, t4 * 128:(t4 + 1) * 128], WD[:, fc, half * 512:(half + 1) * 512],
                                                                  start=(fc == 0), stop=(fc == 7)), reads=[BA, BWD], writes=[Bpdd])
                            asl = acc[:, tl, half * 512:(half + 1) * 512]
                            if e_ == 0:
                                P.op("vector", lambda e: e.tensor_scalar(asl, pdd[:], Gs[:, tl, e_:e_ + 1], None, ALU.mult),
                                     reads=[Bpdd, BGs], writes=[Bacc])
                            else:
                                P.op("vector", lambda e: e.scalar_tensor_tensor(asl, pdd[:], Gs[:, tl, e_:e_ + 1], asl, ALU.mult, ALU.add),
                                     reads=[Bpdd, BGs, Bacc], writes=[Bacc])
                while pieces:
                    pieces.pop(0)()
                wi += 1
            for tl in range(tps):
                i = tl % 2
                r0 = t0 + tl * 128
                P.load("sync", xt[i][:], X1[r0:r0 + 128, :], Bxt[i])
                X, BX = xt[i], Bxt[i]
                for half in range(2):
                    P.op("tensor", lambda e: e.matmul(px[:, half * 4:(half + 1) * 4, :].rearrange("p a b -> p (a b)"), GT[:, tl, :],
                                                      bdn[:, half * 512:(half + 1) * 512], start=True, stop=True), reads=[BGT, Bbdn], writes=[Bpx])
                P.op("vector", lambda e: e.tensor_tensor(acc[:, tl, :], acc[:, tl, :], px[:].rearrange("p a b -> p (a b)"), ALU.add),
                     reads=[Bacc, Bpx], writes=[Bacc])
                P.op("vector", lambda e: e.scalar_tensor_tensor(X[:], X[:], DN_ALPHA, acc[:, tl, :], ALU.mult, ALU.add), reads=[BX, Bacc], writes=[BX])
                layernorm(P, S, X[:], BX, gam[:], Bg, bet[:], Bb)
                P.store("sync", XOUT[r0:r0 + 128, :], X[:], BX)


def wcast_pieces(P, l, WGUB, WDNB, w, engs=("scalar", "vector", "gpsimd")):
    NS = 4
    st = [P.sb("wc_st%d" % i, [128, 2 * D], F32) for i in range(NS)]
    Bst = [Buf("wc_st%d" % i) for i in range(NS)]
    ob = [P.sb("wc_ob%d" % i, [128, 2 * D], BF16) for i in range(NS)]
    Bob = [Buf("wc_ob%d" % i) for i in range(NS)]
    cnt = [0]

    def cast(i):
        en = engs[cnt[0] % len(engs)]
        if en == "scalar":
            P.op("scalar", lambda e: e.activation(ob[i][:], st[i][:], AF.Copy), reads=[Bst[i]], writes=[Bob[i]])
        else:
            P.op(en, lambda e: e.tensor_copy(ob[i][:], st[i][:]), reads=[Bst[i]], writes=[Bob[i]])

    for e_ in range(NE):
        for k in range(8):
            def f(e_=e_, k=k):
                i = cnt[0] % NS
                P.load("sync", st[i][:], w["w_gu"][l, e_, k * 128:(k + 1) * 128, :], Bst[i])
                cast(i)
                P.store("sync", WGUB[e_, k * 128:(k + 1) * 128, :], ob[i][:], Bob[i])
                cnt[0] += 1
            yield f
        for k2 in range(4):
            def f(e_=e_, k2=k2):
                i = cnt[0] % NS
                P.load("sync", st[i][:].rearrange("p (two n) -> p two n", two=2),
                       w["w_dn"][l, e_, k2 * 256:(k2 + 1) * 256, :].rearrange("(two p) n -> p two n", p=128), Bst[i])
                cast(i)
                P.store("sync", WDNB[e_, k2 * 256:(k2 + 1) * 256, :].rearrange("(two p) n -> p two n", p=128),
                        ob[i][:].rearrange("p (two n) -> p two n", two=2), Bob[i])
                cnt[0] += 1
            yield f


def phase_wcast(P, l, WGUB, WDNB, w):
    with P.phase():
        for f in wcast_pieces(P, l, WGUB, WDNB, w):
            f()
```

```python
import numpy as np
import concourse.bass as bass
import concourse.mybir as mybir
from concourse.bass_utils import run_bass_kernel_spmd
from contextlib import ExitStack

F32 = mybir.dt.float32
BF16 = mybir.dt.bfloat16
U32 = mybir.dt.uint32
ALU = mybir.AluOpType
AF = mybir.ActivationFunctionType
AX = mybir.AxisListType


class Buf:
    __slots__ = ("name", "lw", "rd", "wsem", "rsem")

    def __init__(self, name):
        self.name = name
        self.lw = None
        self.rd = []
        self.wsem = None
        self.rsem = None


class Prog:
    def __init__(self, nc, stack):
        self.nc = nc
        self.gstack = stack
        self.stack = stack
        self.engs = {}
        for n in ("tensor", "vector", "scalar", "gpsimd", "sync"):
            e = getattr(nc, n)
            sem = stack.enter_context(nc.semaphore("es_" + n))
            self.engs[n] = dict(h=e, sem=sem, cnt=0, seen={}, seen_d={})
        self.sems = []
        self.free_sems = {"hw": [], "sw": []}
        self.sem_kind = {}
        self.phase_bufs = []
        self.ninstr = 0
        self.mute = False
        self.pid = 0
        import os
        self.cut = float(os.environ.get('ML_CUT', '99'))

    def sb(self, name, shape, dt):
        return self.stack.enter_context(self.nc.sbuf_tensor("%s_p%d" % (name, self.pid), list(shape), dt))

    def ps(self, name, shape, dt=F32):
        return self.stack.enter_context(self.nc.psum_tensor("%s_p%d" % (name, self.pid), list(shape), dt))

    def _getsem(self, en):
        kind = "sw" if en == "gpsimd" else "hw"
        if self.free_sems[kind]:
            return self.free_sems[kind].pop()
        h = self.gstack.enter_context(self.nc.semaphore("ds%d" % len(self.sems)))
        self.sems.append([h, 0])
        self.sem_kind[len(self.sems) - 1] = kind
        return len(self.sems) - 1

    def _need(self, en, dep):
        E = self.engs[en]
        if dep[0] == 'e':
            _, pn, cnt = dep
            if pn == en and en == "tensor":
                return
            if E["seen"].get(pn, 0) >= cnt:
                return
            E["seen"][pn] = cnt
            E["h"].wait_ge(self.engs[pn]["sem"], cnt)
        else:
            _, k, val = dep
            if E["seen_d"].get(k, 0) >= val:
                return
            E["seen_d"][k] = val
            E["h"].wait_ge(self.sems[k][0], val)

    def _deps(self, en, reads, writes):
        for b in reads:
            if b.lw is not None:
                self._need(en, b.lw)
        for b in writes:
            if b.lw is not None:
                self._need(en, b.lw)
            for r in b.rd:
                self._need(en, r)

    def mark(self, k):
        if k > self.cut:
            self.mute = True

    def op(self, en, fn, reads=(), writes=()):
        if self.mute:
            return None
        self._deps(en, reads, writes)
        E = self.engs[en]
        ins = fn(E["h"])
        E["cnt"] += 1
        ins.then_inc(E["sem"], 1)
        tag = ('e', en, E["cnt"])
        for b in reads:
            b.rd.append(tag)
        for b in writes:
            b.lw = tag
            b.rd = []
        self.ninstr += 1
        return ins

    def load(self, en, out, in_, sbuf, extra_reads=(), **kw):
        if self.mute:
            return None
        self._deps(en, extra_reads, [sbuf])
        if sbuf.wsem is None:
            sbuf.wsem = self._getsem(en)
            self.phase_bufs.append(sbuf)
        s = self.sems[sbuf.wsem]
        s[1] += 16
        ins = self.engs[en]["h"].dma_start(out=out, in_=in_, **kw)
        ins.then_inc(s[0], 16)
        sbuf.lw = ('d', sbuf.wsem, s[1])
        sbuf.rd = []
        self.ninstr += 1
        return ins

    def store(self, en, out, in_, sbuf, **kw):
        if self.mute:
            return None
        self._deps(en, [sbuf], [])
        if sbuf.rsem is None:
            sbuf.rsem = self._getsem(en)
            self.phase_bufs.append(sbuf)
        s = self.sems[sbuf.rsem]
        s[1] += 16
        ins = self.engs[en]["h"].dma_start(out=out, in_=in_, **kw)
        ins.then_inc(s[0], 16)
        sbuf.rd.append(('d', sbuf.rsem, s[1]))
        self.ninstr += 1
        return ins

    def barrier(self):
        S = self.engs["sync"]
        for n, E in self.engs.items():
            if n != "sync" and E["cnt"] > 0:
                self._need("sync", ('e', n, E["cnt"]))
        for k, (h, v) in enumerate(self.sems):
            if v > 0:
                self._need("sync", ('d', k, v))
        ins = S["h"].nop()
        S["cnt"] += 1
        ins.then_inc(S["sem"], 1)
        for n in self.engs:
            if n != "sync":
                self._need(n, ('e', "sync", S["cnt"]))
        for n, E in self.engs.items():
            for m, F in self.engs.items():
                if m != n:
                    E["seen"][m] = max(E["seen"].get(m, 0), F["cnt"])
            for k, (h, v) in enumerate(self.sems):
                E["seen_d"][k] = max(E["seen_d"].get(k, 0), v)
        for b in self.phase_bufs:
            for k in (b.wsem, b.rsem):
                if k is not None:
                    self.free_sems[self.sem_kind[k]].append(k)
            b.wsem = b.rsem = None
        self.phase_bufs = []

    class _Phase:
        def __init__(self, P):
            self.P = P
        def __enter__(self):
            self.es = ExitStack()
            self.es.__enter__()
            self.P.stack = self.es
            self.P.pid += 1
            return self
        def __exit__(self, *a):
            self.P.barrier()
            self.P.stack = self.P.gstack
            return self.es.__exit__(*a)

    def phase(self):
        return Prog._Phase(self)


D = 1024
DIN = 6920
QK0, V0, OG0, IG0, FG0, RW0, CAQ0, GATE0 = 0, 512, 1024, 1536, 1540, 1544, 3336, 3848
NSEQ = 2
MEM = 256
NE = 32
DN_ALPHA = 4.0 ** 0.25
LN_EPS = 1e-5


def bcast_load(P, name, vec_ap, n, eng="sync"):
    t = P.sb(name, [128, n], F32)
    B = Buf(name)
    P.load(eng, t[:], vec_ap.partition_broadcast(128), B)
    return t, B


class LNScratch:
    def __init__(self, P, tag):
        self.st6 = P.sb("ln_st6" + tag, [128, 2, 6], F32)
        self.mv = P.sb("ln_mv" + tag, [128, 2], F32)
        self.rstd = P.sb("ln_rstd" + tag, [128, 1], F32)
        self.B = Buf("ln_scr" + tag)


def layernorm(P, S, xt, Bx, gam, Bg, bet, Bb, eps=LN_EPS):
    for c in range(2):
        P.op("vector", lambda e: e.bn_stats(S.st6[:, c, :], xt[:, c * 512:(c + 1) * 512]), reads=[Bx], writes=[S.B])
    P.op("vector", lambda e: e.bn_aggr(S.mv[:], S.st6[:].rearrange("p a b -> p (a b)")), reads=[S.B], writes=[S.B])
    P.op("scalar", lambda e: e.activation(S.rstd[:], S.mv[:, 1:2], AF.Ln, bias=eps, scale=1.0), reads=[S.B], writes=[S.B])
    P.op("scalar", lambda e: e.activation(S.rstd[:], S.rstd[:], AF.Exp, scale=-0.5), reads=[S.B], writes=[S.B])
    P.op("vector", lambda e: e.tensor_scalar(xt, xt, S.mv[:, 0:1], S.rstd[:], ALU.subtract, ALU.mult), reads=[Bx, S.B], writes=[Bx])
    P.op("vector", lambda e: e.tensor_tensor(xt, xt, gam, ALU.mult), reads=[Bx, Bg], writes=[Bx])
    P.op("vector", lambda e: e.tensor_tensor(xt, xt, bet, ALU.add), reads=[Bx, Bb], writes=[Bx])


def make_ident(P, dt, name):
    t = P.sb(name, [128, 128], dt)
    B = Buf(name)
    P.op("gpsimd", lambda e: e.memset(t[:], 0.0), writes=[B])
    P.op("gpsimd", lambda e: e.affine_select(t[:], t[:], pattern=[[-1, 128]], compare_op=ALU.not_equal,
                                             fill=1.0, base=0, channel_multiplier=1), reads=[B], writes=[B])
    return t, B


def phase_proj(P, l, NT, xin, XRES, U, w):
    nt = NT // 128
    with P.phase():
        wb = P.sb("pa_wb", [128, 8, DIN], BF16)
        Bwb = Buf("pa_wb")
        for k in range(8):
            for c4 in range(4):
                P.load("gpsimd", wb[:, k, c4 * 1730:(c4 + 1) * 1730],
                       w["w_in"][l, k * 128:(k + 1) * 128, c4 * 1730:(c4 + 1) * 1730], Bwb)
        identb, Bidb = make_ident(P, BF16, "pa_identb")
        if l == 0:
            gam, Bg = bcast_load(P, "pa_g", w["ln_in_g"], D)
            bet, Bb = bcast_load(P, "pa_b", w["ln_in_b"], D)
        S = LNScratch(P, "pa")
        xt = [P.sb("pa_xt%d" % i, [128, D], F32) for i in range(2)]
        Bxt = [Buf("pa_xt%d" % i) for i in range(2)]
        xb = P.sb("pa_xb", [128, D], BF16)
        Bxb = Buf("pa_xb")
        xT = P.sb("pa_xT", [128, 8, 128], BF16)
        BxT = Buf("pa_xT")
        pt = P.ps("pa_pt", [128, 8, 128], BF16)
        Bpt = Buf("pa_pt")
        NPY = 3
        py = [P.ps("pa_py%d" % i, [128, 512], F32) for i in range(NPY)]
        Bpy = [Buf("pa_py%d" % i) for i in range(NPY)]
        NU = 4
        us = [P.sb("pa_us%d" % i, [128, 512], F32) for i in range(NU)]
        Bus = [Buf("pa_us%d" % i) for i in range(NU)]
        P.load("sync", xt[0][:], xin[0:128, :], Bxt[0])
        cc = 0
        for i in range(nt):
            X, BX = xt[i % 2], Bxt[i % 2]
            if i + 1 < nt:
                P.load("sync", xt[(i + 1) % 2][:], xin[(i + 1) * 128:(i + 2) * 128, :], Bxt[(i + 1) % 2])
            if l == 0:
                layernorm(P, S, X[:], BX, gam[:], Bg, bet[:], Bb)
                P.store("sync", XRES[i * 128:(i + 1) * 128, :], X[:], BX)
            P.op("scalar", lambda e: e.activation(xb[:], X[:], AF.Copy), reads=[BX], writes=[Bxb])
            for k in range(8):
                P.op("tensor", lambda e: e.transpose(pt[:, k, :], xb[:, k * 128:(k + 1) * 128], identb[:]),
                     reads=[Bxb, Bidb], writes=[Bpt])
            P.op("vector", lambda e: e.tensor_copy(xT[:], pt[:]), reads=[Bpt], writes=[BxT])
            for c in range(14):
                c0 = c * 512
                cw = min(512, DIN - c0)
                pp, Bp = py[cc % NPY], Bpy[cc % NPY]
                uu, Bu = us[cc % NU], Bus[cc % NU]
                for k in range(8):
                    P.op("tensor", lambda e: e.matmul(pp[:, :cw], xT[:, k, :], wb[:, k, c0:c0 + cw],
                                                      start=(k == 0), stop=(k == 7)),
                         reads=[BxT, Bwb], writes=[Bp])
                if cc % 2 == 0:
                    P.op("scalar", lambda e: e.activation(uu[:, :cw], pp[:, :cw], AF.Copy), reads=[Bp], writes=[Bu])
                else:
                    P.op("vector", lambda e: e.tensor_copy(uu[:, :cw], pp[:, :cw]), reads=[Bp], writes=[Bu])
                P.store("sync", U[i * 128:(i + 1) * 128, c0:c0 + cw], uu[:, :cw], Bu)
                cc += 1


def dram_in(nc, name, shape):
    return nc.dram_tensor(name, list(shape), F32, kind="ExternalInput").ap()


W_SHAPES = dict(
    ln_in_g=(D,), ln_in_b=(D,), mem_ln_g=(D,), mem_ln_b=(D,), w_in=(2, D, DIN),
    ml_conv_w=(2, 4, 512), ml_conv_b=(2, 512), ml_ig_b=(2, 4), ml_fg_b=(2, 4), ml_norm_g=(2, 512),
    rw_mu=(2, 1792), rw_w0=(2, 512), rw_w_up=(2, 64, 512), rw_a0=(2, 512), rw_a_up=(2, 64, 512),
    rw_g_up=(2, 128, 512), rw_kk=(2, 512), rw_ka=(2, 512), rw_rk=(2, 8, 64), rw_ln_g=(2, 512), rw_ln_b=(2, 512),
    ca_w_kv=(2, D, 1024), gate_b=(2, 3 * D), w_br_ml=(2, 512, D), w_br_rw=(2, 512, D), w_br_ca=(2, 512, D),
    w_o=(2, D, D), ln1_g=(2, D), ln1_b=(2, D), router_w=(2, D, NE), router_b=(2, NE),
    w_gu=(2, NE, D, 2 * D), b_gu=(2, NE, 2 * D), w_dn=(2, NE, D, D), b_dn=(2, NE, D), ln2_g=(2, D), ln2_b=(2, D),
)


def build(T=4096, nlayers=2, phases=None, dbg=()):
    NT = NSEQ * T
    nc = bass.Bass("TRN2", target_bir_lowering=False)
    x = dram_in(nc, "x", (NT, D))
    mem = dram_in(nc, "mem", (NSEQ * MEM, D))
    w = {k: dram_in(nc, k, s) for k, s in W_SHAPES.items()}
    out = nc.dram_tensor("out", [NT, D], F32, kind="ExternalOutput").ap()

    def scratch(name, shape, dt=F32):
        kind = "ExternalOutput" if name in dbg else "Internal"
        return nc.dram_tensor(name, list(shape), dt, kind=kind).ap()

    XRES = scratch("XRES", (NT, D))
    U = scratch("U", (NT, DIN))
    HCAT = scratch("HCAT", (NT, 1536))
    RWOP = scratch("RWOP", (NT, 6, 512))
    RWOPB = scratch("RWOPB", (NT, 5, 3, 512), BF16)
    RWG = scratch("RWG", (NT, 512))
    RWB = scratch("RWB", (NT, 8))
    RWO = scratch("RWO", (NT, 512))
    X1 = scratch("X1", (NT, D))
    XRES2 = scratch("XRES2", (NT, D))
    WGUB = scratch("WGUB", (NE, D, 2 * D), BF16)
    WDNB = scratch("WDNB", (NE, D, D), BF16)
    with ExitStack() as st:
        P = Prog(nc, st)
        on = lambda n: phases is None or n in phases
        for l in range(nlayers):
            xin = x if l == 0 else XRES2
            xres = XRES if l == 0 else XRES2
            xout = out if l == nlayers - 1 else XRES2
            if on("proj"):
                phase_proj(P, l, NT, xin, XRES, U, w)
            if on("mlstm"):
                phase_mlstm(P, l, T, U, HCAT, w)
            if on("ca"):
                phase_ca(P, l, T, U, HCAT, mem, w)
            if on("rwpre"):
                phase_rw_pre(P, l, T, U, RWOP, RWOPB, RWG, RWB, w)
            if on("rwscan"):
                phase_rw_scan(P, l, T, RWOP, RWOPB, RWO,
                              side_factory=(lambda l=l: wcast_pieces(P, l, WGUB, WDNB, w, engs=("scalar",))) if on("moe") else None)
            if on("rwpost"):
                phase_rw_post(P, l, T, RWOP, RWG, RWB, RWO, HCAT, w)
            if on("merge"):
                phase_merge(P, l, NT, U, HCAT, xres, X1, w)
            if on("moe"):
                if not on("rwscan"):
                    phase_wcast(P, l, WGUB, WDNB, w)
                phase_moe(P, l, NT, X1, xout, WGUB, WDNB, w)
        P.barrier()
    print("instructions:", P.ninstr, "dma sems:", len(P.sems))
    return nc


_NC_CACHE = {}


def kernel(**inputs):
    ncores = 8
    T = 4096
    if T not in _NC_CACHE:
        _NC_CACHE[T] = build(T=T)
    nc = _NC_CACHE[T]
    wmap = {k: np.ascontiguousarray(inputs[k], dtype=np.float32) for k in W_SHAPES}
    x = np.asarray(inputs["x"], dtype=np.float32)
    mem = np.asarray(inputs["mem"], dtype=np.float32)
    in_maps = []
    for c in range(ncores):
        m = dict(wmap)
        m["x"] = np.ascontiguousarray(x[NSEQ * c:NSEQ * (c + 1)].reshape(NSEQ * T, D))
        m["mem"] = np.ascontiguousarray(mem[NSEQ * c:NSEQ * (c + 1)].reshape(NSEQ * MEM, D))
        in_maps.append(m)
    res = run_bass_kernel_spmd(nc, in_maps, core_ids=list(range(ncores)))
    outs = [np.asarray(r["out"]).reshape(NSEQ, T, D) for r in res.results]
    return np.concatenate(outs, axis=0).astype(np.float32)


def make_mask_le(P, name):
    t = P.sb(name, [128, 128], F32)
    B = Buf(name)
    P.op("gpsimd", lambda e: e.memset(t[:], 1.0), writes=[B])
    P.op("gpsimd", lambda e: e.affine_select(t[:], t[:], pattern=[[1, 128]], compare_op=ALU.is_ge,
                                             fill=0.0, base=0, channel_multiplier=-1), reads=[B], writes=[B])
    return t, B


def phase_mlstm(P, l, T, U, HCAT, w):
    nchunk = T // 128
    with P.phase():
        identf, Bidf = make_ident(P, F32, "ml_identf")
        identb, Bidb = make_ident(P, BF16, "ml_identb")
        mask, Bmask = make_mask_le(P, "ml_mask")
        sel = P.sb("ml_sel", [4, 4, 128], F32)
        Bsel = Buf("ml_sel")
        P.op("gpsimd", lambda e: e.memset(sel[:], 0.0), writes=[Bsel])
        P.op("gpsimd", lambda e: e.affine_select(sel[:], sel[:], pattern=[[-1, 4], [0, 128]], compare_op=ALU.not_equal,
                                                 fill=1.0, base=0, channel_multiplier=1), reads=[Bsel], writes=[Bsel])
        cw = P.sb("ml_cw", [64, 8, 4], F32)
        Bcw = Buf("ml_cw")
        for jj in range(4):
            P.load("sync", cw[:, :, jj], w["ml_conv_w"][l, jj].rearrange("(blk p) -> p blk", p=64), Bcw,
                   allow_slow_non_contiguous=True)
        cb = P.sb("ml_cb", [64, 8], F32)
        Bcb = Buf("ml_cb")
        P.load("sync", cb[:], w["ml_conv_b"][l].rearrange("(blk p) -> p blk", p=64), Bcb,
               allow_slow_non_contiguous=True)
        gb8 = P.sb("ml_gb8", [128, 8], F32)
        Bgb8 = Buf("ml_gb8")
        P.load("sync", gb8[:, 0:4], w["ml_ig_b"][l].partition_broadcast(128), Bgb8)
        P.load("sync", gb8[:, 4:8], w["ml_fg_b"][l].partition_broadcast(128), Bgb8)
        normg, Bng = bcast_load(P, "ml_normg", w["ml_norm_g"][l], 512)
        ones4 = P.sb("ml_ones4", [4, 128], F32)
        Bones4 = Buf("ml_ones4")
        P.op("vector", lambda e: e.memset(ones4[:], 1.0), writes=[Bones4])
        zeros4 = P.sb("ml_zeros4", [4, 128], F32)
        Bz4 = Buf("ml_zeros4")
        P.op("vector", lambda e: e.memset(zeros4[:], 0.0), writes=[Bz4])
        onesb = P.sb("ml_onesb", [128, 2], BF16)
        Bonesb = Buf("ml_onesb")
        P.op("vector", lambda e: e.memset(onesb[:], 1.0), writes=[Bonesb])

        C32 = P.sb("ml_C32", [64, 4, 132], F32)
        BC32 = Buf("ml_C32")
        Cb = P.sb("ml_Cb", [64, 4, 132], BF16)
        BCb = Buf("ml_Cb")
        uqk = [P.sb("ml_uqk%d" % i, [128, 512], F32) for i in range(2)]
        Buqk = [Buf("ml_uqk%d" % i) for i in range(2)]
        vt = [P.sb("ml_vt%d" % i, [128, 512], F32) for i in range(2)]
        Bvt = [Buf("ml_vt%d" % i) for i in range(2)]
        og = [P.sb("ml_og%d" % i, [128, 512], F32) for i in range(2)]
        Bog = [Buf("ml_og%d" % i) for i in range(2)]
        gt = [P.sb("ml_gt%d" % i, [128, 8], F32) for i in range(2)]
        Bgt = [Buf("ml_gt%d" % i) for i in range(2)]
        uT = [P.sb("ml_uT%d" % i, [64, 8, 131], F32) for i in range(2)]
        BuT = [Buf("ml_uT%d" % i) for i in range(2)]
        Fg = [P.sb("ml_F%d" % i, [4, 5, 128], F32) for i in range(2)]
        BF = [Buf("ml_F%d" % i) for i in range(2)]
        acc = P.sb("ml_acc", [64, 8, 128], F32)
        Bacc = Buf("ml_acc")
        qkb = P.sb("ml_qkb", [64, 8, 128], BF16)
        Bqkb = Buf("ml_qkb")
        ktok = P.sb("ml_ktok", [128, 4, 64], BF16)
        Bktok = Buf("ml_ktok")
        kw = P.sb("ml_kw", [128, 4, 64], BF16)
        Bkw = Buf("ml_kw")
        z8 = P.sb("ml_z8", [128, 8], F32)
        Bz8 = Buf("ml_z8")
        T6 = P.sb("ml_T6", [4, 6, 128], F32)
        BT6 = Buf("ml_T6")
        d41 = P.sb("ml_d41", [4, 1], F32)
        Bd41 = Buf("ml_d41")
        tokA = P.sb("ml_tokA", [128, 4], F32)
        BtokA = Buf("ml_tokA")
        tokE = P.sb("ml_tokE", [128, 4, 4], F32)
        BtokE = Buf("ml_tokE")
        Dm = P.sb("ml_D", [128, 4, 128], F32)
        BD = Buf("ml_D")
        Wt = P.sb("ml_Wt", [128, 4, 128], BF16)
        BWt = Buf("ml_Wt")
        vext = P.sb("ml_vext", [128, 4, 132], BF16)
        Bvext = Buf("ml_vext")
        P.op("vector", lambda e: e.memset(vext[:], 1.0), writes=[Bvext])
        numI = P.sb("ml_numI", [128, 4, 128], F32)
        BnumI = Buf("ml_numI")
        tot = P.sb("ml_tot", [128, 4, 128], F32)
        Btot = Buf("ml_tot")
        sm = P.sb("ml_sm", [128, 8, 4], F32)
        Bsm = Buf("ml_sm")
        st6 = P.sb("ml_st6", [128, 4, 6], F32)
        mv = P.sb("ml_mv", [128, 4, 2], F32)
        Bst = Buf("ml_st")
        sg = P.sb("ml_sg", [128, 512], F32)
        Bsg = Buf("ml_sg")
        hml = [P.sb("ml_hml%d" % i, [128, 512], F32) for i in range(2)]
        Bhml = [Buf("ml_hml%d" % i) for i in range(2)]
        b0 = P.ps("ml_b0", [128, 512], F32); Bb0 = Buf("ml_b0")
        b1 = P.ps("ml_b1", [128, 512], F32); Bb1 = Buf("ml_b1")
        pnG = P.ps("ml_pnG", [128, 4, 128], F32); BpnG = Buf("ml_pnG")
        pS = P.ps("ml_pS", [128, 4, 128], F32); BpS = Buf("ml_pS")
        pI = P.ps("ml_pI", [128, 4, 128], F32); BpI = Buf("ml_pI")
        pC = P.ps("ml_pC", [128, 4, 128], F32); BpC = Buf("ml_pC")
        pK = P.ps("ml_pK", [128, 4, 64], BF16); BpK = Buf("ml_pK")
        b7 = P.ps("ml_b7", [128, 512], F32); Bb7 = Buf("ml_b7")
        ptq = [b0[0:64, :].rearrange("p (a b) -> p a b", a=4), b7[0:64, :].rearrange("p (a b) -> p a b", a=4)]
        Bptq = [Bb0, Bb7]
        pU = [b0[0:64, 0:264].rearrange("p (a b) -> p a b", a=2), b7[0:64, 0:264].rearrange("p (a b) -> p a b", a=2)]
        pg = b1[0:4, 0:256].rearrange("p (a b) -> p a b", a=2)
        pT = b1[:, 256:276].rearrange("p (a b) -> p a b", a=5)
        pD = b1[:, 280:288]

        def loads(b, c, i):
            r0 = b * T + c * 128
            P.load("sync", uqk[i][:], U[r0:r0 + 128, QK0:QK0 + 512], Buqk[i])
            P.load("sync", vt[i][:], U[r0:r0 + 128, V0:V0 + 512], Bvt[i])
            P.load("sync", og[i][:], U[r0:r0 + 128, OG0:OG0 + 512], Bog[i])
            P.load("sync", gt[i][:], U[r0:r0 + 128, IG0:IG0 + 8], Bgt[i])

        it = 0
        loads(0, 0, 0)
        for b in range(NSEQ):
            P.op("gpsimd", lambda e: e.memset(C32[:], 0.0), writes=[BC32])
            P.op("gpsimd", lambda e: e.memset(Cb[:], 0.0), writes=[BCb])
            for c in range(nchunk):
                i = it % 2
                j = 1 - i
                nb, ncn = (b, c + 1) if c + 1 < nchunk else (b + 1, 0)
                if nb < NSEQ:
                    loads(nb, ncn, j)
                first = (c == 0)
                for blk in range(8):
                    P.op("tensor", lambda e: e.transpose(ptq[blk // 4][:, blk % 4, :], uqk[i][:, blk * 64:(blk + 1) * 64], identf[:]),
                         reads=[Buqk[i], Bidf], writes=[Bptq[blk // 4]])
                if first:
                    P.op("gpsimd", lambda e: e.memset(uT[i][:, :, 0:3], 0.0), writes=[BuT[i]])
                else:
                    P.op("gpsimd", lambda e: e.tensor_copy(uT[i][:, :, 0:3], uT[j][:, :, 128:131]), reads=[BuT[j]], writes=[BuT[i]])
                P.op("vector", lambda e: e.tensor_copy(uT[i][:, 0:4, 3:131], ptq[0]), reads=[Bb0], writes=[BuT[i]])
                P.op("vector", lambda e: e.tensor_copy(uT[i][:, 4:8, 3:131], ptq[1]), reads=[Bb7], writes=[BuT[i]])
                for blk in range(8):
                    P.op("scalar", lambda e: e.activation(acc[:, blk, :], uT[i][:, blk, 3:131], AF.Identity,
                                                          bias=cb[:, blk:blk + 1], scale=cw[:, blk, 3:4]),
                         reads=[BuT[i], Bcb, Bcw], writes=[Bacc])
                    for dd in range(1, 4):
                        P.op("vector", lambda e: e.scalar_tensor_tensor(acc[:, blk, :], uT[i][:, blk, 3 - dd:131 - dd],
                                                                        cw[:, blk, 3 - dd:4 - dd], acc[:, blk, :], ALU.mult, ALU.add),
                             reads=[BuT[i], Bcw, Bacc], writes=[Bacc])
                P.op("scalar", lambda e: e.activation(acc[:], acc[:], AF.Silu), reads=[Bacc], writes=[Bacc])
                P.op("vector", lambda e: e.tensor_scalar(qkb[:, 0:4, :], acc[:, 0:4, :], 0.125, None, ALU.mult), reads=[Bacc], writes=[Bqkb])
                P.op("gpsimd", lambda e: e.tensor_copy(qkb[:, 4:8, :], acc[:, 4:8, :]), reads=[Bacc], writes=[Bqkb])
                P.mark(1)
                for blk in range(4):
                    P.op("tensor", lambda e: e.transpose(pK[:, blk, :], qkb[:, 4 + blk, :], identb[0:64, 0:64]), reads=[Bqkb, Bidb], writes=[BpK])
                P.op("scalar", lambda e: e.activation(ktok[:], pK[:], AF.Copy), reads=[BpK], writes=[Bktok])
                P.mark(2)
                P.op("vector", lambda e: e.tensor_tensor(z8[:], gt[i][:], gb8[:], ALU.add), reads=[Bgt[i], Bgb8], writes=[Bz8])
                P.op("scalar", lambda e: e.activation(z8[:, 4:8], z8[:, 4:8], AF.Exp, scale=-1.0), reads=[Bz8], writes=[Bz8])
                P.op("scalar", lambda e: e.activation(z8[:, 4:8], z8[:, 4:8], AF.Ln, bias=1.0, scale=1.0), reads=[Bz8], writes=[Bz8])
                P.op("tensor", lambda e: e.transpose(pg[:, 0, :], z8[:, 0:4], identf[:]), reads=[Bz8, Bidf], writes=[Bb1])
                P.op("tensor", lambda e: e.transpose(pg[:, 1, :], z8[:, 4:8], identf[:]), reads=[Bz8, Bidf], writes=[Bb1])
                F = Fg[i]
                Fp = Fg[j]
                P.op("vector", lambda e: e.tensor_copy(F[:, 0, :], pg[:, 0, :]), reads=[Bb1], writes=[BF[i]])
                P.op("vector", lambda e: e.tensor_scalar(F[:, 1, :], pg[:, 1, :], -1.0, None, ALU.mult), reads=[Bb1], writes=[BF[i]])
                P.op("vector", lambda e: e.tensor_tensor_scan(F[:, 2, :], ones4[:], F[:, 1, :],
                                                              0.0 if first else Fp[:, 2, 127:128], ALU.mult, ALU.add),
                     reads=[BF[i], BF[j], Bones4], writes=[BF[i]])
                P.op("vector", lambda e: e.tensor_tensor(F[:, 3, :], F[:, 0, :], F[:, 2, :], ALU.subtract), reads=[BF[i]], writes=[BF[i]])
                P.op("vector", lambda e: e.tensor_tensor_scan(F[:, 4, :], F[:, 3, :], F[:, 3, :],
                                                              0.0 if first else Fp[:, 4, 127:128], ALU.max, ALU.max),
                     reads=[BF[i], BF[j]], writes=[BF[i]])
                gprev = zeros4[:, 0:1] if first else Fp[:, 4, 127:128]
                P.op("vector", lambda e: e.tensor_copy(T6[:, 0, :], F[:, 3, :]), reads=[BF[i]], writes=[BT6])
                P.op("vector", lambda e: e.scalar_tensor_tensor(T6[:, 1, :], F[:, 2, :], -1.0, F[:, 4, :], ALU.mult, ALU.subtract),
                     reads=[BF[i]], writes=[BT6])
                P.op("vector", lambda e: e.tensor_scalar(T6[:, 2, :], F[:, 4, :], -1.0, gprev, ALU.mult, ALU.add),
                     reads=[BF[i], BF[j], Bz4], writes=[BT6])
                P.op("vector", lambda e: e.tensor_scalar(T6[:, 3, :], F[:, 3, :], F[:, 4, 127:128], None, ALU.subtract),
                     reads=[BF[i]], writes=[BT6])
                P.op("vector", lambda e: e.tensor_tensor(d41[:], gprev, F[:, 4, 127:128], ALU.subtract),
                     reads=[BF[i], BF[j], Bz4], writes=[Bd41])
                P.op("vector", lambda e: e.tensor_scalar(T6[:, 4, :], zeros4[:], d41[:, 0:1], None, ALU.add),
                     reads=[Bz4, Bd41], writes=[BT6])
                P.op("vector", lambda e: e.tensor_scalar(T6[:, 5, :], F[:, 4, :], -1.0, None, ALU.mult), reads=[BF[i]], writes=[BT6])
                for r in range(5):
                    P.op("tensor", lambda e: e.transpose(pT[:, r, :], T6[:, r, :], identf[0:4, 0:4]), reads=[BT6, Bidf], writes=[Bb1])
                P.op("vector", lambda e: e.tensor_copy(tokA[:], pT[:, 0, :]), reads=[Bb1], writes=[BtokA])
                P.op("scalar", lambda e: e.activation(tokE[:], pT[:, 1:5, :], AF.Exp), reads=[Bb1], writes=[BtokE])
                P.mark(3)
                for h in range(4):
                    P.op("tensor", lambda e: e.matmul(pnG[:, h, :], sel[:, h, :], T6[:, 5, :], start=True, stop=True),
                         reads=[Bsel, BT6], writes=[BpnG])
                for h in range(4):
                    P.op("tensor", lambda e: e.matmul(pS[:, h, :], qkb[:, 4 + h, :], qkb[:, h, :],
                                                      start=True, stop=True), reads=[Bqkb], writes=[BpS])
                P.mark(3.1)
                for h in range(4):
                    P.op("vector", lambda e: e.tensor_scalar(Dm[:, h, :], pnG[:, h, :], tokA[:, h:h + 1], 0.0, ALU.add, ALU.min),
                         reads=[BpnG, BtokA], writes=[BD])
                P.mark(3.2)
                P.op("scalar", lambda e: e.activation(Dm[:], Dm[:], AF.Exp), reads=[BD], writes=[BD])
                P.mark(3.3)
                P.op("gpsimd", lambda e: e.tensor_tensor(Dm[:], Dm[:], mask[:].unsqueeze(1).to_broadcast([128, 4, 128]), ALU.mult),
                     reads=[BD, Bmask], writes=[BD])
                P.mark(3.4)
                P.op("vector", lambda e: e.tensor_tensor(Wt[:], Dm[:], pS[:], ALU.mult), reads=[BD, BpS], writes=[BWt])
                P.mark(3.5)
                P.op("gpsimd", lambda e: e.tensor_copy(vext[:, :, 0:128], vt[i][:].rearrange("p (h v) -> p h v", h=4)),
                     reads=[Bvt[i]], writes=[Bvext])
                P.mark(4)
                for h in range(4):
                    P.op("tensor", lambda e: e.matmul(pI[:, h, :], Wt[:, h, :], vext[:, h, 0:128], start=True, stop=True),
                         reads=[BWt, Bvext], writes=[BpI])
                    P.op("tensor", lambda e: e.matmul(pC[:, h, :], qkb[:, h, :], Cb[:, h, 0:128],
                                                      start=True, stop=True), reads=[Bqkb, BCb], writes=[BpC])
                    P.op("tensor", lambda e: e.matmul(pD[:, h:h + 1], Wt[:, h, :], onesb[:, 0:1], start=True, stop=True),
                         reads=[BWt, Bonesb], writes=[Bb1])
                    P.op("tensor", lambda e: e.matmul(pD[:, 4 + h:5 + h], qkb[:, h, :], Cb[:, h, 128:129],
                                                      start=True, stop=True), reads=[Bqkb, BCb], writes=[Bb1])
                P.op("scalar", lambda e: e.activation(numI[:], pI[:], AF.Copy), reads=[BpI], writes=[BnumI])
                for h in range(4):
                    P.op("vector", lambda e: e.scalar_tensor_tensor(tot[:, h, :], pC[:, h, :], tokE[:, 1, h:h + 1], numI[:, h, :],
                                                                    ALU.mult, ALU.add), reads=[BpC, BtokE, BnumI], writes=[Btot])
                P.op("vector", lambda e: e.tensor_tensor(sm[:, 0, :], pD[:, 4:8], tokE[:, 1, :], ALU.mult), reads=[Bb1, BtokE], writes=[Bsm])
                P.op("vector", lambda e: e.tensor_tensor(sm[:, 1, :], pD[:, 0:4], sm[:, 0, :], ALU.add), reads=[Bb1, Bsm], writes=[Bsm])
                P.op("vector", lambda e: e.tensor_scalar(sm[:, 2, :], sm[:, 1, :], -1.0, None, ALU.mult), reads=[Bsm], writes=[Bsm])
                P.op("vector", lambda e: e.tensor_tensor(sm[:, 2, :], sm[:, 2, :], sm[:, 1, :], ALU.max), reads=[Bsm], writes=[Bsm])
                P.op("vector", lambda e: e.tensor_tensor(sm[:, 3, :], sm[:, 2, :], tokE[:, 0, :], ALU.max), reads=[Bsm, BtokE], writes=[Bsm])
                P.op("vector", lambda e: e.reciprocal(sm[:, 4, :], sm[:, 3, :]), reads=[Bsm], writes=[Bsm])
                P.op("vector", lambda e: e.tensor_tensor(tot[:], tot[:], sm[:, 4, :].unsqueeze(2).to_broadcast([128, 4, 128]), ALU.mult),
                     reads=[Btot, Bsm], writes=[Btot])
                P.mark(5)
                for h in range(4):
                    P.op("vector", lambda e: e.bn_stats(st6[:, h, :], tot[:, h, :]), reads=[Btot], writes=[Bst])
                for h in range(4):
                    P.op("vector", lambda e: e.bn_aggr(mv[:, h, :], st6[:, h, :]), reads=[Bst], writes=[Bst])
                P.op("scalar", lambda e: e.activation(sm[:, 5, :], mv[:, :, 1], AF.Ln, bias=LN_EPS, scale=1.0), reads=[Bst], writes=[Bsm])
                P.op("scalar", lambda e: e.activation(sm[:, 5, :], sm[:, 5, :], AF.Exp, scale=-0.5), reads=[Bsm], writes=[Bsm])
                for h in range(4):
                    P.op("vector", lambda e: e.tensor_scalar(tot[:, h, :], tot[:, h, :], mv[:, h, 0:1], sm[:, 5, h:h + 1],
                                                             ALU.subtract, ALU.mult), reads=[Btot, Bst, Bsm], writes=[Btot])
                H, BH = hml[i], Bhml[i]
                P.op("scalar", lambda e: e.activation(sg[:], og[i][:], AF.Sigmoid), reads=[Bog[i]], writes=[Bsg])
                P.op("vector", lambda e: e.tensor_tensor(H[:], tot[:].rearrange("p h v -> p (h v)"), normg[:], ALU.mult),
                     reads=[Btot, Bng], writes=[BH])
                P.op("vector", lambda e: e.tensor_tensor(H[:], H[:], sg[:], ALU.mult), reads=[BH, Bsg], writes=[BH])
                r0 = b * T + c * 128
                P.store("sync", HCAT[r0:r0 + 128, 0:512], H[:], BH)
                P.mark(6)
                for h in range(4):
                    P.op("vector", lambda e: e.tensor_scalar(kw[:, h, :], ktok[:, h, :],
                                                             tokE[:, 2, h:h + 1], None, ALU.mult), reads=[Bktok, BtokE], writes=[Bkw])
                for h in range(4):
                    P.op("tensor", lambda e: e.matmul(pU[h // 2][:, h % 2, 0:129], kw[:, h, :], vext[:, h, 0:129], start=True, stop=True),
                         reads=[Bkw, Bvext], writes=[Bptq[h // 2]])
                for h in range(4):
                    P.op("vector", lambda e: e.scalar_tensor_tensor(C32[:, h, 0:129], C32[:, h, 0:129],
                                                                    tokE[0:64, 3, h:h + 1], pU[h // 2][:, h % 2, 0:129],
                                                                    ALU.mult, ALU.add), reads=[BC32, BtokE, Bptq[h // 2]], writes=[BC32])
                P.op("scalar", lambda e: e.activation(Cb[:], C32[:], AF.Copy), reads=[BC32], writes=[BCb])
                P.mute = False
                it += 1


def phase_ca(P, l, T, U, HCAT, mem, w):
    ntile = T // 128
    with P.phase():
        identb, Bidb = make_ident(P, BF16, "ca_identb")
        wkv = P.sb("ca_wkv", [128, 8, 1024], BF16)
        Bwkv = Buf("ca_wkv")
        for k in range(8):
            P.load("gpsimd", wkv[:, k, :], w["ca_w_kv"][l, k * 128:(k + 1) * 128, :], Bwkv)
        gam, Bg = bcast_load(P, "ca_g", w["mem_ln_g"], D)
        bet, Bb = bcast_load(P, "ca_b", w["mem_ln_b"], D)
        S = LNScratch(P, "ca")
        mt_ = P.sb("ca_mt", [128, D], F32); Bmt = Buf("ca_mt")
        mb = P.sb("ca_mb", [128, D], BF16); Bmb = Buf("ca_mb")
        memT = P.sb("ca_memT", [128, 8, 256], BF16); BmemT = Buf("ca_memT")
        kT = P.sb("ca_kT", [128, 4, 256], BF16); BkT = Buf("ca_kT")
        Vv = P.sb("ca_V", [128, 2, 512], BF16); BV = Buf("ca_V")
        qt = [P.sb("ca_qt%d" % i, [128, 512], F32) for i in range(2)]
        Bqt = [Buf("ca_qt%d" % i) for i in range(2)]
        qb = P.sb("ca_qb", [128, 512], BF16); Bqb = Buf("ca_qb")
        qT = P.sb("ca_qT", [128, 4, 128], BF16); BqT = Buf("ca_qT")
        mx = P.sb("ca_mx", [128, 4], F32); Bmx = Buf("ca_mx")
        rs = P.sb("ca_rs", [128, 4], F32); Brs = Buf("ca_rs")
        Pm = P.sb("ca_P", [128, 4, 256], BF16); BPm = Buf("ca_P")
        PT = P.sb("ca_PT", [128, 4, 2, 128], BF16); BPT = Buf("ca_PT")
        ho = [P.sb("ca_ho%d" % i, [128, 512], F32) for i in range(2)]
        Bho = [Buf("ca_ho%d" % i) for i in range(2)]
        pmt = P.ps("ca_pmt", [128, 8, 128], BF16); Bpmt = Buf("ca_pmt")
        pkv = P.ps("ca_pkv", [128, 512], F32); Bpkv = Buf("ca_pkv")
        pSc = P.ps("ca_pSc", [128, 4, 256], F32); BpSc = Buf("ca_pSc")
        pPT = P.ps("ca_pPT", [128, 4, 2, 128], BF16); BpPT = Buf("ca_pPT")
        pO = P.ps("ca_pO", [128, 4, 128], F32); BpO = Buf("ca_pO")
        pq = pmt[:, 0:4, :]
        it = 0
        for b in range(NSEQ):
            for mtile in range(2):
                r0 = b * MEM + mtile * 128
                P.load("sync", mt_[:], mem[r0:r0 + 128, :], Bmt)
                layernorm(P, S, mt_[:], Bmt, gam[:], Bg, bet[:], Bb)
                P.op("scalar", lambda e: e.activation(mb[:], mt_[:], AF.Copy), reads=[Bmt], writes=[Bmb])
                for k in range(8):
                    P.op("tensor", lambda e: e.transpose(pmt[:, k, :], mb[:, k * 128:(k + 1) * 128], identb[:]),
                         reads=[Bmb, Bidb], writes=[Bpmt])
                P.op("vector", lambda e: e.tensor_copy(memT[:, :, mtile * 128:(mtile + 1) * 128], pmt[:]), reads=[Bpmt], writes=[BmemT])
            for h in range(4):
                for k in range(8):
                    P.op("tensor", lambda e: e.matmul(pkv[:, 0:256], wkv[:, k, h * 128:(h + 1) * 128], memT[:, k, :],
                                                      start=(k == 0), stop=(k == 7)), reads=[Bwkv, BmemT], writes=[Bpkv])
                P.op("vector", lambda e: e.tensor_copy(kT[:, h, :], pkv[:, 0:256]), reads=[Bpkv], writes=[BkT])
            for mtile in range(2):
                for k in range(8):
                    P.op("tensor", lambda e: e.matmul(pkv[:], memT[:, k, mtile * 128:(mtile + 1) * 128], wkv[:, k, 512:1024],
                                                      start=(k == 0), stop=(k == 7)), reads=[Bwkv, BmemT], writes=[Bpkv])
                P.op("vector", lambda e: e.tensor_copy(Vv[:, mtile, :], pkv[:]), reads=[Bpkv], writes=[BV])
            P.load("sync", qt[it % 2][:], U[b * T:b * T + 128, CAQ0:CAQ0 + 512], Bqt[it % 2])
            for c in range(ntile):
                i = it % 2
                if c + 1 < ntile:
                    r1 = b * T + (c + 1) * 128
                    P.load("sync", qt[1 - i][:], U[r1:r1 + 128, CAQ0:CAQ0 + 512], Bqt[1 - i])
                P.op("scalar", lambda e: e.activation(qb[:], qt[i][:], AF.Copy, scale=128.0 ** -0.5), reads=[Bqt[i]], writes=[Bqb])
                for h in range(4):
                    P.op("tensor", lambda e: e.transpose(pq[:, h, :], qb[:, h * 128:(h + 1) * 128], identb[:]),
                         reads=[Bqb, Bidb], writes=[Bpmt])
                P.op("vector", lambda e: e.tensor_copy(qT[:], pq), reads=[Bpmt], writes=[BqT])
                for h in range(4):
                    P.op("tensor", lambda e: e.matmul(pSc[:, h, :], qT[:, h, :], kT[:, h, :], start=True, stop=True),
                         reads=[BqT, BkT], writes=[BpSc])
                P.op("vector", lambda e: e.tensor_reduce(mx[:], pSc[:], AX.X, ALU.max), reads=[BpSc], writes=[Bmx])
                P.op("vector", lambda e: e.tensor_scalar(mx[:], mx[:], -1.0, None, ALU.mult), reads=[Bmx], writes=[Bmx])
                for h in range(4):
                    P.op("scalar", lambda e: e.activation(Pm[:, h, :], pSc[:, h, :], AF.Exp, bias=mx[:, h:h + 1], scale=1.0,
                                                          accum_out=rs[:, h:h + 1]), reads=[BpSc, Bmx], writes=[BPm, Brs])
                for h in range(4):
                    for mtile in range(2):
                        P.op("tensor", lambda e: e.transpose(pPT[:, h, mtile, :], Pm[:, h, mtile * 128:(mtile + 1) * 128], identb[:]),
                             reads=[BPm, Bidb], writes=[BpPT])
                P.op("vector", lambda e: e.tensor_copy(PT[:], pPT[:]), reads=[BpPT], writes=[BPT])
                for h in range(4):
                    for mtile in range(2):
                        P.op("tensor", lambda e: e.matmul(pO[:, h, :], PT[:, h, mtile, :], Vv[:, mtile, h * 128:(h + 1) * 128],
                                                          start=(mtile == 0), stop=(mtile == 1)), reads=[BPT, BV], writes=[BpO])
                P.op("vector", lambda e: e.reciprocal(rs[:], rs[:]), reads=[Brs], writes=[Brs])
                H, BH = ho[i], Bho[i]
                P.op("vector", lambda e: e.tensor_tensor(H[:].rearrange("p (h v) -> p h v", h=4), pO[:],
                                                         rs[:].unsqueeze(2).to_broadcast([128, 4, 128]), ALU.mult),
                     reads=[BpO, Brs], writes=[BH])
                r0 = b * T + c * 128
                P.store("sync", HCAT[r0:r0 + 128, 1024:1536], H[:], BH)
                it += 1


def phase_rw_pre(P, l, T, U, RWOP, RWOPB, RWG, RWB, w):
    ntile = T // 128
    with P.phase():
        identb, Bidb = make_ident(P, BF16, "rp_identb")
        mu, Bmu = bcast_load(P, "rp_mu", w["rw_mu"][l], 1792)
        w0, Bw0 = bcast_load(P, "rp_w0", w["rw_w0"][l], 512)
        a0, Ba0 = bcast_load(P, "rp_a0", w["rw_a0"][l], 512)
        kkb, Bkkb = bcast_load(P, "rp_kk", w["rw_kk"][l], 512)
        kab, Bkab = bcast_load(P, "rp_ka", w["rw_ka"][l], 512)
        rkb, Brkb = bcast_load(P, "rp_rk", w["rw_rk"][l].rearrange("h k -> (h k)"), 512)
        wup = P.sb("rp_wup", [64, 512], BF16); Bwup = Buf("rp_wup")
        aup = P.sb("rp_aup", [64, 512], BF16); Baup = Buf("rp_aup")
        gup = P.sb("rp_gup", [128, 512], BF16); Bgup = Buf("rp_gup")
        P.load("gpsimd", wup[:], w["rw_w_up"][l], Bwup)
        P.load("gpsimd", aup[:], w["rw_a_up"][l], Baup)
        P.load("gpsimd", gup[:], w["rw_g_up"][l], Bgup)
        ucur = [P.sb("rp_ucur%d" % i, [128, 1792], F32) for i in range(2)]
        Bucur = [Buf("rp_ucur%d" % i) for i in range(2)]
        uprev = [P.sb("rp_uprev%d" % i, [128, 1792], F32) for i in range(2)]
        Buprev = [Buf("rp_uprev%d" % i) for i in range(2)]
        xs = P.sb("rp_xs", [128, 1792], F32); Bxs = Buf("rp_xs")
        lb = P.sb("rp_lb", [128, 256], BF16); Blb = Buf("rp_lb")
        lT = P.sb("rp_lT", [128, 3, 128], BF16); BlT = Buf("rp_lT")
        zt = P.sb("rp_zt", [128, 512], F32); Bzt = Buf("rp_zt")
        nz = P.sb("rp_nz", [128, 512], F32); Bnz = Buf("rp_nz")
        az = P.sb("rp_az", [128, 512], F32); Baz = Buf("rp_az")
        av = P.sb("rp_av", [128, 512], F32); Bav = Buf("rp_av")
        t1 = P.sb("rp_t1", [128, 512], F32); Bt1 = Buf("rp_t1")
        ss = P.sb("rp_ss", [128, 8], F32); Bss = Buf("rp_ss")
        ops = [P.sb("rp_ops%d" % i, [128, 6, 512], F32) for i in range(2)]
        Bops = [Buf("rp_ops%d" % i) for i in range(2)]
        gg = [P.sb("rp_g%d" % i, [128, 512], F32) for i in range(2)]
        Bgg = [Buf("rp_g%d" % i) for i in range(2)]
        bc = [P.sb("rp_bc%d" % i, [128, 8], F32) for i in range(2)]
        Bbc = [Buf("rp_bc%d" % i) for i in range(2)]
        ob = [P.sb("rp_ob%d" % i, [128, 5, 3, 512], BF16) for i in range(2)]
        Bob = [Buf("rp_ob%d" % i) for i in range(2)]
        rres = P.sb("rp_rres", [128, 5, 512], F32); Brres = Buf("rp_rres")
        plT = P.ps("rp_plT", [128, 3, 128], BF16); BplT = Buf("rp_plT")
        pw = P.ps("rp_pw", [128, 512], F32); Bpw = Buf("rp_pw")
        pa = P.ps("rp_pa", [128, 512], F32); Bpa = Buf("rp_pa")
        pg = P.ps("rp_pg", [128, 512], F32); Bpg = Buf("rp_pg")

        def loads(b, c, i):
            r0 = b * T + c * 128
            P.load("sync", ucur[i][:], U[r0:r0 + 128, RW0:RW0 + 1792], Bucur[i])
            if c == 0:
                P.op("vector", lambda e: e.memset(uprev[i][0:1, :], 0.0), writes=[Buprev[i]])
                P.load("sync", uprev[i][1:128, :], U[r0:r0 + 127, RW0:RW0 + 1792], Buprev[i])
            else:
                P.load("sync", uprev[i][:], U[r0 - 1:r0 + 127, RW0:RW0 + 1792], Buprev[i])

        tiles = [(b, c) for b in range(NSEQ) for c in range(ntile)]
        loads(0, 0, 0)
        for it, (b, c) in enumerate(tiles):
            i = it % 2
            if it + 1 < len(tiles):
                loads(tiles[it + 1][0], tiles[it + 1][1], 1 - i)
            O, BO = ops[i], Bops[i]
            P.op("vector", lambda e: e.tensor_tensor(xs[:], uprev[i][:], ucur[i][:], ALU.subtract), reads=[Buprev[i], Bucur[i]], writes=[Bxs])
            P.op("gpsimd", lambda e: e.tensor_tensor(xs[:], xs[:], mu[:], ALU.mult), reads=[Bxs, Bmu], writes=[Bxs])
            P.op("vector", lambda e: e.tensor_tensor(xs[:], xs[:], ucur[i][:], ALU.add), reads=[Bxs, Bucur[i]], writes=[Bxs])
            P.op("scalar", lambda e: e.activation(O[:, 0, :], xs[:, 0:512], AF.Copy), reads=[Bxs], writes=[BO])
            P.op("scalar", lambda e: e.activation(O[:, 5, :], xs[:, 1024:1536], AF.Copy), reads=[Bxs], writes=[BO])
            P.op("scalar", lambda e: e.activation(lb[:, 0:64], xs[:, 1536:1600], AF.Tanh), reads=[Bxs], writes=[Blb])
            P.op("scalar", lambda e: e.activation(lb[:, 128:256], xs[:, 1664:1792], AF.Sigmoid), reads=[Bxs], writes=[Blb])
            P.op("vector", lambda e: e.tensor_copy(lb[:, 64:128], xs[:, 1600:1664]), reads=[Bxs], writes=[Blb])
            P.op("tensor", lambda e: e.transpose(plT[0:64, 0, :], lb[:, 0:64], identb[:]), reads=[Blb, Bidb], writes=[BplT])
            P.op("tensor", lambda e: e.transpose(plT[0:64, 1, :], lb[:, 64:128], identb[:]), reads=[Blb, Bidb], writes=[BplT])
            P.op("tensor", lambda e: e.transpose(plT[:, 2, :], lb[:, 128:256], identb[:]), reads=[Blb, Bidb], writes=[BplT])
            P.op("vector", lambda e: e.tensor_copy(lT[0:64, 0:2, :], plT[0:64, 0:2, :]), reads=[BplT], writes=[BlT])
            P.op("vector", lambda e: e.tensor_copy(lT[:, 2, :], plT[:, 2, :]), reads=[BplT], writes=[BlT])
            P.op("tensor", lambda e: e.matmul(pw[:], lT[0:64, 0, :], wup[:], start=True, stop=True), reads=[BlT, Bwup], writes=[Bpw])
            P.op("tensor", lambda e: e.matmul(pa[:], lT[0:64, 1, :], aup[:], start=True, stop=True), reads=[BlT, Baup], writes=[Bpa])
            P.op("tensor", lambda e: e.matmul(pg[:], lT[:, 2, :], gup[:], start=True, stop=True), reads=[BlT, Bgup], writes=[Bpg])
            P.op("vector", lambda e: e.tensor_tensor(zt[:], pw[:], w0[:], ALU.add), reads=[Bpw, Bw0], writes=[Bzt])
            P.op("vector", lambda e: e.tensor_scalar(nz[:], zt[:], -1.0, None, ALU.mult), reads=[Bzt], writes=[Bnz])
            P.op("vector", lambda e: e.tensor_tensor(az[:], zt[:], nz[:], ALU.max), reads=[Bzt, Bnz], writes=[Baz])
            P.op("scalar", lambda e: e.activation(az[:], az[:], AF.Exp, scale=-1.0), reads=[Baz], writes=[Baz])
            P.op("scalar", lambda e: e.activation(az[:], az[:], AF.Ln, bias=1.0, scale=1.0), reads=[Baz], writes=[Baz])
            P.op("vector", lambda e: e.scalar_tensor_tensor(az[:], nz[:], 0.0, az[:], ALU.max, ALU.add), reads=[Bnz, Baz], writes=[Baz])
            P.op("scalar", lambda e: e.activation(az[:], az[:], AF.Exp, bias=-0.5, scale=-1.0), reads=[Baz], writes=[Baz])
            P.op("scalar", lambda e: e.activation(O[:, 1, :], az[:], AF.Exp, scale=-1.0), reads=[Baz], writes=[BO])
            P.op("vector", lambda e: e.tensor_tensor(av[:], pa[:], a0[:], ALU.add), reads=[Bpa, Ba0], writes=[Bav])
            P.op("scalar", lambda e: e.activation(av[:], av[:], AF.Sigmoid), reads=[Bav], writes=[Bav])
            P.op("scalar", lambda e: e.activation(gg[i][:], pg[:], AF.Copy), reads=[Bpg], writes=[Bgg[i]])
            kr = xs[:, 512:1024]
            P.op("vector", lambda e: e.tensor_tensor(O[:, 3, :], kr, kkb[:], ALU.mult), reads=[Bxs, Bkkb], writes=[BO])
            P.op("gpsimd", lambda e: e.tensor_tensor(t1[:], O[:, 3, :], O[:, 3, :], ALU.mult), reads=[BO], writes=[Bt1])
            P.op("vector", lambda e: e.tensor_reduce(ss[:], t1[:].rearrange("p (h k) -> p h k", h=8), AX.X, ALU.add), reads=[Bt1], writes=[Bss])
            P.op("vector", lambda e: e.tensor_scalar(ss[:], ss[:], 1e-24, None, ALU.max), reads=[Bss], writes=[Bss])
            P.op("scalar", lambda e: e.activation(ss[:], ss[:], AF.Sqrt), reads=[Bss], writes=[Bss])
            P.op("vector", lambda e: e.reciprocal(ss[:], ss[:]), reads=[Bss], writes=[Bss])
            P.op("vector", lambda e: e.tensor_tensor(O[:, 3, :].rearrange("p (h k) -> p h k", h=8), O[:, 3, :].rearrange("p (h k) -> p h k", h=8),
                                                     ss[:].unsqueeze(2).to_broadcast([128, 8, 64]), ALU.mult), reads=[BO, Bss], writes=[BO])
            P.op("vector", lambda e: e.scalar_tensor_tensor(t1[:], av[:], -1.0, kab[:], ALU.add, ALU.mult), reads=[Bav, Bkab], writes=[Bt1])
            P.op("vector", lambda e: e.scalar_tensor_tensor(O[:, 2, :], t1[:], 1.0, kr, ALU.add, ALU.mult), reads=[Bt1, Bxs], writes=[BO])
            P.op("gpsimd", lambda e: e.tensor_tensor(O[:, 4, :], O[:, 3, :], av[:], ALU.mult), reads=[BO, Bav], writes=[BO])
            P.op("vector", lambda e: e.tensor_tensor(t1[:], O[:, 0, :], O[:, 2, :], ALU.mult), reads=[BO], writes=[Bt1])
            P.op("gpsimd", lambda e: e.tensor_tensor(t1[:], t1[:], rkb[:], ALU.mult), reads=[Bt1, Brkb], writes=[Bt1])
            P.op("vector", lambda e: e.tensor_reduce(bc[i][:], t1[:].rearrange("p (h k) -> p h k", h=8), AX.X, ALU.add), reads=[Bt1], writes=[Bbc[i]])
            r0 = b * T + c * 128
            OB, BOB = ob[i], Bob[i]
            P.op("scalar", lambda e: e.activation(OB[:, :, 0, :], O[:, 0:5, :], AF.Copy), reads=[BO], writes=[BOB])
            P.op("gpsimd", lambda e: e.tensor_tensor(rres[:], O[:, 0:5, :], OB[:, :, 0, :], ALU.subtract), reads=[BO, BOB], writes=[Brres])
            P.op("scalar", lambda e: e.activation(OB[:, :, 1, :], rres[:], AF.Copy), reads=[Brres], writes=[BOB])
            P.op("gpsimd", lambda e: e.tensor_tensor(rres[:], rres[:], OB[:, :, 1, :], ALU.subtract), reads=[Brres, BOB], writes=[Brres])
            P.op("scalar", lambda e: e.activation(OB[:, :, 2, :], rres[:], AF.Copy), reads=[Brres], writes=[BOB])
            P.store("sync", RWOPB[r0:r0 + 128, :, :, :], OB[:], BOB)
            P.store("sync", RWOP[r0:r0 + 128, :, :], O[:], BO)
            P.store("sync", RWG[r0:r0 + 128, :], gg[i][:], Bgg[i])
            P.store("sync", RWB[r0:r0 + 128, :], bc[i][:], Bbc[i])


def phase_rw_scan(P, l, T, RWOP, RWOPB, RWO, side_factory=None):
    nblk = T // 128
    with P.phase():
        identf, Bidf = make_ident(P, F32, "rs_identf")
        E = P.sb("rs_E", [128, 64, 128], BF16); BE = Buf("rs_E")
        P.op("gpsimd", lambda e: e.memset(E[:], 0.0), writes=[BE])
        for j in range(2):
            v_ = E[j * 64:(j + 1) * 64, :, j * 64:(j + 1) * 64]
            P.op("gpsimd", lambda e: e.affine_select(v_, v_, pattern=[[-1, 64], [0, 64]], compare_op=ALU.not_equal,
                                                     fill=1.0, base=0, channel_multiplier=1), reads=[BE], writes=[BE])
        Sp = [P.sb("rs_S%d" % i, [128, 512], F32) for i in range(2)]
        BSp = [Buf("rs_S%d" % i) for i in range(2)]
        P.op("vector", lambda e: e.memset(Sp[0][:], 0.0), writes=[BSp[0]])
        NB = 8
        t4 = [P.sb("rs_t4_%d" % i, [128, NB, 512], F32) for i in range(2)]
        Bt4 = [Buf("rs_t4_%d" % i) for i in range(2)]
        Xs = [P.sb("rs_X%d" % i, [128, 15, 2, 256], BF16) for i in range(2)]
        BX = [Buf("rs_X%d" % i) for i in range(2)]
        vt = [P.sb("rs_vt%d" % i, [128, 2, 4, 128], F32) for i in range(2)]
        Bvt = [Buf("rs_vt%d" % i) for i in range(2)]
        Vc = [P.sb("rs_Vc%d" % i, [128, 8, 128], F32) for i in range(2)]
        BVc = [Buf("rs_Vc%d" % i) for i in range(2)]
        Ob = [P.sb("rs_Ob%d" % i, [128, 128, 8], F32) for i in range(2)]
        BOb = [Buf("rs_Ob%d" % i) for i in range(2)]
        otok = [P.sb("rs_ot%d" % i, [128, 8, 128], F32) for i in range(2)]
        Bot = [Buf("rs_ot%d" % i) for i in range(2)]
        NR = 3
        R = [P.sb("rs_R%d" % i, [128, 5, 512], F32) for i in range(NR)]
        BR = [[Buf("rs_R%d_%d" % (i, o)) for o in range(5)] for i in range(NR)]
        vk = [P.sb("rs_vk%d" % i, [128, 512], F32) for i in range(2)]
        Bvk = [Buf("rs_vk%d" % i) for i in range(2)]
        tmp = P.sb("rs_tmp", [128, 512], F32); Btmp = Buf("rs_tmp")
        sa = P.sb("rs_sa", [128, 8], F32); Bsa = Buf("rs_sa")
        pdir = {3: P.ps("rs_pkk", [128, 512], F32), 1: P.ps("rs_pw", [128, 512], F32), 4: P.ps("rs_pkka", [128, 512], F32)}
        Bpdir = {3: Buf("rs_pkk"), 1: Buf("rs_pw"), 4: Buf("rs_pkka")}
        pkr = P.ps("rs_pkr", [128, 512], F32); Bpkr = Buf("rs_pkr")
        pv = P.ps("rs_pv", [128, 8, 128], F32); Bpv = Buf("rs_pv")
        po, Bpo = pv, Bpv
        ptmp = P.ps("rs_ptmp", [128, 512], F32); Bptmp = Buf("rs_ptmp")

        def g3(ap):
            return ap.rearrange("p (g k) -> p g k", g=8)

        def load_seg(s, i):
            for half in range(2):
                for b in range(NSEQ):
                    r0 = b * T + s * 64
                    P.load("sync", Xs[i][half * 64:(half + 1) * 64, :, b, :],
                           RWOPB[r0:r0 + 64, :, :, half * 256:(half + 1) * 256].rearrange("t o q c -> t (o q) c"), BX[i])

        def load_v(blk, i):
            for b in range(NSEQ):
                r0 = b * T + blk * 128
                for g in range(4):
                    P.load("sync", vt[i][:, b, g, :].rearrange("p (j v) -> p j v", j=2),
                           RWOP[r0:r0 + 128, 5, :].rearrange("t (j g v) -> t g j v", j=2, g=4)[:, g], Bvt[i])

        side = list(side_factory()) if side_factory is not None else []
        side_every = max(1, (T - 8) // max(1, len(side))) if side else 0
        load_seg(0, 0)
        load_v(0, 0)
        pc = 0
        step = 0
        for blk in range(nblk):
            bi = blk % 2
            if blk + 1 < nblk:
                load_v(blk + 1, 1 - bi)
            for bg in range(8):
                P.op("tensor", lambda e: e.transpose(pv[:, bg, :], vt[bi][:, bg // 4, bg % 4, :], identf[:]),
                     reads=[Bvt[bi], Bidf], writes=[Bpv])
            P.op("scalar", lambda e: e.activation(Vc[bi][:], pv[:], AF.Copy), reads=[Bpv], writes=[BVc[bi]])
            for sg in range(2):
                s = blk * 2 + sg
                xi = s % 2
                if s + 1 < 2 * nblk:
                    load_seg(s + 1, 1 - xi)
                for t in range(64):
                    tt = sg * 64 + t
                    ri = step % NR
                    for o in (3, 2, 1, 4, 0):
                        if o in pdir:
                            pp, Bp = pdir[o], Bpdir[o]
                        else:
                            pp, Bp = pkr, Bpkr
                        for q in range(3):
                            P.op("tensor", lambda e: e.matmul(pp[:], E[:, t, :], Xs[xi][:, o * 3 + q, :, :].rearrange("p b c -> p (b c)"),
                                                              start=(q == 0), stop=(q == 2)), reads=[BE, BX[xi]], writes=[Bp])
                        if o not in pdir:
                            P.op("scalar", lambda e: e.activation(R[ri][:, o, :], pp[:], AF.Copy), reads=[Bp], writes=[BR[ri][o]])
                    P.mark(11)
                    r_b, k_b = R[ri][:, 0, :], R[ri][:, 2, :]
                    Br, Bk = BR[ri][0], BR[ri][2]
                    w_b, kk_b, kka_b = pdir[1][:], pdir[3][:], pdir[4][:]
                    Bw, Bkk, Bkka = Bpdir[1], Bpdir[3], Bpdir[4]
                    vi = step % 2
                    P.op("gpsimd", lambda e: e.tensor_tensor(g3(vk[vi][:]), g3(k_b), Vc[bi][:, :, tt].unsqueeze(2).to_broadcast([128, 8, 64]), ALU.mult),
                         reads=[Bk, BVc[bi]], writes=[Bvk[vi]])
                    P.mark(12)
                    So, BSo = Sp[step % 2], BSp[step % 2]
                    Sn, BSn = Sp[(step + 1) % 2], BSp[(step + 1) % 2]
                    P.op("vector", lambda e: e.tensor_tensor(tmp[:], So[:], kk_b, ALU.mult), reads=[BSo, Bkk], writes=[Btmp])
                    P.op("vector", lambda e: e.tensor_reduce(sa[:], g3(tmp[:]), AX.X, ALU.add), reads=[Btmp], writes=[Bsa])
                    P.mark(13)
                    P.op("vector", lambda e: e.tensor_tensor(Sn[:], So[:], w_b, ALU.mult), reads=[BSo, Bw], writes=[BSn])
                    P.op("vector", lambda e: e.tensor_tensor(g3(ptmp[:]), g3(kka_b), sa[:].unsqueeze(2).to_broadcast([128, 8, 64]), ALU.mult),
                         reads=[Bkka, Bsa], writes=[Bptmp])
                    P.op("vector", lambda e: e.tensor_tensor(Sn[:], Sn[:], ptmp[:], ALU.subtract), reads=[BSn, Bptmp], writes=[BSn])
                    P.op("vector", lambda e: e.tensor_tensor(Sn[:], Sn[:], vk[vi][:], ALU.add), reads=[BSn, Bvk[vi]], writes=[BSn])
                    ti, tj = (step // NB) % 2, step % NB
                    P.op("gpsimd", lambda e: e.tensor_tensor(t4[ti][:, tj, :], Sn[:], r_b, ALU.mult), reads=[BSn, Br], writes=[Bt4[ti]])
                    if tj == NB - 1:
                        P.op("vector", lambda e: e.tensor_reduce(Ob[bi][:, tt - NB + 1:tt + 1, :], t4[ti][:].rearrange("p n (g k) -> p n g k", g=8),
                                                                 AX.X, ALU.add), reads=[Bt4[ti]], writes=[BOb[bi]])
                    P.mute = False
                    step += 1
                    if side and step % side_every == 0:
                        side.pop(0)()
            P.mark(14)
            for bg in range(8):
                P.op("tensor", lambda e: e.transpose(po[:, bg, :], Ob[bi][:, :, bg], identf[:]), reads=[BOb[bi], Bidf], writes=[Bpo])
            P.op("scalar", lambda e: e.activation(otok[bi][:], po[:], AF.Copy), reads=[Bpo], writes=[Bot[bi]])
            for b in range(NSEQ):
                r0 = b * T + blk * 128
                for g in range(4):
                    P.store("sync", RWO[r0:r0 + 128, :].rearrange("t (j g v) -> t g j v", j=2, g=4)[:, g],
                            otok[bi][:, b * 4 + g, :].rearrange("p (j v) -> p j v", j=2), Bot[bi])
            P.mute = False
        while side:
            side.pop(0)()


def phase_rw_post(P, l, T, RWOP, RWG, RWB, RWO, HCAT, w):
    ntile = NSEQ * T // 128
    with P.phase():
        lng, Blng = bcast_load(P, "rq_lng", w["rw_ln_g"][l], 512)
        lnb, Blnb = bcast_load(P, "rq_lnb", w["rw_ln_b"][l], 512)
        ot = [P.sb("rq_o%d" % i, [128, 512], F32) for i in range(2)]
        Bot = [Buf("rq_o%d" % i) for i in range(2)]
        vv = [P.sb("rq_v%d" % i, [128, 512], F32) for i in range(2)]
        Bvv = [Buf("rq_v%d" % i) for i in range(2)]
        gg = [P.sb("rq_g%d" % i, [128, 512], F32) for i in range(2)]
        Bgg = [Buf("rq_g%d" % i) for i in range(2)]
        bc = [P.sb("rq_bc%d" % i, [128, 8], F32) for i in range(2)]
        Bbc = [Buf("rq_bc%d" % i) for i in range(2)]
        st6 = P.sb("rq_st6", [128, 8, 6], F32)
        mv = P.sb("rq_mv", [128, 8, 2], F32)
        rstd = P.sb("rq_rstd", [128, 8], F32)
        Bst = Buf("rq_st")
        hh = [P.sb("rq_h%d" % i, [128, 512], F32) for i in range(2)]
        Bhh = [Buf("rq_h%d" % i) for i in range(2)]

        def g3(ap):
            return ap.rearrange("p (g k) -> p g k", g=8)

        def loads(it, i):
            r0 = it * 128
            P.load("sync", ot[i][:], RWO[r0:r0 + 128, :], Bot[i])
            P.load("sync", vv[i][:], RWOP[r0:r0 + 128, 5, :], Bvv[i])
            P.load("sync", gg[i][:], RWG[r0:r0 + 128, :], Bgg[i])
            P.load("sync", bc[i][:], RWB[r0:r0 + 128, :], Bbc[i])

        loads(0, 0)
        for it in range(ntile):
            i = it % 2
            if it + 1 < ntile:
                loads(it + 1, 1 - i)
            O, BO = ot[i], Bot[i]
            for h in range(8):
                P.op("vector", lambda e: e.bn_stats(st6[:, h, :], O[:, h * 64:(h + 1) * 64]), reads=[BO], writes=[Bst])
            for h in range(8):
                P.op("vector", lambda e: e.bn_aggr(mv[:, h, :], st6[:, h, :]), reads=[Bst], writes=[Bst])
            P.op("scalar", lambda e: e.activation(rstd[:], mv[:, :, 1], AF.Ln, bias=64e-5, scale=1.0), reads=[Bst], writes=[Bst])
            P.op("scalar", lambda e: e.activation(rstd[:], rstd[:], AF.Exp, scale=-0.5), reads=[Bst], writes=[Bst])
            H, BH = hh[i], Bhh[i]
            P.op("vector", lambda e: e.tensor_tensor(g3(H[:]), g3(O[:]), mv[:, :, 0].unsqueeze(2).to_broadcast([128, 8, 64]), ALU.subtract),
                 reads=[BO, Bst], writes=[BH])
            P.op("vector", lambda e: e.tensor_tensor(g3(H[:]), g3(H[:]), rstd[:].unsqueeze(2).to_broadcast([128, 8, 64]), ALU.mult),
                 reads=[BH, Bst], writes=[BH])
            P.op("gpsimd", lambda e: e.tensor_tensor(H[:], H[:], lng[:], ALU.mult), reads=[BH, Blng], writes=[BH])
            P.op("gpsimd", lambda e: e.tensor_tensor(H[:], H[:], lnb[:], ALU.add), reads=[BH, Blnb], writes=[BH])
            P.op("vector", lambda e: e.tensor_tensor(g3(vv[i][:]), g3(vv[i][:]), bc[i][:].unsqueeze(2).to_broadcast([128, 8, 64]), ALU.mult),
                 reads=[Bvv[i], Bbc[i]], writes=[Bvv[i]])
            P.op("vector", lambda e: e.tensor_tensor(H[:], H[:], vv[i][:], ALU.add), reads=[BH, Bvv[i]], writes=[BH])
            P.op("vector", lambda e: e.tensor_tensor(H[:], H[:], gg[i][:], ALU.mult), reads=[BH, Bgg[i]], writes=[BH])
            r0 = it * 128
            P.store("sync", HCAT[r0:r0 + 128, 512:1024], H[:], BH)


def phase_merge(P, l, NT, U, HCAT, XRES, X1, w):
    ntile = NT // 128
    with P.phase():
        identb, Bidb = make_ident(P, BF16, "mg_identb")
        wbr = P.sb("mg_wbr", [128, 12, D], BF16); Bwbr = Buf("mg_wbr")
        for bi, nm in enumerate(("w_br_ml", "w_br_rw", "w_br_ca")):
            for k in range(4):
                P.load("gpsimd", wbr[:, bi * 4 + k, :], w[nm][l, k * 128:(k + 1) * 128, :], Bwbr)
        wo = P.sb("mg_wo", [128, 8, D], BF16); Bwo = Buf("mg_wo")
        for k in range(8):
            P.load("gpsimd", wo[:, k, :], w["w_o"][l, k * 128:(k + 1) * 128, :], Bwo)
        gbias, Bgb = bcast_load(P, "mg_gb", w["gate_b"][l], 3 * D)
        gam, Bg = bcast_load(P, "mg_g", w["ln1_g"][l], D)
        bet, Bb = bcast_load(P, "mg_b", w["ln1_b"][l], D)
        S = LNScratch(P, "mg")
        hc = [P.sb("mg_hc%d" % i, [128, 1536], F32) for i in range(2)]
        Bhc = [Buf("mg_hc%d" % i) for i in range(2)]
        ug = [P.sb("mg_ug%d" % i, [128, 3 * D], F32) for i in range(2)]
        Bug = [Buf("mg_ug%d" % i) for i in range(2)]
        xr = [P.sb("mg_xr%d" % i, [128, D], F32) for i in range(2)]
        Bxr = [Buf("mg_xr%d" % i) for i in range(2)]
        hb = P.sb("mg_hb", [128, 1536], BF16); Bhb = Buf("mg_hb")
        hT = P.sb("mg_hT", [128, 12, 128], BF16); BhT = Buf("mg_hT")
        ym = P.sb("mg_ym", [128, D], F32); Bym = Buf("mg_ym")
        tmp = P.sb("mg_tmp", [128, 512], F32); Btmp = Buf("mg_tmp")
        ymb = P.sb("mg_ymb", [128, D], BF16); Bymb = Buf("mg_ymb")
        yT = P.sb("mg_yT", [128, 8, 128], BF16); ByT = Buf("mg_yT")
        pt = P.ps("mg_pt", [128, 16, 128], BF16); Bpt = Buf("mg_pt")
        pb = [P.ps("mg_pb%d" % i, [128, 512], F32) for i in range(3)]
        Bpb = [Buf("mg_pb%d" % i) for i in range(3)]

        def loads(it, i):
            r0 = it * 128
            P.load("sync", hc[i][:], HCAT[r0:r0 + 128, :], Bhc[i])
            P.load("sync", ug[i][:], U[r0:r0 + 128, GATE0:GATE0 + 3 * D], Bug[i])
            P.load("sync", xr[i][:], XRES[r0:r0 + 128, :], Bxr[i])

        loads(0, 0)
        pc = 0
        for it in range(ntile):
            i = it % 2
            if it + 1 < ntile:
                loads(it + 1, 1 - i)
            P.op("scalar", lambda e: e.activation(hb[:], hc[i][:], AF.Copy), reads=[Bhc[i]], writes=[Bhb])
            for k in range(12):
                P.op("tensor", lambda e: e.transpose(pt[:, k, :], hb[:, k * 128:(k + 1) * 128], identb[:]), reads=[Bhb, Bidb], writes=[Bpt])
            P.op("vector", lambda e: e.tensor_copy(hT[:], pt[:, 0:12, :]), reads=[Bpt], writes=[BhT])
            P.op("gpsimd", lambda e: e.tensor_tensor(ug[i][:], ug[i][:], gbias[:], ALU.add), reads=[Bug[i], Bgb], writes=[Bug[i]])
            P.op("scalar", lambda e: e.activation(ug[i][:], ug[i][:], AF.Sigmoid), reads=[Bug[i]], writes=[Bug[i]])
            for br in range(3):
                for half in range(2):
                    pp, Bp = pb[pc % 3], Bpb[pc % 3]
                    pc += 1
                    for k in range(4):
                        P.op("tensor", lambda e: e.matmul(pp[:], hT[:, br * 4 + k, :], wbr[:, br * 4 + k, half * 512:(half + 1) * 512],
                                                          start=(k == 0), stop=(k == 3)), reads=[BhT, Bwbr], writes=[Bp])
                    gsl = ug[i][:, br * D + half * 512:br * D + (half + 1) * 512]
                    ysl = ym[:, half * 512:(half + 1) * 512]
                    if br == 0:
                        P.op("vector", lambda e: e.tensor_tensor(ysl, pp[:], gsl, ALU.mult), reads=[Bp, Bug[i]], writes=[Bym])
                    else:
                        P.op("vector", lambda e: e.tensor_tensor(tmp[:], pp[:], gsl, ALU.mult), reads=[Bp, Bug[i]], writes=[Btmp])
                        P.op("gpsimd", lambda e: e.tensor_tensor(ysl, ysl, tmp[:], ALU.add), reads=[Bym, Btmp], writes=[Bym])
            P.op("scalar", lambda e: e.activation(ymb[:], ym[:], AF.Copy), reads=[Bym], writes=[Bymb])
            for k in range(8):
                P.op("tensor", lambda e: e.transpose(pt[:, k, :], ymb[:, k * 128:(k + 1) * 128], identb[:]), reads=[Bymb, Bidb], writes=[Bpt])
            P.op("vector", lambda e: e.tensor_copy(yT[:], pt[:, 0:8, :]), reads=[Bpt], writes=[ByT])
            X, BX = xr[i], Bxr[i]
            for half in range(2):
                pp, Bp = pb[pc % 3], Bpb[pc % 3]
                pc += 1
                for k in range(8):
                    P.op("tensor", lambda e: e.matmul(pp[:], yT[:, k, :], wo[:, k, half * 512:(half + 1) * 512],
                                                      start=(k == 0), stop=(k == 7)), reads=[ByT, Bwo], writes=[Bp])
                xsl = X[:, half * 512:(half + 1) * 512]
                P.op("vector", lambda e: e.scalar_tensor_tensor(xsl, xsl, DN_ALPHA, pp[:], ALU.mult, ALU.add), reads=[BX, Bp], writes=[BX])
            layernorm(P, S, X[:], BX, gam[:], Bg, bet[:], Bb)
            P.store("sync", X1[it * 128:(it + 1) * 128, :], X[:], BX)


def phase_moe(P, l, NT, X1, XOUT, WGUB, WDNB, w):
    NTS = min(1024, NT)
    QT = min(512, NTS)
    nsup = NT // NTS
    tps = NTS // 128
    with P.phase():
        identf, Bidf = make_ident(P, F32, "me_identf")
        rwf = P.sb("me_rw", [128, 8, NE], F32); Brwf = Buf("me_rw")
        P.load("sync", rwf[:], w["router_w"][l].rearrange("(k p) e -> p k e", p=128), Brwf)
        rbias, Brb = bcast_load(P, "me_rb", w["router_b"][l], NE)
        bdn = P.sb("me_bdn", [NE, D], F32); Bbdn = Buf("me_bdn")
        P.load("sync", bdn[:], w["b_dn"][l], Bbdn)
        bgu = P.sb("me_bgu", [128, NE, 8, 2], F32); Bbgu = Buf("me_bgu")
        for e_ in range(NE):
            P.load("sync", bgu[:, e_, :, :], w["b_gu"][l, e_].rearrange("(i p two) -> p i two", p=128, two=2), Bbgu)
        P.op("vector", lambda e: e.tensor_scalar(bgu[:, :, :, 1], bgu[:, :, :, 1], 1.0, None, ALU.add), reads=[Bbgu], writes=[Bbgu])
        gam, Bg = bcast_load(P, "me_g", w["ln2_g"][l], D)
        bet, Bb = bcast_load(P, "me_b", w["ln2_b"][l], D)
        S = LNScratch(P, "me")
        xt = [P.sb("me_xt%d" % i, [128, D], F32) for i in range(2)]
        Bxt = [Buf("me_xt%d" % i) for i in range(2)]
        xTf = P.sb("me_xTf", [128, 8, 128], F32); BxTf = Buf("me_xTf")
        xT = P.sb("me_xT", [128, 8, NTS], BF16); BxT = Buf("me_xT")
        lg = P.sb("me_lg", [128, NE], F32); Blg = Buf("me_lg")
        top8 = P.sb("me_top8", [128, 8], F32); Btop8 = Buf("me_top8")
        msk = P.sb("me_msk", [128, NE], F32); Bmsk = Buf("me_msk")
        ssum = P.sb("me_ssum", [128, 1], F32); Bssum = Buf("me_ssum")
        G = P.sb("me_G", [128, tps, NE], F32); BG = Buf("me_G")
        Gs = P.sb("me_Gs", [128, tps, NE], F32); BGs = Buf("me_Gs")
        GT = P.sb("me_GT", [NE, tps, 128], F32); BGT = Buf("me_GT")
        acc = P.sb("me_acc", [128, tps, D], F32); Bacc = Buf("me_acc")
        wgu = [P.sb("me_wgu%d" % i, [128, 8, 2 * D], BF16) for i in range(2)]
        Bwgu = [Buf("me_wgu%d" % i) for i in range(2)]
        wdn = [P.sb("me_wdn%d" % i, [128, 8, D], BF16) for i in range(2)]
        Bwdn = [Buf("me_wdn%d" % i) for i in range(2)]
        actT = [P.sb("me_actT%d" % i, [128, 8, QT], BF16) for i in range(2)]
        BactT = [Buf("me_actT%d" % i) for i in range(2)]
        glu = P.sb("me_glu", [128, QT], F32); Bglu = Buf("me_glu")
        sgm = P.sb("me_sgm", [128, QT], F32); Bsgm = Buf("me_sgm")
        lin = P.sb("me_lin", [128, QT], F32); Blin = Buf("me_lin")
        pg = [P.ps("me_pg%d" % i, [128, 512], F32) for i in range(2)]
        Bpg = [Buf("me_pg%d" % i) for i in range(2)]
        pl = [P.ps("me_pl%d" % i, [128, 512], F32) for i in range(2)]
        Bpl = [Buf("me_pl%d" % i) for i in range(2)]
        pd = [P.ps("me_pd%d" % i, [128, 512], F32) for i in range(2)]
        Bpd = [Buf("me_pd%d" % i) for i in range(2)]
        px = P.ps("me_px", [128, 8, 128], F32); Bpx = Buf("me_px")

        def w_pieces(e_, i):
            def f1():
                P.load("sync", wgu[i][:], WGUB[e_].rearrange("(k p) n -> p k n", p=128), Bwgu[i])
            def f2():
                P.load("sync", wdn[i][:], WDNB[e_].rearrange("(k p) n -> p k n", p=128), Bwdn[i])
            yield f1
            yield f2

        def load_w(e_, i):
            for f in w_pieces(e_, i):
                f()

        wi = 0
        pc = 0
        ai = 0
        for sp in range(nsup):
            t0 = sp * NTS
            load_w(0, wi % 2)
            P.load("sync", xt[0][:], X1[t0:t0 + 128, :], Bxt[0])
            for tl in range(tps):
                i = tl % 2
                if tl + 1 < tps:
                    P.load("sync", xt[1 - i][:], X1[t0 + (tl + 1) * 128:t0 + (tl + 2) * 128, :], Bxt[1 - i])
                for k in range(8):
                    P.op("tensor", lambda e: e.transpose(px[:, k, :], xt[i][:, k * 128:(k + 1) * 128], identf[:]), reads=[Bxt[i], Bidf], writes=[Bpx])
                P.op("vector", lambda e: e.tensor_copy(xTf[:], px[:]), reads=[Bpx], writes=[BxTf])
                P.op("scalar", lambda e: e.activation(xT[:, :, tl * 128:(tl + 1) * 128], xTf[:], AF.Copy), reads=[BxTf], writes=[BxT])
                for k in range(8):
                    P.op("tensor", lambda e: e.matmul(px[:, 0, 0:NE], xTf[:, k, :], rwf[:, k, :], start=(k == 0), stop=(k == 7)),
                         reads=[BxTf, Brwf], writes=[Bpx])
                P.op("vector", lambda e: e.tensor_tensor(lg[:], px[:, 0, 0:NE], rbias[:], ALU.add), reads=[Bpx, Brb], writes=[Blg])
                P.op("vector", lambda e: e.max(out=top8[:], in_=lg[:]), reads=[Blg], writes=[Btop8])
                P.op("vector", lambda e: e.tensor_scalar(msk[:], lg[:], top8[:, 3:4], None, ALU.is_ge), reads=[Blg, Btop8], writes=[Bmsk])
                P.op("vector", lambda e: e.tensor_scalar(top8[:, 0:1], top8[:, 0:1], -1.0, None, ALU.mult), reads=[Btop8], writes=[Btop8])
                P.op("scalar", lambda e: e.activation(lg[:], lg[:], AF.Exp, bias=top8[:, 0:1], scale=1.0), reads=[Blg, Btop8], writes=[Blg])
                P.op("vector", lambda e: e.tensor_tensor(lg[:], lg[:], msk[:], ALU.mult), reads=[Blg, Bmsk], writes=[Blg])
                P.op("vector", lambda e: e.tensor_reduce(ssum[:], lg[:], AX.X, ALU.add), reads=[Blg], writes=[Bssum])
                P.op("vector", lambda e: e.reciprocal(ssum[:], ssum[:]), reads=[Bssum], writes=[Bssum])
                P.op("vector", lambda e: e.tensor_scalar(G[:, tl, :], lg[:], ssum[:, 0:1], None, ALU.mult), reads=[Blg, Bssum], writes=[BG])
                P.op("vector", lambda e: e.tensor_scalar(Gs[:, tl, :], lg[:], ssum[:, 0:1], 1.0 / 1.702, ALU.mult, ALU.mult), reads=[Blg, Bssum], writes=[BGs])
                P.op("tensor", lambda e: e.transpose(px[0:NE, 1, :], G[:, tl, :], identf[:]), reads=[BG, Bidf], writes=[Bpx])
                P.op("vector", lambda e: e.tensor_copy(GT[:, tl, :], px[0:NE, 1, :]), reads=[Bpx], writes=[BGT])
            for e_ in range(NE):
                ww = wi % 2
                pieces = list(w_pieces(e_ + 1, (wi + 1) % 2)) if e_ + 1 < NE else []
                nslots = (NTS // QT) * 8
                pps = -(-len(pieces) // nslots) if pieces else 0
                WG, BWG, WD, BWD = wgu[ww], Bwgu[ww], wdn[ww], Bwdn[ww]
                for q in range(NTS // QT):
                    A, BA = actT[ai % 2], BactT[ai % 2]
                    ai += 1
                    for fc in range(8):
                        pgg, Bpgg = pg[pc % 2], Bpg[pc % 2]
                        pll, Bpll = pl[pc % 2], Bpl[pc % 2]
                        pc += 1
                        for k in range(8):
                            P.op("tensor", lambda e: e.matmul(pgg[:, 0:QT], WG[:, k, fc * 256:(fc + 1) * 256:2], xT[:, k, q * QT:(q + 1) * QT],
                                                              start=(k == 0), stop=(k == 7)), reads=[BWG, BxT], writes=[Bpgg])
                        for k in range(8):
                            P.op("tensor", lambda e: e.matmul(pll[:, 0:QT], WG[:, k, fc * 256 + 1:(fc + 1) * 256:2], xT[:, k, q * QT:(q + 1) * QT],
                                                              start=(k == 0), stop=(k == 7)), reads=[BWG, BxT], writes=[Bpll])
                        P.op("vector", lambda e: e.tensor_scalar(glu[:], pgg[:, 0:QT], bgu[:, e_, fc, 0:1], 7.0, ALU.add, ALU.min),
                             reads=[Bpgg, Bbgu], writes=[Bglu])
                        P.op("scalar", lambda e: e.activation(sgm[:], glu[:], AF.Silu, scale=1.702), reads=[Bglu], writes=[Bsgm])
                        P.op("vector", lambda e: e.tensor_scalar(lin[:], pll[:, 0:QT], bgu[:, e_, fc, 1:2], -6.0, ALU.add, ALU.max),
                             reads=[Bpll, Bbgu], writes=[Blin])
                        P.op("vector", lambda e: e.scalar_tensor_tensor(A[:, fc, :], lin[:], 8.0, sgm[:], ALU.min, ALU.mult),
                             reads=[Blin, Bsgm], writes=[BA])
                        for _ in range(pps):
                            if pieces:
                                pieces.pop(0)()
                    for t4 in range(QT // 128):
                        tl = q * (QT // 128) + t4
                        for half in range(2):
                            pdd, Bpdd = pd[pc % 2], Bpd[pc % 2]
                            pc += 1
                            for fc in range(8):
                                P.op("tensor", lambda e: e.matmul(pdd[:], A[:, fc, t4 * 128:(t4 + 1) * 128], WD[:, fc, half * 512:(half + 1) * 512],
                                                                  start=(fc == 0), stop=(fc == 7)), reads=[BA, BWD], writes=[Bpdd])
                            asl = acc[:, tl, half * 512:(half + 1) * 512]
                            if e_ == 0:
                                P.op("vector", lambda e: e.tensor_scalar(asl, pdd[:], Gs[:, tl, e_:e_ + 1], None, ALU.mult),
                                     reads=[Bpdd, BGs], writes=[Bacc])
                            else:
                                P.op("vector", lambda e: e.scalar_tensor_tensor(asl, pdd[:], Gs[:, tl, e_:e_ + 1], asl, ALU.mult, ALU.add),
                                     reads=[Bpdd, BGs, Bacc], writes=[Bacc])
                while pieces:
                    pieces.pop(0)()
                wi += 1
            for tl in range(tps):
                i = tl % 2
                r0 = t0 + tl * 128
                P.load("sync", xt[i][:], X1[r0:r0 + 128, :], Bxt[i])
                X, BX = xt[i], Bxt[i]
                for half in range(2):
                    P.op("tensor", lambda e: e.matmul(px[:, half * 4:(half + 1) * 4, :].rearrange("p a b -> p (a b)"), GT[:, tl, :],
                                                      bdn[:, half * 512:(half + 1) * 512], start=True, stop=True), reads=[BGT, Bbdn], writes=[Bpx])
                P.op("vector", lambda e: e.tensor_tensor(acc[:, tl, :], acc[:, tl, :], px[:].rearrange("p a b -> p (a b)"), ALU.add),
                     reads=[Bacc, Bpx], writes=[Bacc])
                P.op("vector", lambda e: e.scalar_tensor_tensor(X[:], X[:], DN_ALPHA, acc[:, tl, :], ALU.mult, ALU.add), reads=[BX, Bacc], writes=[BX])
                layernorm(P, S, X[:], BX, gam[:], Bg, bet[:], Bb)
                P.store("sync", XOUT[r0:r0 + 128, :], X[:], BX)


def wcast_pieces(P, l, WGUB, WDNB, w, engs=("scalar", "vector", "gpsimd")):
    NS = 4
    st = [P.sb("wc_st%d" % i, [128, 2 * D], F32) for i in range(NS)]
    Bst = [Buf("wc_st%d" % i) for i in range(NS)]
    ob = [P.sb("wc_ob%d" % i, [128, 2 * D], BF16) for i in range(NS)]
    Bob = [Buf("wc_ob%d" % i) for i in range(NS)]
    cnt = [0]

    def cast(i):
        en = engs[cnt[0] % len(engs)]
        if en == "scalar":
            P.op("scalar", lambda e: e.activation(ob[i][:], st[i][:], AF.Copy), reads=[Bst[i]], writes=[Bob[i]])
        else:
            P.op(en, lambda e: e.tensor_copy(ob[i][:], st[i][:]), reads=[Bst[i]], writes=[Bob[i]])

    for e_ in range(NE):
        for k in range(8):
            def f(e_=e_, k=k):
                i = cnt[0] % NS
                P.load("sync", st[i][:], w["w_gu"][l, e_, k * 128:(k + 1) * 128, :], Bst[i])
                cast(i)
                P.store("sync", WGUB[e_, k * 128:(k + 1) * 128, :], ob[i][:], Bob[i])
                cnt[0] += 1
            yield f
        for k2 in range(4):
            def f(e_=e_, k2=k2):
                i = cnt[0] % NS
                P.load("sync", st[i][:].rearrange("p (two n) -> p two n", two=2),
                       w["w_dn"][l, e_, k2 * 256:(k2 + 1) * 256, :].rearrange("(two p) n -> p two n", p=128), Bst[i])
                cast(i)
                P.store("sync", WDNB[e_, k2 * 256:(k2 + 1) * 256, :].rearrange("(two p) n -> p two n", p=128),
                        ob[i][:].rearrange("p (two n) -> p two n", two=2), Bob[i])
                cnt[0] += 1
            yield f


def phase_wcast(P, l, WGUB, WDNB, w):
    with P.phase():
        for f in wcast_pieces(P, l, WGUB, WDNB, w):
            f()
```
